# Optimizing a Trainium2 kernel written in Bass

```python
import math
import jax
import jax.numpy as jnp
from jax import lax
import numpy as np

D_MODEL = 1024
BATCH = 8
SEQ = 2048
DEPTH = 2

CTX_LEN = 256
GRID_W = 64
N_BRANCH = 4
BRANCH_W = D_MODEL // N_BRANCH

MLA_HEADS = BRANCH_W // 64
MLA_NOPE = 64
MLA_ROPE = 32
MLA_V = BRANCH_W // MLA_HEADS
MLA_Q_RANK = D_MODEL // 4
MLA_KV_RANK = D_MODEL // 8

NA_HEADS = BRANCH_W // 64
NA_DH = BRANCH_W // NA_HEADS
NA_WIN_R = 8
NA_WIN_C = 16

S5_GROUP = 16
S5_GROUPS = BRANCH_W // S5_GROUP
S5_STATE = 64

M2_HEADS = BRANCH_W // 64
M2_HEADDIM = BRANCH_W // M2_HEADS
M2_STATE = 128
M2_GROUPS = 2
M2_CONV = 4
M2_CHUNK = 128
M2_CONV_CH = BRANCH_W + 2 * M2_GROUPS * M2_STATE

N_EXPERTS = 16
EXPERT_FF = D_MODEL
CAPACITY_FACTOR = 2

Q_BLOCK = 128
ROPE_THETA = 10000.0
EPS = 1e-6

IN_SPLITS = (MLA_Q_RANK, MLA_KV_RANK, MLA_ROPE, 3 * BRANCH_W, BRANCH_W, BRANCH_W, M2_CONV_CH, 2 * M2_HEADS, N_BRANCH * D_MODEL)
N_IN = sum(IN_SPLITS)

kernel_name = 'hybrid_mla_na_s5_ssd_ecmoe_dit'


def rmsnorm(x, g):
    x32 = x.astype(jnp.float32)
    y = x32 * lax.rsqrt(jnp.mean(x32 * x32, axis=-1, keepdims=True) + EPS)
    return (y * g.astype(jnp.float32)).astype(x.dtype)


def modulate(h, shift, scale):
    return h * (1.0 + scale) + shift


def split_cols(p, sizes):
    return jnp.split(p, np.cumsum(sizes)[:-1].tolist(), axis=-1)


def rope_1d(x, pos):
    d = x.shape[-1]
    inv_freq = 1.0 / (ROPE_THETA ** (jnp.arange(0, d, 2, dtype=jnp.float32) / d))
    ang = pos.astype(jnp.float32)[:, None] * inv_freq[None, :]
    cos = jnp.concatenate([jnp.cos(ang), jnp.cos(ang)], axis=-1)
    sin = jnp.concatenate([jnp.sin(ang), jnp.sin(ang)], axis=-1)
    x32 = x.astype(jnp.float32)
    x1, x2 = jnp.split(x32, 2, axis=-1)
    rot = jnp.concatenate([-x2, x1], axis=-1)
    return (x32 * cos + rot * sin).astype(x.dtype)


def rope_2d(x, row, col):
    half = x.shape[-1] // 2
    return jnp.concatenate([rope_1d(x[..., :half], row), rope_1d(x[..., half:], col)], axis=-1)


def split_heads(t, n_heads):
    b, s, _ = t.shape
    return t.reshape(b, s, n_heads, -1).transpose(0, 2, 1, 3)


def merge_heads(t):
    b, h, s, d = t.shape
    return t.transpose(0, 2, 1, 3).reshape(b, s, h * d)


def block_attention(q, k, v, scale):
    b, h, t, d = q.shape
    nb = t // Q_BLOCK
    q_blocks = q.reshape(b, h, nb, Q_BLOCK, d).transpose(2, 0, 1, 3, 4)

    def one_block(qb):
        s = jnp.einsum('bhqd,bhkd->bhqk', qb, k).astype(jnp.float32) * scale
        p = jax.nn.softmax(s, axis=-1).astype(v.dtype)
        return jnp.einsum('bhqk,bhkd->bhqd', p, v)

    o = lax.map(one_block, q_blocks)
    return o.transpose(1, 2, 0, 3, 4).reshape(b, h, t, v.shape[-1])


def mla_mixer(cq_lat, ckv_lat, kr_lat, cq_ctx, ckv_ctx, kr_ctx, g_cq, g_ckv, w_uq, w_ukv, row, col, ctx_out):
    def keys_values(ckv, kr):
        b, t, _ = ckv.shape
        kvh = (rmsnorm(ckv, g_ckv) @ w_ukv).reshape(b, t, MLA_HEADS, MLA_NOPE + MLA_V)
        k_rope = jnp.broadcast_to(kr[:, :, None, :], (b, t, MLA_HEADS, MLA_ROPE))
        k = jnp.concatenate([kvh[..., :MLA_NOPE], k_rope], axis=-1)
        return k.transpose(0, 2, 1, 3), kvh[..., MLA_NOPE:].transpose(0, 2, 1, 3)

    def queries(cq):
        return split_heads(rmsnorm(cq, g_cq) @ w_uq, MLA_HEADS)

    scale = (MLA_NOPE + MLA_ROPE) ** -0.5
    k_lat, v_lat = keys_values(ckv_lat, rope_2d(kr_lat, row, col))
    k_ctx, v_ctx = keys_values(ckv_ctx, kr_ctx)
    q_lat = queries(cq_lat)
    q_lat = jnp.concatenate([q_lat[..., :MLA_NOPE], rope_2d(q_lat[..., MLA_NOPE:], row, col)], axis=-1)
    o_lat = block_attention(q_lat, jnp.concatenate([k_lat, k_ctx], axis=2),
                            jnp.concatenate([v_lat, v_ctx], axis=2), scale)
    o_ctx = merge_heads(block_attention(queries(cq_ctx), k_ctx, v_ctx, scale)) if ctx_out else None
    return merge_heads(o_lat), o_ctx


def na_mixer(qkv_lat, qkv_ctx, rpb, ctx_out):
    b, s, _ = qkv_lat.shape
    rows = s // GRID_W
    wr = min(NA_WIN_R, rows)
    scale = NA_DH ** -0.5
    q, k, v = [split_heads(t, NA_HEADS) for t in jnp.split(qkv_lat, 3, axis=-1)]
    qc, kc, vc = [split_heads(t, NA_HEADS) for t in jnp.split(qkv_ctx, 3, axis=-1)]

    def to_grid(t):
        return t.reshape(b, NA_HEADS, rows, GRID_W, NA_DH)

    kg, vg = to_grid(k), to_grid(v)
    q_rows = to_grid(q).transpose(2, 0, 1, 3, 4)
    row_start = jnp.clip(jnp.arange(rows) - wr // 2, 0, rows - wr)
    qcol = jnp.arange(GRID_W)
    col_idx = jnp.clip(qcol - NA_WIN_C // 2, 0, GRID_W - NA_WIN_C)[:, None] + jnp.arange(NA_WIN_C)[None, :]
    col_off = col_idx - qcol[:, None] + (NA_WIN_C - 1)
    rpb32 = rpb.astype(jnp.float32)
    n_win = wr * NA_WIN_C

    def one_row(args):
        r, qr = args
        rs = row_start[r]
        kw = lax.dynamic_slice_in_dim(kg, rs, wr, axis=2)[:, :, :, col_idx]
        vw = lax.dynamic_slice_in_dim(vg, rs, wr, axis=2)[:, :, :, col_idx]
        row_off = rs + jnp.arange(wr) - r + (NA_WIN_R - 1)
        bias = rpb32[:, row_off[None, :, None], col_off[:, None, :]]
        s_win = jnp.einsum('bhqd,bhrqcd->bhqrc', qr, kw).astype(jnp.float32) * scale + bias[None]
        s_ctx = jnp.einsum('bhqd,bhkd->bhqk', qr, kc).astype(jnp.float32) * scale
        s_all = jnp.concatenate([s_win.reshape(b, NA_HEADS, GRID_W, n_win), s_ctx], axis=-1)
        p = jax.nn.softmax(s_all, axis=-1).astype(vg.dtype)
        p_win = p[..., :n_win].reshape(b, NA_HEADS, GRID_W, wr, NA_WIN_C)
        return (jnp.einsum('bhqrc,bhrqcd->bhqd', p_win, vw)
                + jnp.einsum('bhqk,bhkd->bhqd', p[..., n_win:], vc))

    o = lax.map(one_row, (jnp.arange(rows), q_rows))
    o_lat = o.transpose(1, 0, 3, 2, 4).reshape(b, s, NA_HEADS * NA_DH)
    o_ctx = merge_heads(block_attention(qc, kc, vc, scale)) if ctx_out else None
    return o_lat, o_ctx


def _linear_combine(e1, e2):
    a1, b1 = e1
    a2, b2 = e2
    return a1 * a2, a2 * b1 + b2


def s5_scan(abar, bbar, u, h0, reverse):
    bu = jnp.einsum('gpc,btgc->btgp', bbar, u.astype(jnp.complex64))
    a = jnp.broadcast_to(abar, bu.shape)
    a_cum, h = lax.associative_scan(_linear_combine, (a, bu), reverse=reverse, axis=1)
    return h + a_cum * h0[:, None]


def s5_mixer(u_lat, u_ctx, a_re, a_im, log_step, b_re, b_im, c_re, c_im, d_skip, w_glu, b_glu, ctx_out):
    f32 = jnp.float32

    def groups(u):
        return u.reshape(u.shape[0], u.shape[1], S5_GROUPS, S5_GROUP)

    def readout(cm, h):
        y = jnp.real(jnp.einsum('gcp,btgp->btgc', cm, h))
        return y.reshape(h.shape[0], h.shape[1], BRANCH_W)

    def glu(y):
        z = jax.nn.gelu(y)
        return z * jax.nn.sigmoid(z @ w_glu + b_glu)

    ul, uc = groups(u_lat), groups(u_ctx)
    d32 = d_skip.astype(f32)
    y_lat = d32 * u_lat.astype(f32)
    y_ctx = d32 * u_ctx.astype(f32) if ctx_out else None
    h_zero = jnp.zeros((u_ctx.shape[0], S5_GROUPS, S5_STATE), jnp.complex64)
    for d, rev in ((0, False), (1, True)):
        a = lax.complex(a_re[d].astype(f32), a_im[d].astype(f32))
        abar = jnp.exp(jnp.exp(log_step[d].astype(f32))[:, None] * a)
        bbar = ((abar - 1.0) / a)[:, :, None] * lax.complex(b_re[d].astype(f32), b_im[d].astype(f32))
        cm = lax.complex(c_re[d].astype(f32), c_im[d].astype(f32))
        h_ctx = s5_scan(abar, bbar, uc, h_zero, rev)
        h_lat = s5_scan(abar, bbar, ul, h_ctx[:, 0] if rev else h_ctx[:, -1], rev)
        y_lat = y_lat + readout(cm, h_lat)
        if ctx_out:
            y_ctx = y_ctx + readout(cm, h_ctx)
    o_ctx = glu(y_ctx).astype(u_ctx.dtype) if ctx_out else None
    return glu(y_lat).astype(u_lat.dtype), o_ctx


def dw_conv_centred(x, w, b):
    k = w.shape[0]
    y = lax.conv_general_dilated(x, w[:, None, :].astype(x.dtype), window_strides=(1,),
                                 padding=[(k // 2, k - 1 - k // 2)],
                                 dimension_numbers=('NWC', 'WIO', 'NWC'),
                                 feature_group_count=x.shape[-1])
    return y + b


def ssd_chunked(x, dt, a, bm, cm, h0):
    b, t, h, p = x.shape
    nc = t // M2_CHUNK
    rep = h // bm.shape[2]
    bh = jnp.repeat(bm, rep, axis=2).reshape(b, nc, M2_CHUNK, h, -1)
    ch = jnp.repeat(cm, rep, axis=2).reshape(b, nc, M2_CHUNK, h, -1)
    xdt = (x * dt[..., None]).reshape(b, nc, M2_CHUNK, h, p)
    a_cum = jnp.cumsum((dt * a).reshape(b, nc, M2_CHUNK, h), axis=2)
    causal = jnp.tril(jnp.ones((M2_CHUNK, M2_CHUNK), dtype=bool))[None, None, :, :, None]
    seg = a_cum[:, :, :, None, :] - a_cum[:, :, None, :, :]
    decay = jnp.exp(jnp.where(causal, seg, -jnp.inf))
    y_diag = jnp.einsum('bclsh,bcshp->bclhp', jnp.einsum('bclhn,bcshn->bclsh', ch, bh) * decay, xdt)
    states = jnp.einsum('bcsh,bcshn,bcshp->bchpn', jnp.exp(a_cum[:, :, -1:, :] - a_cum), bh, xdt)
    chunk_decay = jnp.exp(a_cum[:, :, -1, :])

    def step(state, inp):
        s_c, d_c = inp
        return state * d_c[:, :, None, None] + s_c, state

    h_last, h_in = lax.scan(step, h0, (jnp.moveaxis(states, 1, 0), jnp.moveaxis(chunk_decay, 1, 0)))
    y_off = jnp.einsum('bclhn,cbhpn,bclh->bclhp', ch, h_in, jnp.exp(a_cum))
    return (y_diag + y_off).reshape(b, t, h, p), h_last


def ssd_final_state(x, dt, a, bm):
    rep = x.shape[2] // bm.shape[2]
    a_cum = jnp.cumsum(dt * a, axis=1)
    w = jnp.exp(a_cum[:, -1:, :] - a_cum) * dt
    return jnp.einsum('bth,bthn,bthp->bhpn', w, jnp.repeat(bm, rep, axis=2), x)


def _flip(t, rev):
    return jnp.flip(t, axis=1) if rev else t


def m2_mixer(z_lat, xbc_lat, dt_lat, z_ctx, xbc_ctx, dt_ctx, conv_w, conv_b, a_log, dt_bias, d_skip, g_norm, ctx_out):
    f32 = jnp.float32

    def prep(xbc, dt_raw):
        b, t, _ = xbc.shape
        xbc = jax.nn.silu(dw_conv_centred(xbc, conv_w, conv_b)).astype(f32)
        xs, bm, cm = split_cols(xbc, (BRANCH_W, M2_GROUPS * M2_STATE, M2_GROUPS * M2_STATE))
        dt = jax.nn.softplus(dt_raw.astype(f32).reshape(b, t, 2, M2_HEADS) + dt_bias.astype(f32))
        return (xs.reshape(b, t, M2_HEADS, M2_HEADDIM), bm.reshape(b, t, M2_GROUPS, M2_STATE),
                cm.reshape(b, t, M2_GROUPS, M2_STATE), dt)

    def gated_norm(y, z):
        b, t = z.shape[:2]
        return rmsnorm(y.reshape(b, t, BRANCH_W) * jax.nn.silu(z.astype(f32)), g_norm).astype(z.dtype)

    xl, bl, cl, dtl = prep(xbc_lat, dt_lat)
    xc, bc, cc, dtc = prep(xbc_ctx, dt_ctx)
    d32 = d_skip.astype(f32)[:, None]
    y_lat = d32 * xl
    y_ctx = d32 * xc if ctx_out else None
    h_zero = jnp.zeros((xc.shape[0], M2_HEADS, M2_HEADDIM, M2_STATE), f32)
    for d in (0, 1):
        rev = d == 1
        a = -jnp.exp(a_log[d].astype(f32))
        if ctx_out:
            yc, h_ctx = ssd_chunked(_flip(xc, rev), _flip(dtc[:, :, d], rev), a, _flip(bc, rev), _flip(cc, rev), h_zero)
            y_ctx = y_ctx + _flip(yc, rev)
        else:
            h_ctx = ssd_final_state(_flip(xc, rev), _flip(dtc[:, :, d], rev), a, _flip(bc, rev))
        yl, _ = ssd_chunked(_flip(xl, rev), _flip(dtl[:, :, d], rev), a, _flip(bl, rev), _flip(cl, rev), h_ctx)
        y_lat = y_lat + _flip(yl, rev)
    o_ctx = gated_norm(y_ctx, z_ctx) if ctx_out else None
    return gated_norm(y_lat, z_lat), o_ctx


def token_mixer(h_lat, h_ctx, w_in, mla_g_cq, mla_g_ckv, mla_w_uq, mla_w_ukv, na_rpb,
                s5_a_re, s5_a_im, s5_log_step, s5_b_re, s5_b_im, s5_c_re, s5_c_im, s5_d, s5_w_glu, s5_b_glu,
                m2_conv_w, m2_conv_b, m2_a_log, m2_dt_bias, m2_d, m2_g_norm, w_branch, w_out, ctx_out):
    s = h_lat.shape[1]
    pos = jnp.arange(s)
    row, col = pos // GRID_W, pos % GRID_W
    pl = split_cols(h_lat @ w_in, IN_SPLITS)
    if ctx_out:
        pc = split_cols(h_ctx @ w_in, IN_SPLITS)
    else:
        pc = split_cols(h_ctx @ w_in[:, :N_IN - N_BRANCH * D_MODEL], IN_SPLITS[:-1])
    o_mla, oc_mla = mla_mixer(pl[0], pl[1], pl[2], pc[0], pc[1], pc[2], mla_g_cq, mla_g_ckv,
                              mla_w_uq, mla_w_ukv, row, col, ctx_out)
    o_na, oc_na = na_mixer(pl[3], pc[3], na_rpb, ctx_out)
    o_s5, oc_s5 = s5_mixer(pl[4], pc[4], s5_a_re, s5_a_im, s5_log_step, s5_b_re, s5_b_im,
                           s5_c_re, s5_c_im, s5_d, s5_w_glu, s5_b_glu, ctx_out)
    o_m2, oc_m2 = m2_mixer(pl[5], pl[6], pl[7], pc[5], pc[6], pc[7], m2_conv_w, m2_conv_b,
                           m2_a_log, m2_dt_bias, m2_d, m2_g_norm, ctx_out)

    def merge(outs, gate_logits):
        gl = gate_logits.reshape(gate_logits.shape[:-1] + (N_BRANCH, D_MODEL))
        y = jax.nn.sigmoid(gl[..., 0, :]) * (outs[0] @ w_branch[0])
        for j in range(1, N_BRANCH):
            y = y + jax.nn.sigmoid(gl[..., j, :]) * (outs[j] @ w_branch[j])
        return y @ w_out

    y_lat = merge((o_mla, o_na, o_s5, o_m2), pl[8])
    y_ctx = merge((oc_mla, oc_na, oc_s5, oc_m2), pc[8]) if ctx_out else None
    return y_lat, y_ctx


def expert_choice_ffn(h, w_router, w_gate, w_up, w_down):
    b, t, _ = h.shape
    cap = CAPACITY_FACTOR * t // N_EXPERTS
    aff = jax.nn.softmax(jnp.einsum('btd,de->bte', h, w_router).astype(jnp.float32), axis=-1)
    g, idx = lax.top_k(jnp.swapaxes(aff, 1, 2), cap)
    bidx = jnp.arange(b)[:, None, None]
    xs = h[bidx, idx]
    act = jax.nn.silu(jnp.einsum('becd,edf->becf', xs, w_gate)) * jnp.einsum('becd,edf->becf', xs, w_up)
    ys = jnp.einsum('becf,efd->becd', act, w_down) * g[..., None].astype(h.dtype)
    return jnp.zeros_like(h).at[bidx, idx].add(ys)


def setup_inputs(seed: int = 0) -> dict:
    key = jax.random.key(seed)
    keys = iter(jax.random.split(key, 48))

    def normal(shape, std):
        return jax.random.normal(next(keys), shape, jnp.float32) * std

    def gain(shape):
        return 1.0 + normal(shape, 0.02)

    def log_uniform(shape, lo, hi):
        return jax.random.uniform(next(keys), shape, jnp.float32, minval=math.log(lo), maxval=math.log(hi))

    L = DEPTH
    W = BRANCH_W
    dt0 = jnp.exp(log_uniform((L, 2, M2_HEADS), 1e-3, 1e-1))
    return {
        'x': normal((BATCH, SEQ, D_MODEL), 1.0),
        'c': normal((BATCH, D_MODEL), 1.0),
        'ctx': normal((BATCH, CTX_LEN, D_MODEL), 1.0),
        'c_ctx': normal((D_MODEL,), 1.0),
        'w_ada': normal((L, D_MODEL, 6 * D_MODEL), 0.2 * D_MODEL ** -0.5),
        'b_ada': normal((L, 6 * D_MODEL), 0.02),
        'g_pre_mix': gain((L, D_MODEL)),
        'g_post_mix': gain((L, D_MODEL)),
        'g_pre_ffn': gain((L, D_MODEL)),
        'g_post_ffn': gain((L, D_MODEL)),
        'w_in': normal((L, D_MODEL, N_IN), D_MODEL ** -0.5),
        'mla_g_cq': gain((L, MLA_Q_RANK)),
        'mla_g_ckv': gain((L, MLA_KV_RANK)),
        'mla_w_uq': normal((L, MLA_Q_RANK, MLA_HEADS * (MLA_NOPE + MLA_ROPE)), MLA_Q_RANK ** -0.5),
        'mla_w_ukv': normal((L, MLA_KV_RANK, MLA_HEADS * (MLA_NOPE + MLA_V)), MLA_KV_RANK ** -0.5),
        'na_rpb': normal((L, NA_HEADS, 2 * NA_WIN_R - 1, 2 * NA_WIN_C - 1), 0.1),
        's5_a_re': -0.5 + normal((L, 2, S5_GROUPS, S5_STATE), 0.01),
        's5_a_im': jnp.pi * jnp.arange(S5_STATE, dtype=jnp.float32) + normal((L, 2, S5_GROUPS, S5_STATE), 0.01),
        's5_log_step': log_uniform((L, 2, S5_GROUPS), 1e-3, 1e-1),
        's5_b_re': normal((L, 2, S5_GROUPS, S5_STATE, S5_GROUP), (2 * S5_GROUP) ** -0.5),
        's5_b_im': normal((L, 2, S5_GROUPS, S5_STATE, S5_GROUP), (2 * S5_GROUP) ** -0.5),
        's5_c_re': normal((L, 2, S5_GROUPS, S5_GROUP, S5_STATE), (2 * S5_STATE) ** -0.5),
        's5_c_im': normal((L, 2, S5_GROUPS, S5_GROUP, S5_STATE), (2 * S5_STATE) ** -0.5),
        's5_d': normal((L, W), 1.0),
        's5_w_glu': normal((L, W, W), W ** -0.5),
        's5_b_glu': normal((L, W), 0.02),
        'm2_conv_w': normal((L, M2_CONV, M2_CONV_CH), M2_CONV ** -0.5),
        'm2_conv_b': normal((L, M2_CONV_CH), 0.02),
        'm2_a_log': jnp.log(jax.random.uniform(next(keys), (L, 2, M2_HEADS), jnp.float32, 1.0, 16.0)),
        'm2_dt_bias': dt0 + jnp.log(-jnp.expm1(-dt0)),
        'm2_d': 1.0 + normal((L, M2_HEADS), 0.1),
        'm2_g_norm': gain((L, W)),
        'w_branch': normal((L, N_BRANCH, W, D_MODEL), W ** -0.5),
        'w_out': normal((L, D_MODEL, D_MODEL), D_MODEL ** -0.5),
        'w_router': normal((L, D_MODEL, N_EXPERTS), D_MODEL ** -0.5),
        'w_gate': normal((L, N_EXPERTS, D_MODEL, EXPERT_FF), D_MODEL ** -0.5),
        'w_up': normal((L, N_EXPERTS, D_MODEL, EXPERT_FF), D_MODEL ** -0.5),
        'w_down': normal((L, N_EXPERTS, EXPERT_FF, D_MODEL), EXPERT_FF ** -0.5),
    }


def reference(x, c, ctx, c_ctx, w_ada, b_ada, g_pre_mix, g_post_mix, g_pre_ffn, g_post_ffn, w_in,
              mla_g_cq, mla_g_ckv, mla_w_uq, mla_w_ukv, na_rpb,
              s5_a_re, s5_a_im, s5_log_step, s5_b_re, s5_b_im, s5_c_re, s5_c_im, s5_d, s5_w_glu, s5_b_glu,
              m2_conv_w, m2_conv_b, m2_a_log, m2_dt_bias, m2_d, m2_g_norm,
              w_branch, w_out, w_router, w_gate, w_up, w_down):
    xc = ctx
    silu_c = jax.nn.silu(c)
    silu_cc = jax.nn.silu(c_ctx)
    for l in range(DEPTH):
        ctx_out = l < DEPTH - 1
        mod = jnp.split(silu_c @ w_ada[l] + b_ada[l], 6, axis=-1)
        shift1, scale1, gate1, shift2, scale2, gate2 = [m[:, None, :] for m in mod]
        cshift1, cscale1, cgate1, cshift2, cscale2, cgate2 = jnp.split(silu_cc @ w_ada[l] + b_ada[l], 6)

        h = modulate(rmsnorm(x, g_pre_mix[l]), shift1, scale1)
        hc = modulate(rmsnorm(xc, g_pre_mix[l]), cshift1, cscale1)
        y, yc = token_mixer(h, hc, w_in[l], mla_g_cq[l], mla_g_ckv[l], mla_w_uq[l], mla_w_ukv[l], na_rpb[l],
                            s5_a_re[l], s5_a_im[l], s5_log_step[l], s5_b_re[l], s5_b_im[l], s5_c_re[l],
                            s5_c_im[l], s5_d[l], s5_w_glu[l], s5_b_glu[l], m2_conv_w[l], m2_conv_b[l],
                            m2_a_log[l], m2_dt_bias[l], m2_d[l], m2_g_norm[l], w_branch[l], w_out[l], ctx_out)
        x = x + gate1 * rmsnorm(y, g_post_mix[l])
        h = modulate(rmsnorm(x, g_pre_ffn[l]), shift2, scale2)
        x = x + gate2 * rmsnorm(expert_choice_ffn(h, w_router[l], w_gate[l], w_up[l], w_down[l]), g_post_ffn[l])

        if ctx_out:
            xc = xc + cgate1 * rmsnorm(yc, g_post_mix[l])
            hc = modulate(rmsnorm(xc, g_pre_ffn[l]), cshift2, cscale2)
            xc = xc + cgate2 * rmsnorm(expert_choice_ffn(hc, w_router[l], w_gate[l], w_up[l], w_down[l]), g_post_ffn[l])
    return x
```

```python
import contextlib
import numpy as np
import concourse.bass as bass
import concourse.mybir as mybir
from concourse.bass_utils import run_bass_kernel_spmd

F32 = mybir.dt.float32
BF16 = mybir.dt.bfloat16
ALU = mybir.AluOpType
AF = mybir.ActivationFunctionType
AX = mybir.AxisListType

D = 1024
SEQ = 2048
CTX = 256
T = SEQ + CTX
NT = T // 128
DEPTH = 2
GRID_W = 64
N_IN = 6568
EPS = 1e-6
NEG = -30000.0
DEVSTOP = None
INORDER = ('pe',)
DEVFLAGS = ''
DEVLAYERS = DEPTH
DEVNEXP = 16


class Buf:
    __slots__ = ("name", "w", "r", "excl", "strictw")

    def __init__(self, name):
        self.name = name
        self.strictw = False
        self.excl = False
        self.w = None
        self.r = {}


class TV:
    __slots__ = ("ap", "bufs")

    def __init__(self, ap, bufs):
        self.ap = ap
        self.bufs = bufs

    def __getitem__(self, idx):
        return TV(self.ap[idx], self.bufs)

    def v(self, ap):
        return TV(ap, self.bufs)

    @property
    def shape(self):
        return self.ap.shape


class Eng:
    def __init__(self, name, eng, sem):
        self.name = name
        self.eng = eng
        self.sem = sem
        self.count = 0
        self.known = {}


class K:
    NDSEM = 20

    def __init__(self, nc, es):
        self.nc = nc
        self.es = es
        self.sems = {}
        self.engs = {}
        for name, eng in (("pe", nc.tensor), ("dve", nc.vector), ("act", nc.scalar),
                          ("pool", nc.gpsimd), ("sp", nc.sync)):
            s = es.enter_context(nc.semaphore("s_" + name))
            self.sems[name] = s
            self.engs[name] = Eng(name, eng, name)
        self.dq = {}
        for q in ("sp", "pool", "act"):
            lst = []
            for i in range(self.NDSEM):
                key = "d_%s_%d" % (q, i)
                self.sems[key] = es.enter_context(nc.semaphore(key))
                lst.append([key, 0])
            self.dq[q] = [lst, 0]
        self.ninstr = 0
        self.nwait = 0
        self.inorder = False
        self.uid = 0

    def sb(self, name, shape, dtype):
        self.uid += 1
        name = "%s_%d" % (name, self.uid)
        t = self.es.enter_context(self.nc.sbuf_tensor(name, list(shape), dtype))
        return TV(t.ap(), [Buf(name)])

    def ps(self, name, shape, dtype=F32):
        self.uid += 1
        name = "%s_%d" % (name, self.uid)
        t = self.es.enter_context(self.nc.psum_tensor(name, list(shape), dtype))
        b = Buf(name)
        b.excl = True
        return TV(t.ap(), [b])

    def dram(self, name, shape, dtype, kind="Internal"):
        t = self.nc.dram_tensor(name, list(shape), dtype, kind=kind)
        return TV(t.ap(), [Buf(name)])

    def split(self, tv, n, sl):
        out = []
        bufs = []
        for i in range(n):
            b = Buf("%s.%d" % (tv.bufs[0].name, i))
            bufs.append(b)
            out.append(TV(tv.ap[sl(i)], [b]))
        tv.bufs = bufs
        return out

    def _need(self, E, ev, waits, strict=False):
        if ev is None:
            return
        sem, val, clock = ev
        if E.known.get(sem, 0) >= val:
            return
        if sem == E.sem and self.inorder and E.name in INORDER and not strict:
            return
        if waits.get(sem, (0, None))[0] < val:
            waits[sem] = (val, clock)

    def _deps(self, E, reads, writes, skip_waw=()):
        waits = {}
        for tv in reads:
            if isinstance(tv, TV):
                for b in tv.bufs:
                    self._need(E, b.w, waits, b.strictw)
                    if b.excl:
                        for sem, (val, clock) in b.r.items():
                            if sem != E.sem:
                                self._need(E, (sem, val, clock), waits)
        for tv in writes:
            for b in tv.bufs:
                if b not in skip_waw:
                    self._need(E, b.w, waits, b.strictw)
                    for sem, (val, clock) in b.r.items():
                        self._need(E, (sem, val, clock), waits)
        for sem, (val, clock) in waits.items():
            if E.known.get(sem, 0) >= val:
                continue
            E.eng.wait_ge(self.sems[sem], val)
            self.nwait += 1
            E.known[sem] = val
            if clock:
                for s2, v2 in clock.items():
                    if E.known.get(s2, 0) < v2:
                        E.known[s2] = v2

    def _mark(self, ev, reads, writes):
        sem, val, clock = ev
        for tv in reads:
            if isinstance(tv, TV):
                for b in tv.bufs:
                    old = b.r.get(sem)
                    if old is None or old[0] < val:
                        b.r[sem] = (val, clock)
        for tv in writes:
            for b in tv.bufs:
                b.w = ev
                b.r = {}
                b.strictw = False

    def ins(self, en, fn, reads, writes, skip_waw=()):
        E = self.engs[en]
        self.inorder = True
        self._deps(E, reads, writes, skip_waw)
        self.inorder = False
        inst = fn(E.eng)
        E.count += 1
        inst.then_inc(self.sems[E.sem], 1)
        clock = dict(E.known)
        if en in INORDER:
            clock[E.sem] = E.count
        ev = (E.sem, E.count, clock)
        self._mark(ev, reads, writes)
        self.ninstr += 1
        return ev

    def dma(self, q, pairs, accum=False):
        if accum:
            q = "pool"
        E = self.engs[q]
        lst, idx = self.dq[q]
        slot = lst[idx % self.NDSEM]
        self.dq[q][1] = idx + 1
        key, total = slot
        reads = [p[1] for p in pairs]
        writes = [p[0] for p in pairs]
        self._deps(E, reads, writes)
        if total > 0 and E.known.get(key, 0) < total:
            E.eng.wait_ge(self.sems[key], total)
            E.known[key] = total
        for o, i in pairs:
            if accum:
                E.eng.dma_start(out=o.ap, in_=i.ap, accum_op=ALU.add).then_inc(self.sems[key], 16)
            else:
                E.eng.dma_start(out=o.ap, in_=i.ap).then_inc(self.sems[key], 16)
            total += 16
        slot[1] = total
        ev = (key, total, dict(E.known))
        self._mark(ev, reads, writes)
        self.ninstr += len(pairs)
        return ev

    def wait_all(self, en, tvs):
        E = self.engs[en]
        self._deps(E, tvs, [])

    def mm(self, out, lhsT, rhs, start=True, stop=True):
        skip = () if start else tuple(out.bufs)
        return self.ins("pe", lambda e: e.matmul(out.ap, lhsT.ap, rhs.ap, start=start, stop=stop),
                        [lhsT, rhs], [out], skip_waw=skip)

    def tr(self, out, in_, ident):
        return self.ins("pe", lambda e: e.transpose(out.ap, in_.ap, ident.ap), [in_, ident], [out])

    def act(self, out, in_, func, bias=None, scale=1.0, accum=None, en="act"):
        kw = {}
        rd = [in_]
        if bias is not None:
            kw["bias"] = bias.ap if isinstance(bias, TV) else bias
            rd.append(bias)
        if isinstance(scale, TV):
            kw["scale"] = scale.ap
            rd.append(scale)
        else:
            kw["scale"] = scale
        wr = [out]
        if accum is not None:
            kw["accum_out"] = accum.ap
            wr.append(accum)
        ev = self.ins("act", lambda e: e.activation(out.ap, in_.ap, func, **kw), rd, wr)
        if accum is not None:
            for b in accum.bufs:
                b.strictw = True
        return ev

    def tt(self, en, out, in0, in1, op):
        return self.ins(en, lambda e: e.tensor_tensor(out=out.ap, in0=in0.ap, in1=in1.ap, op=op),
                        [in0, in1], [out])

    def ts(self, en, out, in0, s1, op0, s2=None, op1=None, accum=None):
        a1 = s1.ap if isinstance(s1, TV) else s1
        a2 = s2.ap if isinstance(s2, TV) else s2
        kw = {}
        wr = [out]
        if op1 is not None:
            kw["op1"] = op1
        if accum is not None:
            kw["accum_out"] = accum.ap
            wr.append(accum)
        return self.ins(en, lambda e: e.tensor_scalar(out=out.ap, in0=in0.ap, scalar1=a1, scalar2=a2,
                                                      op0=op0, **kw), [in0, s1, s2], wr)

    def stt(self, out, in0, sc, in1, op0, op1, en="dve"):
        a = sc.ap if isinstance(sc, TV) else sc
        return self.ins(en, lambda e: e.scalar_tensor_tensor(out=out.ap, in0=in0.ap, scalar=a, in1=in1.ap,
                                                             op0=op0, op1=op1), [in0, sc, in1], [out])

    def copy(self, en, out, in_):
        if en == "act":
            return self.ins("act", lambda e: e.copy(out.ap, in_.ap), [in_], [out])
        return self.ins(en, lambda e: e.tensor_copy(out=out.ap, in_=in_.ap), [in_], [out])

    def recip(self, out, in_):
        return self.ins("dve", lambda e: e.reciprocal(out=out.ap, in_=in_.ap), [in_], [out])

    def memset(self, en, out, val):
        return self.ins(en, lambda e: e.memset(out.ap, val), [], [out])

    def scan(self, out, d0, d1, init, op0, op1):
        a = init.ap if isinstance(init, TV) else init
        return self.ins("dve", lambda e: e.tensor_tensor_scan(out=out.ap, data0=d0.ap, data1=d1.ap, initial=a,
                                                              op0=op0, op1=op1), [d0, d1, init], [out])


def col_layout(v):
    v = np.asarray(v, np.float32)
    return np.ascontiguousarray(v.reshape(-1, 128).T)


def host_consts():
    c = {}
    c["ident"] = np.eye(128, dtype=np.float32)
    c["cosf"], c["sinf"] = rope_tables()
    c["jmat"] = np.ascontiguousarray(np.eye(128, dtype=np.float32)[::-1])
    c["m2msk"] = host_m2_masks()
    c.update(host_ffn_consts())
    c.update(host_ffn_consts2())
    return c


def host_layer_inputs(inp, l):
    o = {}
    o["w_ada"] = np.ascontiguousarray(inp["w_ada"][l])
    o["bada"] = col_layout(inp["b_ada"][l])
    g = np.stack([col_layout(inp[n][l]) for n in ("g_pre_mix", "g_post_mix", "g_pre_ffn", "g_post_ffn")], 1)
    o["gvec"] = np.ascontiguousarray(g)
    o["w_in"] = np.ascontiguousarray(inp["w_in"][l])
    return o


class Ctx:
    pass


def build_program(dbg=()):
    nc = bass.Bass("TRN2", target_bir_lowering=False)
    es = contextlib.ExitStack()
    k = K(nc, es)
    C = Ctx()
    C.nc, C.k, C.es, C.dbg = nc, k, es, {}
    C.dbg_names = dbg

    def din(name, shape, dtype=F32):
        return k.dram(name, shape, dtype, kind="ExternalInput")

    C.x_in = din("x", [SEQ, D])
    C.ctx_in = din("ctx", [CTX, D])
    C.cvec = din("cvec", [128, 8, 2])
    C.ident_in = din("ident", [128, 128])
    C.cosf_in = din("cosf", [128, T], BF16)
    C.jmat_in = din("jmat", [128, 128])
    C.m2msk_in = din("m2msk", [128, 4, 128])
    C.selT_in = din("selT", [128, 16, 128])
    C.iotac_in = din("iotac", [128, CSEL])
    C.iotap_in = din("iotap", [128, 3])
    C.facc = k.dram("facc", [T, D], F32)
    C.facc_t = k.split(C.facc, NT, lambda i: (slice(i * 128, (i + 1) * 128), slice(None)))
    C.sinf_in = din("sinf", [128, T], BF16)
    C.otd = k.dram("otd", [8, 128, T], BF16)
    C.ytd2 = k.dram("ytd2", [8, 128, T], BF16)
    C.ytd = k.dram("ytd", [T, D], BF16)
    C.ytd_t = k.split(C.ytd, NT, lambda i: (slice(i * 128, (i + 1) * 128), slice(None)))
    C.L = []
    for l in range(DEPTH):
        Lw = Ctx()
        Lw.w_ada = din("w_ada%d" % l, [D, 6 * D])
        Lw.bada = din("bada%d" % l, [128, 48])
        Lw.gvec = din("gvec%d" % l, [128, 4, 8])
        Lw.w_in = din("w_in%d" % l, [D, N_IN])
        Lw.wfm = din("wfm%d" % l, [D, NFM])
        Lw.wtm = din("wtm%d" % l, [D, 520])
        Lw.wuq = din("wuq%d" % l, [256, 1024])
        Lw.wukv = din("wukv%d" % l, [128, 768])
        Lw.gmla = din("gmla%d" % l, [128, 3])
        Lw.nab = din("nab%d" % l, [6, 4, 128, 5, 128])
        Lw.s5par = din("s5par%d" % l, [3, 2048])
        Lw.m2vec = din("m2vec%d" % l, [128, 6, 5])
        Lw.wbr = din("wbr%d" % l, [1024, 1024])
        Lw.w_router = din("w_router%d" % l, [D, 16])
        Lw.w_gate = din("w_gate%d" % l, [16, D, D])
        Lw.w_up = din("w_up%d" % l, [16, D, D])
        Lw.w_down = din("w_down%d" % l, [16, D, D])
        Lw.wout = din("wout%d" % l, [1024, 1024])
        Lw.m2row = din("m2row%d" % l, [1, 276])
        Lw.s5BT = din("s5BT%d" % l, [128, 2, 8, 2, 128])
        Lw.s5CT = din("s5CT%d" % l, [128, 2, 8, 2, 128])
        Lw.s5vec = din("s5vec%d" % l, [128, 2, 2])
        Lw.s5wglu = din("s5wglu%d" % l, [256, 256])
        Lw.s5wu = din("s5wu%d" % l, [D, 256])
        C.L.append(Lw)
    C.out = k.dram("out", [SEQ, D], F32, kind="ExternalOutput")
    C.xres = k.dram("xres", [T, D], F32)
    C.xres_t = k.split(C.xres, NT, lambda i: (slice(i * 128, (i + 1) * 128), slice(None)))

    C.ident_f = k.sb("ident_f", [128, 128], F32)
    C.ident_b = k.sb("ident_b", [128, 128], BF16)
    C.ones_f = k.sb("ones_f", [128, 128], F32)
    C.ones_b = k.sb("ones_b", [128, 128], BF16)
    k.dma("sp", [(C.ident_f, C.ident_in)])
    k.copy("dve", C.ident_b, C.ident_f)
    k.memset("dve", C.ones_f, 1.0)
    k.memset("dve", C.ones_b, 1.0)
    C.ps = [k.ps("ps%d" % i, [128, 512], F32) for i in range(8)]

    C.xr = [C.ctx_in[i * 128:(i + 1) * 128, :] if i < 2 else C.x_in[(i - 2) * 128:(i - 1) * 128, :] for i in range(NT)]
    C.out_t = k.split(C.out, NT - 2, lambda i: (slice(i * 128, (i + 1) * 128), slice(None)))
    C.final_done = False
    C.last_layer = DEVLAYERS - 1

    for l in range(DEVLAYERS):
        layer(C, l)

    if not C.final_done:
        with Scope(k):
            stg = [k.sb("op%d" % i, [128, D], F32) for i in range(3)]
            for i in range(2, NT):
                s = stg[i % 3]
                k.dma("sp", [(s, C.xr[i])])
                k.dma("pool", [(C.out_t[i - 2], s)])
    if "xres" in C.dbg_names:
        d = k.dram("dbg_xres", [T, D], F32, kind="ExternalOutput")
        with Scope(k):
            stg = [k.sb("dx%d" % i, [128, D], F32) for i in range(2)]
            for i in range(NT):
                k.dma("sp", [(stg[i % 2], C.xr[i])])
                k.dma("sp", [(d[i * 128:(i + 1) * 128, :], stg[i % 2])])
        C.dbg["xres"] = d
    k.wait_all("sp", [C.out] + [C.dbg[n] for n in C.dbg])
    return nc, C


def dbg_tap(C, name, tv, shape, dtype=F32):
    if name not in C.dbg_names:
        return
    k = C.k
    d = k.dram("dbg_" + name, list(shape), dtype, kind="ExternalOutput")
    k.dma("sp", [(d, tv)])
    C.dbg[name] = d


def barrier(k):
    targets = {}
    for name, E in k.engs.items():
        if E.count:
            targets[E.sem] = E.count
    for q, (lst, _) in k.dq.items():
        for key, total in lst:
            if total:
                targets[key] = total
    for name, E in k.engs.items():
        for sem, val in targets.items():
            if E.known.get(sem, 0) < val:
                E.eng.wait_ge(k.sems[sem], val)
                E.known[sem] = val
                k.nwait += 1


class Scope:
    def __init__(self, k):
        self.k = k

    def __enter__(self):
        self.prev = self.k.es
        self.les = contextlib.ExitStack()
        self.k.es = self.les
        return self

    def __exit__(self, *a):
        barrier(self.k)
        self.k.es = self.prev
        self.les.close()
        return False


def bank(C, i, shape=None, dtype=F32):
    return C.ps[i]


def layer(C, l):
    k = C.k
    Lw = C.L[l]
    C.ctx_out = (l < DEPTH - 1)
    with Scope(k):
        mod = k.sb("mod", [128, 48, 2], F32)
        gv = k.sb("gv", [128, 4, 8], F32)
        k.dma("sp", [(gv, Lw.gvec)])
        with Scope(k):
            sc = k.sb("sc", [128, 8, 2], F32)
            scb = k.sb("scb", [128, 8, 2], BF16)
            bada = k.sb("bada", [128, 48], F32)
            k.dma("sp", [(sc, C.cvec), (bada, Lw.bada)])
            k.act(scb, sc, AF.Silu)
            wst = [k.sb("wada%d" % i, [128, 8, 768], F32) for i in range(2)]
            wbf = [k.sb("wadab%d" % i, [128, 8, 768], BF16) for i in range(2)]
            wv = Lw.w_ada.v(Lw.w_ada.ap.rearrange("(k p) n -> p k n", p=128))
            pm = C.ps[0]
            for jb in range(8):
                w = wst[jb % 2]
                wb_ = wbf[jb % 2]
                k.dma("sp" if jb % 2 == 0 else "pool", [(w, wv[:, :, jb * 768:(jb + 1) * 768])])
                k.copy("act", wb_[:, 0:4, :], w[:, 0:4, :])
                k.copy("dve", wb_[:, 4:8, :], w[:, 4:8, :])
                for jj in range(6):
                    j = jb * 6 + jj
                    for kk in range(8):
                        k.mm(pm[:, j * 2:(j + 1) * 2], wb_[:, kk, jj * 128:(jj + 1) * 128], scb[:, kk, :],
                             start=(kk == 0), stop=(kk == 7))
            pmv = pm.v(pm.ap[:, 0:96].rearrange("p (j v) -> p j v", v=2))
            for v in range(2):
                k.tt("dve", mod[:, :, v], pmv[:, :, v], bada, ALU.add)
        dbg_tap(C, "mod%d" % l, mod, [128, 48, 2])
        A1 = k.sb("A1", [128, 8, 2], F32)
        A2 = k.sb("A2", [128, 8, 2], F32)
        G1c = k.sb("G1c", [128, 8, 2], F32)
        G2c = k.sb("G2c", [128, 8, 2], F32)
        for v in range(2):
            k.ts("dve", A1[:, :, v], mod[:, 8:16, v], 1.0, ALU.add)
            k.tt("dve", A1[:, :, v], A1[:, :, v], gv[:, 0, :], ALU.mult)
            k.ts("dve", A2[:, :, v], mod[:, 32:40, v], 1.0, ALU.add)
            k.tt("dve", A2[:, :, v], A2[:, :, v], gv[:, 2, :], ALU.mult)
            k.tt("dve", G1c[:, :, v], mod[:, 16:24, v], gv[:, 1, :], ALU.mult)
            k.tt("dve", G2c[:, :, v], mod[:, 40:48, v], gv[:, 3, :], ALU.mult)
        C.mod, C.A1, C.A2 = mod, A1, A2
        C.G1c, C.G2c = G1c, G2c
        with Scope(k):
            C.G1b = make_gate_tile(C, G1c, "G1b")
            dbg_tap(C, "G1b%d" % l, C.G1b, [128, 2, D])
            mixer(C, l)
        if "f" not in DEVFLAGS:
            with Scope(k):
                C.G2b = make_gate_tile(C, G2c, "G2b")
                if "D" in DEVFLAGS:
                    ffn_phase(C, l)
                else:
                    ffn_phase_gather(C, l)


def make_gate_tile(C, Gc, name):
    k = C.k
    Gb = k.sb(name, [128, 2, D], F32)
    with Scope(k):
        dg = [k.sb("dg%d" % i, [128, 128], F32) for i in range(2)]
        n = 0
        for v in range(2):
            for half in range(2):
                pb = C.ps[1 + (n % 2)]
                for kk in range(4):
                    kc = half * 4 + kk
                    d = dg[kc % 2]
                    k.ts("dve", d, C.ident_f, Gc[:, kc:kc + 1, v], ALU.mult)
                    k.mm(pb[:, kk * 128:(kk + 1) * 128], C.ones_f, d)
                k.copy("act", Gb[:, v, half * 512:(half + 1) * 512], pb)
                n += 1
    return Gb


def norm_to_hT(C, hT, A, Bc, tag, extra=None):
    k = C.k
    hT_t = k.split(hT, NT, lambda i: (slice(None), slice(None), slice(i * 128, (i + 1) * 128)))
    with Scope(k):
        xin = [k.sb("nx%d" % i, [128, D], F32) for i in range(3)]
        junk = k.sb("njunk", [128, D], BF16)
        xn = [k.sb("nxn%d" % i, [128, D], BF16) for i in range(3)]
        st = [k.sb("nst%d" % i, [128, 2], F32) for i in range(3)]

        def stA(i):
            x = xin[i % 3]
            s = st[i % 3]
            k.dma("sp" if i % 2 == 0 else "pool", [(x, C.xr[i])])
            k.act(junk, x, AF.Square, accum=s[:, 0:1])
            k.act(s[:, 1:2], s[:, 0:1], AF.Sqrt, bias=EPS, scale=1.0 / D)
            k.recip(s[:, 1:2], s[:, 1:2])
            k.ts("dve", xn[i % 3], x, s[:, 1:2], ALU.mult)
            if extra is not None:
                extra(i, x, s[:, 1:2])

        def stB(i):
            v = 1 if i < 2 else 0
            xb = xn[i % 3]
            for hh in range(2):
                pb = C.ps[2 + 2 * (i % 2) + hh]
                pt = pb.v(pb.ap.bitcast(BF16)[:, 0:512].rearrange("p (k t) -> p k t", t=128))
                for kk in range(4):
                    kc = hh * 4 + kk
                    k.tr(pt[:, kk, :], xb[:, kc * 128:(kc + 1) * 128], C.ident_b)
                for kk in range(4):
                    kc = hh * 4 + kk
                    dst = hT_t[i][:, kc, :]
                    if hh == 0:
                        k.act(dst, pt[:, kk, :], AF.Identity, bias=Bc[:, kc:kc + 1, v], scale=A[:, kc:kc + 1, v])
                    else:
                        k.ts("dve", dst, pt[:, kk, :], A[:, kc:kc + 1, v], ALU.mult, Bc[:, kc:kc + 1, v], ALU.add)
        stA(0)
        for i in range(NT):
            if i + 1 < NT:
                stA(i + 1)
            stB(i)


def mixer(C, l):
    k = C.k
    yT = None
    with Scope(k):
        hT = k.sb("hT", [128, 8, T], BF16)
        norm_to_hT(C, hT, C.A1, C.mod[:, 0:8, :], "mix")
        dbg_tap(C, "hT%d" % l, hT, [128, 8, T], BF16)
        if "m" not in DEVFLAGS:
            mla_branch(C, l, hT, C.otd)
        if "n" not in DEVFLAGS:
            na_branch(C, l, hT, C.otd)
        if "s" not in DEVFLAGS:
            s5_branch(C, l, hT, C.otd)
        if "d" not in DEVFLAGS:
            m2_branch(C, l, hT, C.otd)
        if "g" not in DEVFLAGS:
            merge_phase(C, l, hT, yT)
    if "g" not in DEVFLAGS:
        outproj_phase(C, l, yT)


NFM = 19 * 128


def _rot_perm(n):
    p = np.arange(n)
    g, i = p // 16, p % 16
    return g * 16 + np.where(i < 8, i + 8, i - 8)


def host_mixer_inputs(inp, l):
    o = {}
    w = inp["w_in"][l]
    z = np.zeros
    ch = [w[:, 0:128], w[:, 128:256], w[:, 256:384]]
    kr = w[:, 384:416]
    ch.append(np.concatenate([z((D, 64), np.float32), kr, z((D, 32), np.float32)], 1))
    ch.append(np.concatenate([z((D, 64), np.float32), kr[:, _rot_perm(32)], z((D, 32), np.float32)], 1))
    ch += [w[:, 416:544], w[:, 544:672]]
    for h in range(4):
        kh = w[:, 672 + h * 64: 672 + (h + 1) * 64]
        ch.append(np.concatenate([kh, z((D, 64), np.float32)] if h % 2 == 0 else [z((D, 64), np.float32), kh], 1))
    ch += [w[:, 1184:1312], w[:, 1312:1440]]
    ch += [w[:, 1696 + i * 128: 1696 + (i + 1) * 128] for i in range(6)]
    o["wfm"] = np.ascontiguousarray(np.concatenate(ch, 1))
    o["wtm"] = np.ascontiguousarray(np.concatenate([w[:, 928:1184], w[:, 1440:1696], w[:, 2464:2472]], 1))
    wuq = inp["mla_w_uq"][l].reshape(256, 4, 96)
    q = np.zeros((256, 4, 2, 128), np.float32)
    q[:, :, 0, 0:96] = wuq
    q[:, :, 1, 64:96] = wuq[:, :, 64:96][:, :, _rot_perm(32)]
    o["wuq"] = q.reshape(256, 1024)
    wukv = inp["mla_w_ukv"][l].reshape(128, 4, 128)
    kv = np.zeros((128, 768), np.float32)
    for h in range(4):
        kv[:, h * 128: h * 128 + 64] = wukv[:, h, 0:64]
        kv[:, 512 + h * 64: 512 + (h + 1) * 64] = wukv[:, h, 64:128]
    o["wukv"] = kv
    o["gmla"] = np.ascontiguousarray(np.concatenate([col_layout(inp["mla_g_cq"][l]), col_layout(inp["mla_g_ckv"][l])], 1))
    o["nab"] = na_bias_tables(inp["na_rpb"][l])
    return o


def na_variant(m):
    return m if m < 3 else (3 if m <= 13 else m - 10)


def na_kb(m):
    return min(int(np.clip(2 * m - 4, 0, 24)), 22)


def na_bias_tables(rpb):
    out = np.full((6, 4, 5, 128, 128), NEG, np.float32)
    for m in (0, 1, 2, 3, 14, 15):
        var = na_variant(m)
        kb = na_kb(m)
        q = np.arange(128)
        r = 2 * m + q // 64
        qc = q % 64
        rs = np.clip(r - 4, 0, 24)
        cs = np.clip(qc - 8, 0, 48)
        j = np.arange(640)
        krow = kb + j // 64
        kcol = j % 64
        inwin = ((krow[:, None] >= rs[None, :]) & (krow[:, None] < rs[None, :] + 8) &
                 (kcol[:, None] >= cs[None, :]) & (kcol[:, None] < cs[None, :] + 16))
        ro = np.clip(krow[:, None] - r[None, :] + 7, 0, 14)
        co = np.clip(kcol[:, None] - qc[None, :] + 15, 0, 30)
        for h in range(4):
            vals = rpb[h][ro, co]
            out[var, h] = np.where(inwin, vals, NEG).astype(np.float32).reshape(5, 128, 128)
    return np.ascontiguousarray(out.transpose(0, 1, 3, 2, 4))


def rope_tables():
    import ml_dtypes
    cosf = np.ones((128, T), np.float32)
    sinf = np.zeros((128, T), np.float32)
    pos = np.arange(SEQ)
    row, col = pos // GRID_W, pos % GRID_W
    inv = 1.0 / (10000.0 ** (np.arange(0, 16, 2, dtype=np.float32) / 16))
    for i in range(32):
        p = row if i < 16 else col
        ang = p.astype(np.float32) * inv[(i % 16) % 8]
        cosf[64 + i, CTX:] = np.cos(ang)
        sinf[64 + i, CTX:] = np.sin(ang)
    return cosf.astype(ml_dtypes.bfloat16), sinf.astype(ml_dtypes.bfloat16)


TBLK = [(0, 512), (512, 512), (1024, 512), (1536, 512), (2048, 256)]
TBLK_LAT = [(256, 512), (768, 512), (1280, 512), (1792, 512)]


class Stager:
    def __init__(self, C, cols, n=3):
        self.k = C.k

    def load(self, dst, src, kdim=8):
        self.k.dma("pool", [(dst, src.v(src.ap.rearrange("(k p) n -> p k n", p=128)))])


def proj_fm(C, hT, w, t0, n, outp):
    for kk in range(8):
        C.k.mm(outp[:, 0:n], w[:, kk, :], hT[:, kk, t0:t0 + n], start=(kk == 0), stop=(kk == 7))


def rstd_bcast(C, dst, ss_psum, n, dim, tmp):
    k = C.k
    k.act(tmp[:, 0:n], ss_psum[:, 0:n], AF.Sqrt, bias=EPS, scale=1.0 / dim)
    k.recip(dst[:, 0:n], tmp[:, 0:n])


def attn_norm_store(C, po, n, h, dst, rec):
    k = C.k
    if h % 2 == 0:
        k.recip(rec[0:64, 0:n], po[64:128, 0:n])
        k.tt("dve", dst[0:64], po[0:64, 0:n], rec[0:64, 0:n], ALU.mult)
    else:
        k.recip(rec[64:128, 0:n], po[0:64, 0:n])
        k.tt("dve", dst[64:128], po[64:128, 0:n], rec[64:128, 0:n], ALU.mult)


def mla_branch(C, l, hT, otd):
    k = C.k
    Lw = C.L[l]
    scale = 96.0 ** -0.5
    with Scope(k):
        QT = [k.sb("QT%d" % h, [128, T], BF16) for h in range(4)]
        KT = [k.sb("KT%d" % h, [128, T], BF16) for h in range(4)]
        Vaug = k.sb("Vaug", [128, NT, 4, 128], BF16)
        cosf = k.sb("cosf", [128, T], BF16)
        sinf = k.sb("sinf", [128, T], BF16)
        if "A" not in DEVFLAGS:
            k.dma("sp", [(cosf, C.cosf_in), (sinf, C.sinf_in)])
        if "B" not in DEVFLAGS:
            k.memset("pool", Vaug, 1.0)
        with Scope(k):
            wm = k.sb("wm", [128, 8, 640], BF16)
            wuq = k.sb("wuq", [128, 2, 1024], BF16)
            wukv = k.sb("wukv", [128, 1, 768], BF16)
            gm = k.sb("gmla", [128, 3], F32)
            k.dma("sp", [(gm, Lw.gmla)])
            stg = Stager(C, 2048, n=3)
            for j in range(5):
                stg.load(wm[:, :, j * 128:(j + 1) * 128], Lw.wfm[:, j * 128:(j + 1) * 128])
            stg.load(wuq, Lw.wuq, kdim=2)
            stg.load(wukv, Lw.wukv, kdim=1)
            for c in range(2):
                k.ts("dve", wuq[:, c, :], wuq[:, c, :], gm[:, c:c + 1], ALU.mult)
            k.ts("dve", wukv[:, 0, :], wukv[:, 0, :], gm[:, 2:3], ALU.mult)
            v1 = wm.v(wm.ap[:, :, 4 * 128 + 64:4 * 128 + 96].rearrange("p k (g s) -> p k g s", s=16)[:, :, :, 0:8])
            if "C" not in DEVFLAGS:
                k.ts("dve", v1, v1, -1.0, ALU.mult)
            for c in range(2 if "D" not in DEVFLAGS else 0):
                v2 = wuq.v(wuq.ap[:, c, :].rearrange("p (h m x) -> p h m x", h=4, m=2)[:, :, 1, 64:96]
                           .rearrange("p h (g s) -> p h g s", s=16)[:, :, :, 0:8])
                k.ts("dve", v2, v2, -1.0, ALU.mult)
            if DEVSTOP == "mla_w":
                dbg_tap(C, "wm%d" % l, wm, [128, 8, 640], BF16)
                dbg_tap(C, "wuq%d" % l, wuq, [128, 2, 1024], BF16)
                return
            cq = [k.sb("cqb%d" % i, [128, 2, 512], BF16) for i in range(2)]
            sq = [k.sb("sqb%d" % i, [128, 2, 512], BF16) for i in range(2)]
            ckv = [k.sb("ckvb%d" % i, [128, 512], BF16) for i in range(2)]
            skv = [k.sb("skvb%d" % i, [128, 512], BF16) for i in range(2)]
            rq = [k.sb("rq%d" % i, [128, 512], F32) for i in range(2)]
            rkv = [k.sb("rkv%d" % i, [128, 512], F32) for i in range(2)]
            tmp = [k.sb("mt%d" % i, [128, 512], F32) for i in range(4)]
            krr = [k.sb("krr%d" % i, [128, 512], BF16) for i in range(2)]
            rc = [k.sb("rc%d" % i, [128, 2], F32) for i in range(4)]
            P = C.ps
            for bi, (t0, n) in enumerate(TBLK):
                cqb, sqb, ckvb, skvb, rqb, rkvb = cq[bi % 2], sq[bi % 2], ckv[bi % 2], skv[bi % 2], rq[bi % 2], rkv[bi % 2]
                for c in range(2):
                    if 'z' not in DEVFLAGS:
                        proj_fm(C, hT, wm[:, :, c * 128:(c + 1) * 128], t0, n, P[c])
                    if 'x' not in DEVFLAGS:
                        k.copy("dve", cqb[:, c, 0:n], P[c][:, 0:n])
                    if 'y' not in DEVFLAGS:
                        k.act(sqb[:, c, 0:n], P[c][:, 0:n], AF.Square)
                if '1' in DEVFLAGS:
                    continue
                for c in range(2):
                    k.mm(P[2][:, 0:n], C.ones_b, sqb[:, c, 0:n], start=(c == 0), stop=(c == 1))
                rstd_bcast(C, rqb, P[2], n, 256, tmp[0])
                if '2' in DEVFLAGS:
                    continue
                proj_fm(C, hT, wm[:, :, 256:384], t0, n, P[3])
                k.copy("dve", ckvb[:, 0:n], P[3][:, 0:n])
                k.act(skvb[:, 0:n], P[3][:, 0:n], AF.Square)
                k.mm(P[2][:, 0:n], C.ones_b, skvb[:, 0:n])
                rstd_bcast(C, rkvb, P[2], n, 128, tmp[0])
                if '3' in DEVFLAGS:
                    continue
                proj_fm(C, hT, wm[:, :, 384:512], t0, n, P[0])
                proj_fm(C, hT, wm[:, :, 512:640], t0, n, P[1])
                k.tt("dve", tmp[1][:, 0:n], P[0][:, 0:n], cosf[:, t0:t0 + n], ALU.mult)
                k.tt("dve", tmp[2][:, 0:n], P[1][:, 0:n], sinf[:, t0:t0 + n], ALU.mult)
                kb = krr[bi % 2]
                k.tt("pool", kb[:, 0:n], tmp[1][:, 0:n], tmp[2][:, 0:n], ALU.add)
                for h in range(4 if 'H' not in DEVFLAGS else 0):
                    pm, pr = P[4 + (h % 2) * 2], P[5 + (h % 2) * 2]
                    for c in range(2):
                        k.mm(pm[:, 0:n], wuq[:, c, h * 256:h * 256 + 128], cqb[:, c, 0:n], start=(c == 0), stop=(c == 1))
                    for c in range(2):
                        k.mm(pr[:, 0:n], wuq[:, c, h * 256 + 128:h * 256 + 256], cqb[:, c, 0:n], start=(c == 0), stop=(c == 1))
                    ta, tb = tmp[(h % 2) * 2], tmp[(h % 2) * 2 + 1]
                    k.tt("dve", ta[:, 0:n], pm[:, 0:n], cosf[:, t0:t0 + n], ALU.mult)
                    k.tt("dve", tb[:, 0:n], pr[:, 0:n], sinf[:, t0:t0 + n], ALU.mult)
                    k.tt("pool", ta[:, 0:n], ta[:, 0:n], tb[:, 0:n], ALU.add)
                    k.tt("pool", QT[h][:, t0:t0 + n], ta[:, 0:n], rqb[:, 0:n], ALU.mult)
                for h in range(4 if 'G' not in DEVFLAGS else 0):
                    pk = P[h % 2]
                    k.mm(pk[:, 0:n], wukv[:, 0, h * 128:(h + 1) * 128], ckvb[:, 0:n])
                    k.tt("dve", KT[h][0:64, t0:t0 + n], pk[0:64, 0:n], rkvb[0:64, 0:n], ALU.mult)
                    k.copy("pool", KT[h][64:128, t0:t0 + n], kb[64:128, 0:n])
                for j in range(n // 128 if 'F' not in DEVFLAGS else 0):
                    ti = t0 // 128 + j
                    pv, pss = P[2 + (j % 2)], P[4 + (j % 2)]
                    k.mm(pv[:, 0:256], ckvb[:, j * 128:(j + 1) * 128], wukv[:, 0, 512:768])
                    k.mm(pss[:, 0:1], skvb[:, j * 128:(j + 1) * 128], C.ones_b[:, 0:1])
                    r = rc[ti % 4]
                    k.act(r[:, 0:1], pss[:, 0:1], AF.Sqrt, bias=EPS, scale=1.0 / 128)
                    k.recip(r[:, 1:2], r[:, 0:1])
                    pvv = pv.v(pv.ap[:, 0:256].rearrange("p (h d) -> p h d", d=64))
                    k.ts("dve", Vaug[:, ti, 0:4:2, 0:64], pvv[:, 0:4:2, :], r[:, 1:2], ALU.mult)
                    k.ts("dve", Vaug[:, ti, 1:4:2, 64:128], pvv[:, 1:4:2, :], r[:, 1:2], ALU.mult)
        dbg_tap(C, "QT0_%d" % l, QT[0], [128, T], BF16)
        dbg_tap(C, "KT0_%d" % l, KT[0], [128, T], BF16)
        dbg_tap(C, "Vaug%d" % l, Vaug, [128, NT, 4, 128], BF16)
        if DEVSTOP == "mla_proj":
            return
        with Scope(k):
            OT = k.sb("OTmla", [128, 2, T], BF16)
            PT = [k.sb("PT%d" % i, [128, 512], BF16) for i in range(3)]
            rec = [k.sb("rec%d" % i, [128, 512], F32) for i in range(2)]
            P = C.ps
            qblocks = [(256 + 512 * i, 512, list(range(NT))) for i in range(4)] + [(0, 256, [0, 1])]
            steps = []
            nb = 0
            for h in range(4):
                for (q0, n, kts) in qblocks:
                    for ki, kt in enumerate(kts):
                        steps.append((h, q0, n, kt, ki == 0, ki == len(kts) - 1, nb))
                    nb += 1
            NS = 4
            PT = PT + [k.sb("PT3", [128, 512], BF16)]

            def qk(i):
                h, q0, n, kt, first, last, nbi = steps[i]
                k.mm(P[i % NS][:, 0:n], KT[h][:, kt * 128:(kt + 1) * 128], QT[h][:, q0:q0 + n])
                k.act(PT[i % NS][:, 0:n], P[i % NS][:, 0:n], AF.Exp, scale=scale)

            def pv(i):
                h, q0, n, kt, first, last, nbi = steps[i]
                po = P[4 + (nbi % 2)]
                k.mm(po[:, 0:n], Vaug[:, kt, h, :], PT[i % NS][:, 0:n], start=first, stop=last)
                if last:
                    attn_norm_store(C, po, n, h, OT[:, h // 2, q0:q0 + n], rec[nbi % 2])
            LOOK = 2
            for i in range(min(LOOK, len(steps))):
                qk(i)
            for i in range(len(steps)):
                if i + LOOK < len(steps):
                    qk(i + LOOK)
                pv(i)
            dbg_tap(C, "OTmla%d" % l, OT, [128, 2, T], BF16)
            k.dma("sp", [(otd[0:2].v(otd.ap[0:2].rearrange("c p t -> p c t")), OT)])


def na_branch(C, l, hT, otd):
    k = C.k
    Lw = C.L[l]
    scale = 0.125
    P = C.ps
    with Scope(k):
        QnT = k.sb("QnT", [128, 2, T], BF16)
        KmT = [k.sb("KmT%d" % h, [128, T], BF16) for h in range(4)]
        Vaug = k.sb("VaugN", [128, NT, 4, 128], BF16)
        k.memset("pool", Vaug, 1.0)
        with Scope(k):
            wq = k.sb("wq", [128, 8, 256], BF16)
            wk = k.sb("wk", [128, 8, 512], BF16)
            wv = k.sb("wv", [128, 8, 256], BF16)
            stg = Stager(C, 2048, n=3)
            for j in range(2):
                stg.load(wq[:, :, j * 128:(j + 1) * 128], Lw.wfm[:, (5 + j) * 128:(6 + j) * 128])
            for j in range(4):
                stg.load(wk[:, :, j * 128:(j + 1) * 128], Lw.wfm[:, (7 + j) * 128:(8 + j) * 128])
            stg.load(wv, Lw.wtm[:, 0:256])
            for bi, (t0, n) in enumerate(TBLK):
                for c in range(2):
                    proj_fm(C, hT, wq[:, :, c * 128:(c + 1) * 128], t0, n, P[c])
                    k.copy("dve" if c == 0 else "act", QnT[:, c, t0:t0 + n], P[c][:, 0:n])
                for h in range(4):
                    proj_fm(C, hT, wk[:, :, h * 128:(h + 1) * 128], t0, n, P[2 + h])
                    k.copy("dve" if h % 2 == 0 else "act", KmT[h][:, t0:t0 + n], P[2 + h][:, 0:n])
                for j in range(n // 128):
                    ti = t0 // 128 + j
                    pv = P[6 + (j % 2)]
                    for kk in range(8):
                        k.mm(pv[:, 0:256], hT[:, kk, ti * 128:(ti + 1) * 128], wv[:, kk, :], start=(kk == 0), stop=(kk == 7))
                    pvv = pv.v(pv.ap[:, 0:256].rearrange("p (h d) -> p h d", d=64))
                    k.copy("dve", Vaug[:, ti, 0:4:2, 0:64], pvv[:, 0:4:2, :])
                    k.copy("dve", Vaug[:, ti, 1:4:2, 64:128], pvv[:, 1:4:2, :])
        with Scope(k):
            OT = k.sb("OTna", [128, 2, T], BF16)
            nb = [k.sb("nab%d" % i, [128, 5, 128], F32) for i in range(2)]
            nbb = [k.sb("nabb%d" % i, [128, 5, 128], BF16) for i in range(2)]
            PT = [k.sb("nPT%d" % i, [128, 896], BF16) for i in range(2)]
            rec = [k.sb("nrec%d" % i, [128, 256], F32) for i in range(2)]
            items = []
            for h in range(4):
                for m in range(16):
                    items.append((h, m))
                items.append((h, -1))
            state = {"var": None, "nload": 0, "bt": None}

            def stageA(i):
                h, m = items[i]
                s = i % 2
                pa, pb = P[3 * s], P[3 * s + 1]
                pt = PT[s]
                if m < 0:
                    q = QnT[:, h // 2, 0:CTX]
                    for j in range(2):
                        k.mm(pa[:, j * 256:(j + 1) * 256], KmT[h][:, j * 128:(j + 1) * 128], q)
                    k.act(pt[:, 0:512], pa, AF.Exp, scale=scale)
                    return
                var = na_variant(m)
                if (h, var) != state["var"]:
                    bt32 = nb[state["nload"] % 2]
                    state["bt"] = nbb[state["nload"] % 2]
                    state["nload"] += 1
                    k.dma("sp", [(bt32, Lw.nab[var, h])])
                    k.act(state["bt"], bt32, AF.Copy, scale=1.0 / scale)
                    state["var"] = (h, var)
                bt = state["bt"]
                q0 = CTX + m * 128
                kt0 = 2 + na_kb(m) // 2
                q = QnT[:, h // 2, q0:q0 + 128]
                for j in range(4):
                    k.mm(pa[:, j * 128:(j + 1) * 128], KmT[h][:, (kt0 + j) * 128:(kt0 + j + 1) * 128], q, start=True, stop=False)
                    k.mm(pa[:, j * 128:(j + 1) * 128], C.ident_b, bt[:, j, :], start=False, stop=True)
                k.mm(pb[:, 0:128], KmT[h][:, (kt0 + 4) * 128:(kt0 + 5) * 128], q, start=True, stop=False)
                k.mm(pb[:, 0:128], C.ident_b, bt[:, 4, :], start=False, stop=True)
                for j in range(2):
                    k.mm(pb[:, 128 + j * 128:256 + j * 128], KmT[h][:, j * 128:(j + 1) * 128], q)
                k.act(pt[:, 0:512], pa, AF.Exp, scale=scale)
                k.act(pt[:, 512:896], pb[:, 0:384], AF.Exp, scale=scale)

            def stageB(i):
                h, m = items[i]
                s = i % 2
                po = P[3 * s + 2]
                pt = PT[s]
                if m < 0:
                    for j in range(2):
                        k.mm(po[:, 0:256], Vaug[:, j, h, :], pt[:, j * 256:(j + 1) * 256], start=(j == 0), stop=(j == 1))
                    attn_norm_store(C, po, 256, h, OT[:, h // 2, 0:CTX], rec[s])
                    return
                q0 = CTX + m * 128
                kt0 = 2 + na_kb(m) // 2
                kts = [kt0 + j for j in range(5)] + [0, 1]
                for j, kt in enumerate(kts):
                    k.mm(po[:, 0:128], Vaug[:, kt, h, :], pt[:, j * 128:(j + 1) * 128], start=(j == 0), stop=(j == 6))
                attn_norm_store(C, po, 128, h, OT[:, h // 2, q0:q0 + 128], rec[s])
            stageA(0)
            for i in range(len(items)):
                if i + 1 < len(items):
                    stageA(i + 1)
                stageB(i)
            dbg_tap(C, "OTna%d" % l, OT, [128, 2, T], BF16)
            k.dma("sp", [(otd[2:4].v(otd.ap[2:4].rearrange("c p t -> p c t")), OT)])


def host_s5_inputs(inp, l):
    o = {}
    ls = np.repeat(inp["s5_log_step"][l][:, :, None], 64, axis=2)
    o["s5par"] = np.ascontiguousarray(np.stack([inp["s5_a_re"][l].reshape(-1), inp["s5_a_im"][l].reshape(-1),
                                                ls.reshape(-1)], 0).astype(np.float32))
    BT = np.zeros((128, 2, 8, 2, 128), np.float32)
    CT = np.zeros((128, 2, 8, 2, 128), np.float32)
    for d in range(2):
        for g in range(16):
            s, gj = g // 2, g % 2
            r0 = (s % 4) * 32 + gj * 16
            for x, (bn, cn) in enumerate((("s5_b_re", "s5_c_re"), ("s5_b_im", "s5_c_im"))):
                BT[r0:r0 + 16, d, s, x, gj * 64:(gj + 1) * 64] = inp[bn][l][d, g].T
                CT[gj * 64:(gj + 1) * 64, d, s, x, r0:r0 + 16] = inp[cn][l][d, g].T
    o["s5BT"] = BT
    o["s5CT"] = CT
    o["s5vec"] = np.ascontiguousarray(np.stack([col_layout(inp["s5_d"][l]), col_layout(inp["s5_b_glu"][l])], -1))
    o["s5wglu"] = np.ascontiguousarray(inp["s5_w_glu"][l])
    o["s5wu"] = np.ascontiguousarray(inp["w_in"][l][:, 1184:1440])
    return o


S5_ORDER = [list(range(NT)), [1, 0] + list(range(NT - 1, 1, -1))]


def bc(tv, shape, axis):
    return tv.v(tv.ap.unsqueeze(axis).to_broadcast(list(shape)))


def s5_branch(C, l, hT, otd):
    k = C.k
    Lw = C.L[l]
    P = C.ps
    HALF_PI = float(np.pi / 2)
    with Scope(k):
        BbT = k.sb("s5BbT", [128, 2, 8, 2, 128], BF16)
        CTb = k.sb("s5CTb", [128, 2, 8, 2, 128], BF16)
        cos16 = k.sb("s5cos16", [128, 16, 128], BF16)
        sin16 = k.sb("s5sin16", [128, 16, 128], BF16)
        rhob = k.sb("s5rhob", [128, 16, 128], F32)
        eL = k.sb("s5eL", [128, 16, 2], F32)
        vec = k.sb("s5vec", [128, 2, 2], F32)
        wglu = k.sb("s5wglu", [128, 2, 256], BF16)
        wu = k.sb("s5wu", [128, 8, 256], BF16)
        Jm = k.sb("s5J", [128, 128], BF16)
        k.dma("sp", [(vec, Lw.s5vec)])
        with Scope(k):
            stg = Stager(C, 2048, n=2)
            stg.load(wu, Lw.s5wu)
            stg.load(wglu, Lw.s5wglu, kdim=2)
            jf = k.sb("jf", [128, 128], F32)
            k.dma("sp", [(jf, C.jmat_in)])
            k.copy("dve", Jm, jf)
        with Scope(k):
            cosT = k.sb("s5cosT", [128, 16, 128], F32)
            sinT = k.sb("s5sinT", [128, 16, 128], F32)
            par = k.sb("s5par", [128, 3, 2048], F32)
            k.dma("sp", [(par, Lw.s5par.v(Lw.s5par.ap.partition_broadcast(128)))])
            are, aim, ls = par[:, 0, :], par[:, 1, :], par[:, 2, :]
            W = [k.sb("s5w%d" % i, [128, 2048], F32) for i in range(7)]
            step, rho, cth, sth, t1, t2, t3 = W
            k.act(step, ls, AF.Exp)
            k.tt("dve", t1, step, are, ALU.mult)
            k.act(rho, t1, AF.Exp)
            k.tt("dve", t1, step, aim, ALU.mult)
            k.act(sth, t1, AF.Sin, scale=1.0 / 16)
            k.act(cth, t1, AF.Sin, scale=1.0 / 16, bias=HALF_PI)
            for _ in range(4):
                k.act(t1, cth, AF.Square)
                k.act(t2, sth, AF.Square)
                k.tt("dve", t3, cth, sth, ALU.mult)
                k.tt("dve", cth, t1, t2, ALU.subtract)
                k.act(sth, t3, AF.Copy, scale=2.0)
            diag = k.sb("s5diag", [128, 16, 3], F32)
            big = k.sb("s5big", [128, 16, 128], F32)
            for i, src in enumerate((rho, cth, sth)):
                sv = src.v(src.ap.rearrange("p (a m) -> p a m", m=128))
                k.tt("dve", big, sv, bc(C.ident_f, [128, 16, 128], 1), ALU.mult)
                k.ins("dve", lambda e, o=diag[:, :, i], b=big: e.tensor_reduce(out=o.ap, in_=b.ap, axis=AX.X, op=ALU.add),
                      [big], [diag])
            nr, ni = t1, t2
            k.tt("dve", nr, rho, cth, ALU.mult)
            k.ts("dve", nr, nr, -1.0, ALU.add)
            k.tt("pool", ni, rho, sth, ALU.mult)
            den = t3
            k.act(den, are, AF.Square)
            k.act(step, aim, AF.Square)
            k.tt("dve", den, den, step, ALU.add)
            k.recip(den, den)
            cr, ci = cth, sth
            k.tt("dve", cr, nr, are, ALU.mult)
            k.tt("pool", step, ni, aim, ALU.mult)
            k.tt("dve", cr, cr, step, ALU.add)
            k.tt("dve", cr, cr, den, ALU.mult)
            k.tt("dve", ci, ni, are, ALU.mult)
            k.tt("pool", step, nr, aim, ALU.mult)
            k.tt("dve", ci, ci, step, ALU.subtract)
            k.tt("dve", ci, ci, den, ALU.mult)
            btf = k.sb("s5btf", [128, 2, 8, 2, 128], F32)
            k.dma("sp", [(btf, Lw.s5BT)])
            crv = cr.v(cr.ap.rearrange("p (d s m) -> p d s m", d=2, s=8))
            civ = ci.v(ci.ap.rearrange("p (d s m) -> p d s m", d=2, s=8))
            tb1 = rho.v(rho.ap.rearrange("p (d s m) -> p d s m", d=2, s=8))
            tb2 = step.v(step.ap.rearrange("p (d s m) -> p d s m", d=2, s=8))
            k.tt("dve", tb1, crv, btf[:, :, :, 0, :], ALU.mult)
            k.tt("pool", tb2, civ, btf[:, :, :, 1, :], ALU.mult)
            k.tt("dve", BbT[:, :, :, 0, :], tb1, tb2, ALU.subtract)
            k.tt("dve", tb1, crv, btf[:, :, :, 1, :], ALU.mult)
            k.tt("pool", tb2, civ, btf[:, :, :, 0, :], ALU.mult)
            k.tt("dve", BbT[:, :, :, 1, :], tb1, tb2, ALU.add)
            k.dma("sp", [(btf, Lw.s5CT)])
            k.copy("dve", CTb[:, :, :, 0, :], btf[:, :, :, 0, :])
            k.ts("dve", CTb[:, :, :, 1, :], btf[:, :, :, 1, :], -1.0, ALU.mult)
            ck = k.sb("s5ck", [128, 16], F32)
            sk = k.sb("s5sk", [128, 16], F32)
            ta = k.sb("s5ta", [128, 16], F32)
            tb_ = k.sb("s5tb", [128, 16], F32)
            k.copy("dve", ck, diag[:, :, 1])
            k.copy("dve", sk, diag[:, :, 2])
            k.memset("dve", cosT[:, :, 0:1], 1.0)
            k.memset("dve", sinT[:, :, 0:1], 0.0)
            big2 = big[:, :, 0:64]
            big3 = big[:, :, 64:128]
            for kk in range(8):
                w = 1 << kk
                if kk < 7:
                    ckb = bc(ck, [128, 16, w], 2)
                    skb = bc(sk, [128, 16, w], 2)
                    k.tt("dve", big2[:, :, 0:w], cosT[:, :, 0:w], ckb, ALU.mult)
                    k.tt("dve", big3[:, :, 0:w], sinT[:, :, 0:w], skb, ALU.mult)
                    k.tt("dve", cosT[:, :, w:2 * w], big2[:, :, 0:w], big3[:, :, 0:w], ALU.subtract)
                    k.tt("dve", big2[:, :, 0:w], cosT[:, :, 0:w], skb, ALU.mult)
                    k.tt("dve", big3[:, :, 0:w], sinT[:, :, 0:w], ckb, ALU.mult)
                    k.tt("dve", sinT[:, :, w:2 * w], big2[:, :, 0:w], big3[:, :, 0:w], ALU.add)
                    k.tt("dve", ta, ck, ck, ALU.mult)
                    k.tt("dve", tb_, sk, sk, ALU.mult)
                    k.tt("dve", sk, ck, sk, ALU.mult)
                    k.ts("dve", sk, sk, 2.0, ALU.mult)
                    k.tt("dve", ck, ta, tb_, ALU.subtract)
                else:
                    k.copy("dve", eL[:, :, 0], ck)
                    k.copy("dve", eL[:, :, 1], sk)
            k.copy("dve", rhob, bc(diag[:, :, 0], [128, 16, 128], 2))
            k.copy("act", cos16, cosT)
            k.copy("act", sin16, sinT)
        dbg_tap(C, "s5rhob%d" % l, rhob, [128, 16, 128])
        dbg_tap(C, "s5BbT%d" % l, BbT, [128, 2, 8, 2, 128], BF16)
        if DEVSTOP == "s5_par":
            return
        uproc = [k.sb("s5up%d" % d, [128, 2, T], BF16) for d in range(2)]
        unat = k.sb("s5un", [128, 2, T], F32)
        pos = [{c: i for i, c in enumerate(S5_ORDER[d])} for d in range(2)]
        with Scope(k):
            ut = [k.sb("s5ut%d" % i, [128, 256], BF16) for i in range(2)]
            for c in range(NT):
                pu = P[c % 2]
                for kk in range(8):
                    k.mm(pu[:, 0:256], hT[:, kk, c * 128:(c + 1) * 128], wu[:, kk, :], start=(kk == 0), stop=(kk == 7))
                u = ut[c % 2]
                k.copy("act", u, pu[:, 0:256])
                pf = P[2 + (c % 2) * 2]
                pr = P[3 + (c % 2) * 2]
                for q in range(2):
                    k.mm(pf[:, q * 128:(q + 1) * 128], u[:, q * 128:(q + 1) * 128], C.ident_b)
                    k.mm(pr[:, q * 128:(q + 1) * 128], u[:, q * 128:(q + 1) * 128], Jm)
                pfv = pf.v(pf.ap[:, 0:256].rearrange("p (q t) -> p q t", q=2))
                prv = pr.v(pr.ap[:, 0:256].rearrange("p (q t) -> p q t", q=2))
                k.copy("dve", uproc[0][:, :, c * 128:(c + 1) * 128], pfv)
                k.copy("act", unat[:, :, c * 128:(c + 1) * 128], pfv)
                i1 = pos[1][c]
                k.copy("dve", uproc[1][:, :, i1 * 128:(i1 + 1) * 128], prv)
        dbg_tap(C, "s5up1_%d" % l, uproc[1], [128, 2, T], BF16)
        yf = k.sb("s5yf", [128, 2, T], F32)
        with Scope(k):
            BQ = k.sb("s5BQ", [128, 8, 2, 512], F32)
            bq = [[None, None] for _ in range(8)]
            subs = k.split(BQ, 16, lambda i: (slice(None), i // 2, i % 2, slice(None)))
            for i in range(16):
                bq[i // 2][i % 2] = subs[i]
            G16 = k.sb("s5G16", [128, 8, 2, 512], BF16)
            g16 = [[None, None] for _ in range(8)]
            subs16 = k.split(G16, 16, lambda i: (slice(None), i // 2, i % 2, slice(None)))
            for i in range(16):
                g16[i // 2][i % 2] = subs16[i]
            tm = [k.sb("s5tm%d" % i, [128, 512], BF16) for i in range(8)]
            p16 = [k.sb("s5p16%d" % i, [128, 512], BF16) for i in range(4)]

            hre = [k.sb("s5hre%d" % i, [128, 512], BF16) for i in range(2)]
            him = [k.sb("s5him%d" % i, [128, 512], BF16) for i in range(2)]
            ini = [k.sb("s5ini%d" % i, [128, 8, 2], F32) for i in range(2)]
            tp_ = [k.sb("s5tp%d" % i, [128, 8], F32) for i in range(4)]
            ytr = [k.sb("s5ytr%d" % i, [128, 256], BF16) for i in range(2)]
            it = 0
            nchunk = 0
            for d in range(2):
                k.memset("pool", ini[nchunk % 2], 0.0)
                cL = eL[:, d * 8:(d + 1) * 8, 0]
                sL = eL[:, d * 8:(d + 1) * 8, 1]
                for bi, (t0, n) in enumerate(TBLK):
                    nch = n // 128

                    def v3(tv):
                        return tv.v(tv.ap[:, 0:n].rearrange("p (c j) -> p c j", j=128))
                    for s in range(8):
                        q = s // 4
                        sd = d * 8 + s
                        pre, pim = P[2 * (s % 2)], P[2 * (s % 2) + 1]
                        k.mm(pre[:, 0:n], BbT[:, d, s, 0, :], uproc[d][:, q, t0:t0 + n])
                        k.mm(pim[:, 0:n], BbT[:, d, s, 1, :], uproc[d][:, q, t0:t0 + n])
                        cb = bc(cos16[:, sd, :], [128, nch, 128], 1)
                        sb_ = bc(sin16[:, sd, :], [128, nch, 128], 1)
                        t = tm[(s % 2) * 4:(s % 2) * 4 + 4]
                        r16, i16 = p16[(s % 2) * 2], p16[(s % 2) * 2 + 1]
                        k.copy("act", r16[:, 0:n], pre[:, 0:n])
                        k.copy("act", i16[:, 0:n], pim[:, 0:n])
                        k.tt("dve", v3(t[0]), v3(r16), cb, ALU.mult)
                        k.tt("dve", v3(t[1]), v3(r16), sb_, ALU.mult)
                        k.tt("dve", v3(t[2]), v3(i16), sb_, ALU.mult)
                        k.tt("dve", v3(t[3]), v3(i16), cb, ALU.mult)
                        k.tt("pool", bq[s][0][:, 0:n], t[0][:, 0:n], t[2][:, 0:n], ALU.add)
                        k.tt("pool", bq[s][1][:, 0:n], t[3][:, 0:n], t[1][:, 0:n], ALU.subtract)
                    for j in range(nch):
                        cur, nxt = ini[nchunk % 2], ini[(nchunk + 1) % 2]
                        nchunk += 1
                        sl = slice(j * 128, (j + 1) * 128)
                        for s in range(8):
                            sd = d * 8 + s
                            for x in range(2):
                                k.scan(g16[s][x][:, sl], rhob[:, sd, :], bq[s][x][:, sl], cur[:, s, x:x + 1], ALU.mult, ALU.add)
                        last = j * 128 + 127
                        cr_ = G16[:, :, 0, last]
                        ci_ = G16[:, :, 1, last]
                        k.tt("pool", tp_[0], cr_, cL, ALU.mult)
                        k.tt("pool", tp_[1], ci_, sL, ALU.mult)
                        k.tt("pool", nxt[:, :, 0], tp_[0], tp_[1], ALU.subtract)
                        k.tt("pool", tp_[2], cr_, sL, ALU.mult)
                        k.tt("pool", tp_[3], ci_, cL, ALU.mult)
                        k.tt("pool", nxt[:, :, 1], tp_[2], tp_[3], ALU.add)
                    if d == 0:
                        py = [P[4], P[5]]
                    else:
                        py = [P[4 + j] for j in range(nch)]
                    for s in range(8):
                        q = s // 4
                        sd = d * 8 + s
                        b = it % 2
                        it += 1
                        cb = bc(cos16[:, sd, :], [128, nch, 128], 1)
                        sb_ = bc(sin16[:, sd, :], [128, nch, 128], 1)
                        t = tm[(s % 2) * 4:(s % 2) * 4 + 4]
                        k.tt("dve", v3(t[0]), v3(g16[s][0]), cb, ALU.mult)
                        k.tt("pool", v3(t[1]), v3(g16[s][1]), sb_, ALU.mult)
                        k.tt("dve", v3(t[2]), v3(g16[s][0]), sb_, ALU.mult)
                        k.tt("pool", v3(t[3]), v3(g16[s][1]), cb, ALU.mult)
                        k.tt("dve", hre[b][:, 0:n], t[0][:, 0:n], t[1][:, 0:n], ALU.subtract)
                        k.tt("dve", him[b][:, 0:n], t[2][:, 0:n], t[3][:, 0:n], ALU.add)
                        first, lastq = (s % 4 == 0), (s % 4 == 3)
                        if d == 0:
                            k.mm(py[q][:, 0:n], CTb[:, d, s, 0, :], hre[b][:, 0:n], start=first, stop=False)
                            k.mm(py[q][:, 0:n], CTb[:, d, s, 1, :], him[b][:, 0:n], start=False, stop=lastq)
                        else:
                            for j in range(nch):
                                sl = slice(j * 128, (j + 1) * 128)
                                k.mm(py[j][:, q * 128:(q + 1) * 128], hre[b][:, sl], CTb[:, d, s, 0, :], start=first, stop=False)
                                k.mm(py[j][:, q * 128:(q + 1) * 128], him[b][:, sl], CTb[:, d, s, 1, :], start=False, stop=lastq)
                        if lastq:
                            if d == 0:
                                k.copy("act", yf[:, q, t0:t0 + n], py[q][:, 0:n])
                            elif q == 1:
                                for j in range(nch):
                                    c = S5_ORDER[1][t0 // 128 + j]
                                    yt = ytr[j % 2]
                                    k.copy("act", yt, py[j][:, 0:256])
                                    pz = P[2 * (j % 2)]
                                    for qq in range(2):
                                        k.mm(pz[:, qq * 128:(qq + 1) * 128], yt[:, qq * 128:(qq + 1) * 128], Jm)
                                    pzv = pz.v(pz.ap[:, 0:256].rearrange("p (q t) -> p q t", q=2))
                                    ysl = yf[:, :, c * 128:(c + 1) * 128]
                                    k.tt("dve", ysl, ysl, pzv, ALU.add)
        for q in range(2):
            k.stt(unat[:, q, :], unat[:, q, :], vec[:, q, 0:1], yf[:, q, :], ALU.mult, ALU.add)
        dbg_tap(C, "s5y%d" % l, unat, [128, 2, T])
        with Scope(k):
            OT = k.sb("OTs5", [128, 2, T], BF16)
            zb = [k.sb("s5z%d" % i, [128, 2, 512], BF16) for i in range(2)]
            g1 = [k.sb("s5g%d" % i, [128, 512], F32) for i in range(4)]
            for bi, (t0, n) in enumerate(TBLK):
                z = zb[bi % 2]
                for q in range(2):
                    y = unat[:, q, t0:t0 + n]
                    a, b_ = g1[q * 2], g1[q * 2 + 1]
                    k.tt("pool", a[:, 0:n], y, y, ALU.mult)
                    k.ts("dve", a[:, 0:n], a[:, 0:n], 0.044715, ALU.mult, 1.0, ALU.add)
                    k.tt("dve", a[:, 0:n], a[:, 0:n], y, ALU.mult)
                    k.act(b_[:, 0:n], a[:, 0:n], AF.Sigmoid, scale=1.5957691216057308)
                    k.tt("dve", z[:, q, 0:n], y, b_[:, 0:n], ALU.mult)
                for q in range(2):
                    pg = P[q]
                    for c in range(2):
                        k.mm(pg[:, 0:n], wglu[:, c, q * 128:(q + 1) * 128], z[:, c, 0:n], start=(c == 0), stop=(c == 1))
                    sg = g1[q * 2]
                    k.act(sg[:, 0:n], pg[:, 0:n], AF.Sigmoid, bias=vec[:, q, 1:2])
                    k.tt("dve", OT[:, q, t0:t0 + n], z[:, q, 0:n], sg[:, 0:n], ALU.mult)
            dbg_tap(C, "OTs5%d" % l, OT, [128, 2, T], BF16)
            k.dma("sp", [(otd[4:6].v(otd.ap[4:6].rearrange("c p t -> p c t")), OT)])


def host_m2_inputs(inp, l):
    o = {}
    cw = inp["m2_conv_w"][l]
    v = np.zeros((128, 6, 5), np.float32)
    for kk in range(4):
        v[:, :, kk] = col_layout(cw[kk])
    v[:, :, 4] = col_layout(inp["m2_conv_b"][l])
    o["m2vec"] = v
    o["m2row"] = np.ascontiguousarray(np.concatenate([inp["m2_a_log"][l].reshape(-1), inp["m2_dt_bias"][l].reshape(-1),
                                                      inp["m2_d"][l].reshape(-1), inp["m2_g_norm"][l].reshape(-1)])[None, :].astype(np.float32))
    return o


def host_m2_masks():
    i = np.arange(128)
    le = (i[:, None] <= i[None, :]).astype(np.float32)
    ge = (i[:, None] >= i[None, :]).astype(np.float32)
    su = (i[:, None] > i[None, :]).astype(np.float32)
    sl = (i[:, None] < i[None, :]).astype(np.float32)
    return np.ascontiguousarray(np.stack([le, ge, su, sl], 0).transpose(1, 0, 2))


def m2_branch(C, l, hT, otd):
    k = C.k
    Lw = C.L[l]
    P = C.ps
    with Scope(k):
        xc = k.sb("m2xc", [128, 6, T], BF16)
        xtok = k.sb("m2xtok", [128, NT, 512], BF16)
        zs = k.sb("m2zs", [128, NT, 256], BF16)
        dt = k.sb("m2dt", [128, NT, 8], F32)
        dA = k.sb("m2dA", [128, NT, 8], F32)
        ysum = k.sb("m2ys", [128, NT, 256], F32)
        row = k.sb("m2row", [128, 276], F32)
        msk = k.sb("m2msk", [128, 4, 128], F32)
        mskb = k.sb("m2mskb", [128, 2, 128], BF16)
        k.dma("sp", [(row, Lw.m2row.v(Lw.m2row.ap.partition_broadcast(128))), (msk, C.m2msk_in)])
        k.copy("dve", mskb, msk[:, 0:2, :])
        LE, GE, SU, SL = msk[:, 0, :], msk[:, 1, :], msk[:, 2, :], msk[:, 3, :]
        aneg = k.sb("m2aneg", [128, 8], F32)
        k.act(aneg, row[:, 0:8], AF.Exp)
        k.ts("dve", aneg, aneg, -1.0, ALU.mult)
        with Scope(k):
            wx = k.sb("m2wx", [128, 8, 768], BF16)
            wz = k.sb("m2wz", [128, 8, 264], BF16)
            cv = k.sb("m2cv", [128, 6, 5], F32)
            k.dma("sp", [(cv, Lw.m2vec)])
            with Scope(k):
                stg = Stager(C, 2112, n=2)
                for j in range(6):
                    stg.load(wx[:, :, j * 128:(j + 1) * 128], Lw.wfm[:, (13 + j) * 128:(14 + j) * 128])
                stg.load(wz, Lw.wtm[:, 256:520])
            xpre = k.sb("m2xpre", [128, 6, T], BF16)
            for bi, (t0, n) in enumerate(TBLK):
                for c6 in range(6):
                    pp = P[c6 % 4]
                    proj_fm(C, hT, wx[:, :, c6 * 128:(c6 + 1) * 128], t0, n, pp)
                    k.copy("dve" if c6 % 2 == 0 else "act", xpre[:, c6, t0:t0 + n], pp[:, 0:n])
            acc = [k.sb("m2acc%d" % i, [128, T], F32) for i in range(2)]
            for c6 in range(6):
                a = acc[c6 % 2]
                en = "dve"
                k.ts(en, a, xpre[:, c6, :], cv[:, c6, 2:3], ALU.mult, cv[:, c6, 4:5], ALU.add)
                for (lo, hi) in ((0, CTX), (CTX, T)):
                    k.stt(a[:, lo + 2:hi], xpre[:, c6, lo:hi - 2], cv[:, c6, 0:1], a[:, lo + 2:hi], ALU.mult, ALU.add)
                    k.stt(a[:, lo + 1:hi], xpre[:, c6, lo:hi - 1], cv[:, c6, 1:2], a[:, lo + 1:hi], ALU.mult, ALU.add)
                    k.stt(a[:, lo:hi - 1], xpre[:, c6, lo + 1:hi], cv[:, c6, 3:4], a[:, lo:hi - 1], ALU.mult, ALU.add)
                k.act(xc[:, c6, :], a, AF.Silu)
            tmpd = [k.sb("m2tmpd%d" % i, [128, 8], F32) for i in range(2)]
            for ti in range(NT):
                pz = P[4 + (ti % 2)]
                for kk in range(8):
                    k.mm(pz[:, 0:264], hT[:, kk, ti * 128:(ti + 1) * 128], wz[:, kk, :], start=(kk == 0), stop=(kk == 7))
                k.act(zs[:, ti, :], pz[:, 0:256], AF.Silu)
                td = tmpd[ti % 2]
                k.tt("dve", td, pz[:, 256:264], row[:, 8:16], ALU.add)
                k.act(td, td, AF.Exp)
                k.act(dt[:, ti, :], td, AF.Ln, bias=1.0)
            k.tt("dve", dA, dt, bc(aneg, [128, NT, 8], 1), ALU.mult)
            for ti in range(NT):
                pt = P[6 + (ti % 2)]
                ptv = pt.v(pt.ap.bitcast(BF16)[:, 0:512].rearrange("p (c t) -> p c t", t=128))
                for c4 in range(4):
                    k.tr(ptv[:, c4, :], xc[:, c4, ti * 128:(ti + 1) * 128], C.ident_b)
                k.copy("dve" if ti % 2 == 0 else "act", xtok[:, ti, :], pt.v(pt.ap.bitcast(BF16)[:, 0:512]))
        dbg_tap(C, "m2xc%d" % l, xc, [128, 6, T], BF16)
        dbg_tap(C, "m2dt%d" % l, dt, [128, NT, 8])
        dsk = row[:, 16:20]
        k.tt("dve", ysum.v(ysum.ap.rearrange("p t (h d) -> p t h d", h=4)),
             xtok.v(xtok.ap[:, :, 0:256].rearrange("p t (h d) -> p t h d", h=4)),
             row.v(row.ap[:, 16:20].unsqueeze(1).unsqueeze(3).to_broadcast([128, NT, 4, 64])), ALU.mult)
        with Scope(k):
            Sst = k.sb("m2S", [128, 8, 64], F32)
            Sbf = k.sb("m2Sb", [128, 8, 64], BF16)
            k.memset("dve", Sst, 0.0)
            k.memset("dve", Sbf, 0.0)
            GTm = [k.sb("m2GT%d" % i, [128, 2, 128], F32) for i in range(2)]
            lhs = [k.sb("m2lhs%d" % i, [128, 128], F32) for i in range(2)]
            ex = [k.sb("m2ex%d" % i, [128, 128], F32) for i in range(2)]
            MT = [k.sb("m2MT%d" % i, [128, 128], BF16) for i in range(2)]
            xd = [k.sb("m2xd%d" % i, [128, 4, 64], BF16) for i in range(2)]
            xw = [k.sb("m2xw%d" % i, [128, 4, 64], BF16) for i in range(2)]
            yo = [k.sb("m2yo%d" % i, [128, 4, 64], F32) for i in range(2)]
            sc8 = [k.sb("m2sc%d" % i, [128, 4, 8], F32) for i in range(2)]
            it = 0
            n2 = 0
            units = [(ci, d) for ci in range(NT) for d in range(2)]

            def pro_heads(ci, d, mid=None):
                dsl = slice(d * 4, (d + 1) * 4)
                c = S5_ORDER[d][ci]
                cs = slice(c * 128, (c + 1) * 128)
                b2 = d
                pc = P[0]
                k.mm(pc[:, 0:8], C.ones_f, dA[:, c, :])
                k.mm(pc[:, 8:16], LE if d == 0 else GE, dA[:, c, :])
                sc = sc8[b2]
                k.copy("act", sc[:, 0, :], pc[:, 0:8])
                k.act(sc[:, 1, :], pc[:, 0:8], AF.Exp)
                k.act(sc[:, 2, :], pc[:, 8:16], AF.Exp)
                k.tt("dve", sc[:, 3, :], sc[:, 0, :], pc[:, 8:16], ALU.subtract)
                k.act(sc[:, 3, :], sc[:, 3, :], AF.Exp)
                gt = GTm[b2]
                for g in range(2):
                    k.mm(P[1][:, g * 128:(g + 1) * 128], xc[:, 2 + g, cs], xc[:, 4 + g, cs])
                pgv = P[1].v(P[1].ap[:, 0:256].rearrange("p (g l) -> p g l", g=2))
                k.tt("dve", gt, pgv, bc(msk[:, d, :], [128, 2, 128], 1), ALU.mult)
                xv = xtok.v(xtok.ap[:, c, 0:256].rearrange("p (h e) -> p h e", h=4))
                k.tt("dve", xd[b2], xv, bc(dt[:, c, dsl], [128, 4, 64], 2), ALU.mult)
                k.tt("pool", xw[b2], xd[b2], bc(sc[:, 3, dsl], [128, 4, 64], 2), ALU.mult)
                PY, PS = P[4 + 2 * b2], P[5 + 2 * b2]
                for h in range(4):
                    g = h // 2
                    dh = d * 4 + h
                    b = h % 2
                    k.act(lhs[b], SU if d == 0 else SL, AF.Copy, scale=dA[:, c, dh:dh + 1])
                    ps_ = P[2 + b]
                    k.mm(ps_[:, 0:128], lhs[b], LE if d == 0 else GE)
                    k.act(ex[b], ps_[:, 0:128], AF.Exp)
                    k.tt("dve", MT[b], ex[b], gt[:, g, :], ALU.mult)
                    k.mm(PY[:, h * 64:(h + 1) * 64], MT[b], xd[b2][:, h, :])
                    k.mm(PY[:, 256 + h * 64:256 + (h + 1) * 64], xc[:, 4 + g, cs], Sbf[:, dh, :])
                    k.mm(PS[:, h * 64:(h + 1) * 64], xtok[:, c, 256 + g * 128:256 + (g + 1) * 128], xw[b2][:, h, :])
                    if h == 1 and mid is not None:
                        mid()

            def epi(ci, d):
                dsl = slice(d * 4, (d + 1) * 4)
                c = S5_ORDER[d][ci]
                b2 = d
                sc = sc8[b2]
                PY, PS = P[4 + 2 * b2], P[5 + 2 * b2]
                pyo = PY.v(PY.ap[:, 256:512].rearrange("p (h e) -> p h e", h=4))
                k.tt("dve", yo[b2], pyo, bc(sc[:, 2, dsl], [128, 4, 64], 2), ALU.mult)
                ysl = ysum[:, c, :]
                k.tt("dve", ysl, ysl, PY[:, 0:256], ALU.add)
                k.tt("pool", ysl, ysl, yo[b2].v(yo[b2].ap.rearrange("p h e -> p (h e)")), ALU.add)
                Sd = Sst[:, dsl, :]
                k.tt("pool", Sd, Sd, bc(sc[:, 1, dsl], [128, 4, 64], 2), ALU.mult)
                k.tt("dve", Sd, Sd, PS.v(PS.ap[:, 0:256].rearrange("p (h e) -> p h e", h=4)), ALU.add)
                k.copy("act", Sbf[:, dsl, :], Sd)
            for i, (ci, d) in enumerate(units):
                pro_heads(ci, d)
                if i >= 1:
                    epi(*units[i - 1])
            epi(*units[-1])
        dbg_tap(C, "m2ys%d" % l, ysum, [128, NT, 256])
        with Scope(k):
            OT = k.sb("OTm2", [128, 2, T], BF16)
            yg = [k.sb("m2yg%d" % i, [128, 256], F32) for i in range(2)]
            yb = [k.sb("m2yb%d" % i, [128, 256], BF16) for i in range(2)]
            junk = k.sb("m2junk", [128, 256], BF16)
            st = [k.sb("m2st%d" % i, [128, 2], F32) for i in range(2)]
            for ti in range(NT):
                y, s = yg[ti % 2], st[ti % 2]
                k.tt("dve", y, ysum[:, ti, :], zs[:, ti, :], ALU.mult)
                k.act(junk, y, AF.Square, accum=s[:, 0:1])
                k.act(s[:, 1:2], s[:, 0:1], AF.Sqrt, bias=EPS, scale=1.0 / 256)
                k.recip(s[:, 1:2], s[:, 1:2])
                k.stt(yb[ti % 2], y, s[:, 1:2], row[:, 20:276], ALU.mult, ALU.mult)
                pt = P[6 + (ti % 2)]
                ptv = pt.v(pt.ap.bitcast(BF16)[:, 0:256].rearrange("p (c t) -> p c t", t=128))
                for q in range(2):
                    k.tr(ptv[:, q, :], yb[ti % 2][:, q * 128:(q + 1) * 128], C.ident_b)
                k.copy("act", OT[:, :, ti * 128:(ti + 1) * 128], ptv)
            dbg_tap(C, "OTm2%d" % l, OT, [128, 2, T], BF16)
            k.dma("sp", [(otd[6:8].v(otd.ap[6:8].rearrange("c p t -> p c t")), OT)])


def host_merge_inputs(inp, l):
    o = {}
    o["wbr"] = np.ascontiguousarray(inp["w_branch"][l].reshape(1024, 1024))
    o["wout"] = np.ascontiguousarray(inp["w_out"][l])
    return o


def merge_phase(C, l, hT, yT):
    k = C.k
    Lw = C.L[l]
    P = C.ps
    with Scope(k):
        OTall = k.sb("OTall", [128, 8, T], BF16)
        k.dma("sp", [(OTall, C.otd.v(C.otd.ap.rearrange("c p t -> p c t")))])
        wb = k.sb("wb", [128, 8, 1024], BF16)
        wg = k.sb("wg", [128, 8, 4, 512], BF16)
        stg = Stager(C, 4096, n=2)
        for hh in range(2):
            stg.load(wb[:, :, hh * 512:(hh + 1) * 512], Lw.wbr[:, hh * 512:(hh + 1) * 512])
        sg = [k.sb("msg%d" % i, [128, 512], F32) for i in range(2)]
        ya = [k.sb("mya%d" % i, [128, 512], F32) for i in range(2)]
        yo = [k.sb("myo%d" % i, [128, 512], BF16) for i in range(2)]
        it = 0
        nq = 0
        for half in range(2):
            for j in range(4):
                c0 = 2472 + j * 1024 + half * 512
                stg.load(wg[:, :, j, :], Lw.w_in[:, c0:c0 + 512])
            for n4 in range(4):
                nch = half * 4 + n4
                for (t0, n) in (TBLK if C.ctx_out else TBLK_LAT):
                    y = ya[nq % 2]
                    nq += 1
                    for j in range(4):
                        b = it % 2
                        it += 1
                        pg, pb = P[2 * b], P[2 * b + 1]
                        for kk in range(8):
                            k.mm(pg[:, 0:n], wg[:, kk, j, n4 * 128:(n4 + 1) * 128], hT[:, kk, t0:t0 + n],
                                 start=(kk == 0), stop=(kk == 7))
                        for c in range(2):
                            k.mm(pb[:, 0:n], wb[:, 2 * j + c, nch * 128:(nch + 1) * 128], OTall[:, 2 * j + c, t0:t0 + n],
                                 start=(c == 0), stop=(c == 1))
                        k.act(sg[b][:, 0:n], pg[:, 0:n], AF.Sigmoid)
                        if j == 0:
                            k.tt("dve", y[:, 0:n], sg[b][:, 0:n], pb[:, 0:n], ALU.mult)
                        else:
                            k.tt("dve", sg[b][:, 0:n], sg[b][:, 0:n], pb[:, 0:n], ALU.mult)
                            k.tt("pool", y[:, 0:n], y[:, 0:n], sg[b][:, 0:n], ALU.add)
                    o = yo[nq % 2]
                    k.copy("act", o[:, 0:n], y[:, 0:n])
                    k.dma("sp", [(C.ytd2[nch, :, t0:t0 + n], o[:, 0:n])])


def outproj_phase(C, l, yT):
    k = C.k
    Lw = C.L[l]
    with Scope(k):
        wout = k.sb("wout", [128, 8, 1024], BF16)
        yT = k.sb("yT", [128, 8, T], BF16)
        k.dma("sp", [(yT, C.ytd2.v(C.ytd2.ap.rearrange("c p t -> p c t")))])
        stg = Stager(C, 4096, n=2)
        for hh in range(2):
            stg.load(wout[:, :, hh * 512:(hh + 1) * 512], Lw.wout[:, hh * 512:(hh + 1) * 512])

        def src(ti, dst):
            for hh in range(2):
                for kk in range(8):
                    k.mm(dst[hh], yT[:, kk, ti * 128:(ti + 1) * 128], wout[:, kk, hh * 512:(hh + 1) * 512],
                         start=(kk == 0), stop=(kk == 7))
        residual_epilogue(C, src, C.G1b, "mix%d" % l)


def residual_epilogue(C, src, Gb, tag, final=False):
    k = C.k
    P = C.ps
    with Scope(k):
        xt = [k.sb("ex%d" % i, [128, D], F32) for i in range(2)]
        ft = [k.sb("ef%d" % i, [128, D], F32) for i in range(2)]
        junk = k.sb("ejunk", [128, 512], BF16)
        st = [k.sb("est%d" % i, [128, 4], F32) for i in range(2)]
        for ti in range(2 if (final or not C.ctx_out) else 0, NT):
            v = 1 if ti < 2 else 0
            b = ti % 2
            dst = [P[2 * b], P[2 * b + 1]]
            k.dma("sp", [(xt[b], C.xr[ti])])
            r_ = src(ti, dst)
            if r_ is not None:
                dst = r_
            s = st[b]
            for hh in range(2):
                k.act(junk, dst[hh], AF.Square, accum=s[:, hh:hh + 1])
            k.tt("dve", s[:, 2:3], s[:, 0:1], s[:, 1:2], ALU.add)
            k.act(s[:, 3:4], s[:, 2:3], AF.Sqrt, bias=EPS, scale=1.0 / D)
            k.recip(s[:, 3:4], s[:, 3:4])
            for hh in range(2):
                sl = slice(hh * 512, (hh + 1) * 512)
                k.stt(ft[b][:, sl], dst[hh], s[:, 3:4], Gb[:, v, sl], ALU.mult, ALU.mult)
                k.tt("pool", xt[b][:, sl], xt[b][:, sl], ft[b][:, sl], ALU.add)
            if final:
                k.dma("sp", [(C.out_t[ti - 2], xt[b])])
            else:
                k.dma("sp", [(C.xres_t[ti], xt[b])])
                C.xr[ti] = C.xres_t[ti]
        if final:
            C.final_done = True


def host_ffn_consts():
    sel = np.zeros((128, 16, 128), np.float32)
    for e in range(16):
        sel[e, e, :] = 1.0
    return {"selT": sel}


def ffn_phase(C, l, inp_names=None):
    k = C.k
    Lw = C.L[l]
    P = C.ps
    with Scope(k):
        h2T = k.sb("h2T", [128, 8, T], BF16)
        coefT = k.sb("coefT", [128, T], F32)
        k.memset("pool", coefT, 0.0)
        with Scope(k):
            wr = k.sb("wr", [128, 8, 16], F32)
            k.dma("sp", [(wr, Lw.w_router.v(Lw.w_router.ap.rearrange("(k p) e -> p k e", p=128)))])
            xnf = [k.sb("rxnf%d" % i, [128, D], F32) for i in range(3)]
            h32 = [k.sb("rh32%d" % i, [128, 8, 128], F32) for i in range(3)]
            sm = [k.sb("rsm%d" % i, [128, 20], F32) for i in range(2)]
            aff = [k.sb("raff%d" % i, [128, 16], F32) for i in range(2)]
            A, Bc = C.A2, C.mod[:, 24:32, :]

            def router(i, x, rstd):
                v = 1 if i < 2 else 0
                b = i % 2
                k.ts("pool", xnf[b], x, rstd, ALU.mult)
                for hh in range(2):
                    pt = P[4 + hh]
                    for kk in range(4):
                        kc = hh * 4 + kk
                        k.tr(pt[:, kk * 128:(kk + 1) * 128], xnf[b][:, kc * 128:(kc + 1) * 128], C.ident_f)
                    for kk in range(4):
                        kc = hh * 4 + kk
                        k.ts("dve", h32[b][:, kc, :], pt[:, kk * 128:(kk + 1) * 128], A[:, kc:kc + 1, v], ALU.mult,
                             Bc[:, kc:kc + 1, v], ALU.add)
                pl = P[6]
                for kk in range(8):
                    k.mm(pl[:, 0:16], h32[b][:, kk, :], wr[:, kk, :], start=(kk == 0), stop=(kk == 7))
                s = sm[b]
                k.ins("dve", lambda e: e.reduce_max(out=s[:, 0:1].ap, in_=pl[:, 0:16].ap, axis=AX.X), [pl], [s])
                k.ts("dve", s[:, 1:2], s[:, 0:1], -1.0, ALU.mult)
                k.act(aff[b], pl[:, 0:16], AF.Exp, bias=s[:, 1:2], accum=s[:, 2:3])
                k.recip(s[:, 3:4], s[:, 2:3])
                k.ts("dve", aff[b], aff[b], s[:, 3:4], ALU.mult)
                pa = P[7]
                k.tr(pa[0:16, 0:128], aff[b], C.ident_f)
                k.copy("act", coefT[0:16, i * 128:(i + 1) * 128], pa[0:16, 0:128])
            norm_to_hT(C, h2T, A, Bc, "ffn", extra=router)
            dbg_tap(C, "aff%d" % l, coefT[0:16, :], [16, T])
            work = k.sb("rwork", [16, T], F32)
            m8 = k.sb("rm8", [16, 8], F32)
            k.copy("dve", work, coefT[0:16, :])
            for (lo, hi, cap) in ((0, CTX, 2 * CTX // 16), (CTX, T, 2 * SEQ // 16)):
                wv_ = work[:, lo:hi]
                for r in range(cap // 8):
                    k.ins("dve", lambda e, w=wv_: e.max(out=m8.ap, in_=w.ap), [wv_], [m8])
                    if r < cap // 8 - 1:
                        k.ins("dve", lambda e, w=wv_: e.match_replace(out=w.ap, in_to_replace=m8.ap, in_values=w.ap,
                                                                     imm_value=-1.0), [m8, wv_], [wv_])
                msk = k.sb("rmsk", [16, hi - lo], F32)
                k.ts("dve", msk, coefT[0:16, lo:hi], m8[:, 7:8], ALU.is_ge)
                k.tt("dve", coefT[0:16, lo:hi], coefT[0:16, lo:hi], msk, ALU.mult)
        dbg_tap(C, "coefT%d" % l, coefT[0:16, :], [16, T])
        dbg_tap(C, "h2T%d" % l, h2T, [128, 8, T], BF16)
        if DEVSTOP == "router":
            return
        with Scope(k):
            sel = k.sb("selT", [128, 16, 128], F32)
            k.dma("sp", [(sel, C.selT_in)])
            Wg = k.sb("Wg", [128, 8, 1024], BF16)
            Wu = k.sb("Wu", [128, 8, 1024], BF16)
            Wd = k.sb("Wd", [128, 8, 1024], BF16)
            stg = Stager(C, 4096, n=2)
            cbs = [k.sb("fcb%d" % i, [128, 512], F32) for i in range(2)]
            actT = [k.sb("factT%d" % i, [128, 8, 512], BF16) for i in range(2)]
            sg = [k.sb("fsg%d" % i, [128, 512], F32) for i in range(2)]
            ost = [k.sb("fost%d" % i, [128, D], F32) for i in range(2)]
            it = 0
            nexp = DEVNEXP
            for e in range(nexp):
                for (Wt, src) in ((Wg, Lw.w_gate), (Wu, Lw.w_up), (Wd, Lw.w_down)):
                    for hh in range(2):
                        stg.load(Wt[:, :, hh * 512:(hh + 1) * 512], src[e, :, hh * 512:(hh + 1) * 512])
                for bi, (t0, n) in enumerate(TBLK):
                    cb = cbs[bi % 2]
                    at = actT[bi % 2]
                    pc = P[6]
                    k.mm(pc[:, 0:n], sel[:, e, :], coefT[:, t0:t0 + n])
                    k.copy("act", cb[:, 0:n], pc[:, 0:n])
                    for f in range(8):
                        b = it % 2
                        it += 1
                        pg, pu = P[2 * b], P[2 * b + 1]
                        proj_fm(C, h2T, Wg[:, :, f * 128:(f + 1) * 128], t0, n, pg)
                        proj_fm(C, h2T, Wu[:, :, f * 128:(f + 1) * 128], t0, n, pu)
                        k.act(sg[b][:, 0:n], pg[:, 0:n], AF.Silu)
                        k.tt("dve", sg[b][:, 0:n], sg[b][:, 0:n], pu[:, 0:n], ALU.mult)
                        k.tt("pool", at[:, f, 0:n], sg[b][:, 0:n], cb[:, 0:n], ALU.mult)
                    for j in range(n // 128):
                        ti = t0 // 128 + j
                        o = ost[ti % 2]
                        for hh in range(2):
                            po = P[4 + hh]
                            for f in range(8):
                                k.mm(po, at[:, f, j * 128:(j + 1) * 128], Wd[:, f, hh * 512:(hh + 1) * 512],
                                     start=(f == 0), stop=(f == 7))
                            k.copy("act" if hh == 0 else "dve", o[:, hh * 512:(hh + 1) * 512], po)
                        k.dma("pool", [(C.facc_t[ti], o)], accum=(e > 0))
    with Scope(k):
        fin = [k.sb("ffin%d" % i, [128, D], F32) for i in range(2)]

        def src(ti, dst):
            f = fin[ti % 2]
            k.dma("pool", [(f, C.facc_t[ti])])
            return [f[:, 0:512], f[:, 512:1024]]
        residual_epilogue(C, src, C.G2b, "ffn%d" % l, final=(l == C.last_layer and "xres" not in C.dbg_names))


CSEL = 2 * CTX // 16 + 2 * SEQ // 16


def host_ffn_consts2():
    ic = np.broadcast_to(np.arange(CSEL, dtype=np.float32)[None, :], (128, CSEL))
    ip = np.arange(128, dtype=np.float32)[:, None] + 128.0 * np.arange(3, dtype=np.float32)[None, :]
    return {"iotac": np.ascontiguousarray(ic), "iotap": np.ascontiguousarray(ip)}


def ffn_phase_gather(C, l):
    k = C.k
    Lw = C.L[l]
    P = C.ps
    A, Bc = C.A2, C.mod[:, 24:32, :]
    with Scope(k):
        xn_tok = k.sb("xn_tok", [128, NT, D], BF16)
        coefT = k.sb("coefT", [128, T], F32)
        posr = k.sb("posr", [128, T], F32)
        tokc = k.sb("tokc", [128, NT, 32], F32)
        Wg = k.sb("Wg", [128, 8, 1024], BF16)
        Wu = k.sb("Wu", [128, 8, 1024], BF16)
        Wd = k.sb("Wd", [128, 8, 1024], BF16)
        for (Wt_, src_) in ((Wg, Lw.w_gate), (Wu, Lw.w_up), (Wd, Lw.w_down)):
            for qq in range(2):
                sv_ = src_[0, :, qq * 512:(qq + 1) * 512]
                k.dma("pool", [(Wt_[:, :, qq * 512:(qq + 1) * 512], sv_.v(sv_.ap.rearrange("(k p) n -> p k n", p=128)))])
        k.memset("pool", coefT, 0.0)
        k.memset("pool", posr, 0.0)
        with Scope(k):
            wr = k.sb("wr", [128, 8, 16], F32)
            k.dma("sp", [(wr, Lw.w_router.v(Lw.w_router.ap.rearrange("(k p) e -> p k e", p=128)))])
            xin = [k.sb("gx%d" % i, [128, D], F32) for i in range(3)]
            junk = k.sb("gjunk", [128, D], BF16)
            st = [k.sb("gst%d" % i, [128, 2], F32) for i in range(3)]
            xnf = [k.sb("rxnf%d" % i, [128, D], F32) for i in range(3)]
            h32 = [k.sb("rh32%d" % i, [128, 8, 128], F32) for i in range(3)]
            sm = [k.sb("rsm%d" % i, [128, 20], F32) for i in range(2)]
            aff = [k.sb("raff%d" % i, [128, 16], F32) for i in range(2)]
            def stA(i):
                x, s_ = xin[i % 3], st[i % 3]
                k.dma("sp", [(x, C.xr[i])])
                k.act(junk, x, AF.Square, accum=s_[:, 0:1])
                k.act(s_[:, 1:2], s_[:, 0:1], AF.Sqrt, bias=EPS, scale=1.0 / D)
                k.recip(s_[:, 1:2], s_[:, 1:2])
                k.ts("dve", xn_tok[:, i, :], x, s_[:, 1:2], ALU.mult)
                k.act(xnf[i % 3], x, AF.Identity, scale=s_[:, 1:2])

            def stB(i):
                v = 1 if i < 2 else 0
                b = i % 3
                for hh in range(2):
                    pt = P[4 + hh]
                    for kk in range(4):
                        kc = hh * 4 + kk
                        k.tr(pt[:, kk * 128:(kk + 1) * 128], xnf[b][:, kc * 128:(kc + 1) * 128], C.ident_f)
                    for kk in range(4):
                        kc = hh * 4 + kk
                        if hh == 0:
                            k.ts("dve", h32[b][:, kc, :], pt[:, kk * 128:(kk + 1) * 128], A[:, kc:kc + 1, v], ALU.mult,
                                 Bc[:, kc:kc + 1, v], ALU.add)
                        else:
                            k.act(h32[b][:, kc, :], pt[:, kk * 128:(kk + 1) * 128], AF.Identity,
                                  bias=Bc[:, kc:kc + 1, v], scale=A[:, kc:kc + 1, v])
                pl = P[6 + (i % 2)]
                for kk in range(8):
                    k.mm(pl[:, 0:16], h32[b][:, kk, :], wr[:, kk, :], start=(kk == 0), stop=(kk == 7))

            def stC(i):
                b = i % 2
                pl = P[6 + (i % 2)]
                sv = sm[b]
                k.ins("dve", lambda e, sv=sv, pl=pl: e.reduce_max(out=sv[:, 0:1].ap, in_=pl[:, 0:16].ap, axis=AX.X), [pl], [sv])
                k.ts("dve", sv[:, 1:2], sv[:, 0:1], -1.0, ALU.mult)
                k.act(aff[b], pl[:, 0:16], AF.Exp, bias=sv[:, 1:2], accum=sv[:, 2:3])
                k.recip(sv[:, 3:4], sv[:, 2:3])
                k.ts("dve", aff[b], aff[b], sv[:, 3:4], ALU.mult)
                pa = P[2 + (i % 2)]
                k.tr(pa[0:16, 0:128], aff[b], C.ident_f)
                k.copy("act", coefT[0:16, i * 128:(i + 1) * 128], pa[0:16, 0:128])
            tiles = list(range(NT)) if C.ctx_out else list(range(2, NT))
            if not C.ctx_out:
                k.memset("pool", xn_tok[:, 0:2, :], 0.0)
            for step in range(len(tiles) + 2):
                if step < len(tiles):
                    stA(tiles[step])
                if 1 <= step <= len(tiles):
                    stB(tiles[step - 1])
                if step >= 2:
                    stC(tiles[step - 2])
            work = k.sb("rwork", [16, T], F32)
            msk = k.sb("rmsk", [16, T], F32)
            m8 = k.sb("rm8", [16, 8], F32)
            k.copy("dve", work, coefT[0:16, :])
            if not C.ctx_out:
                k.memset("dve", msk[:, 0:CTX], 0.0)
            for (lo, hi, cap, off) in ((0, CTX, 2 * CTX // 16, 0), (CTX, T, 2 * SEQ // 16, 2 * CTX // 16)):
                if lo == 0 and not C.ctx_out:
                    continue
                wv_ = work[:, lo:hi]
                for r in range(cap // 8):
                    k.ins("dve", lambda e, w=wv_: e.max(out=m8.ap, in_=w.ap), [wv_], [m8])
                    if r < cap // 8 - 1:
                        k.ins("dve", lambda e, w=wv_: e.match_replace(out=w.ap, in_to_replace=m8.ap, in_values=w.ap,
                                                                     imm_value=-1.0), [m8, wv_], [wv_])
                k.ts("dve", msk[:, lo:hi], coefT[0:16, lo:hi], m8[:, 7:8], ALU.is_ge)
                k.tt("dve", coefT[0:16, lo:hi], coefT[0:16, lo:hi], msk[:, lo:hi], ALU.mult)
                k.scan(posr[0:16, lo:hi], msk[:, lo:hi], msk[:, lo:hi], 0.0, ALU.add, ALU.max)
                k.ts("dve", posr[0:16, lo:hi], posr[0:16, lo:hi], float(off - 1), ALU.add)
            if not C.ctx_out:
                k.memset("dve", tokc[:, 0:2, :], 0.0)
            for ti in (range(NT) if C.ctx_out else range(2, NT)):
                pa = P[6 + (ti % 2)]
                k.tr(pa[:, 0:16], posr[0:16, ti * 128:(ti + 1) * 128], C.ident_f[0:16, 0:16])
                k.tr(pa[:, 16:32], msk[:, ti * 128:(ti + 1) * 128], C.ident_f[0:16, 0:16])
                k.copy("act", tokc[:, ti, :], pa[:, 0:32])
        dbg_tap(C, "coefT%d" % l, coefT[0:16, :], [16, T])
        dbg_tap(C, "posr%d" % l, posr[0:16, :], [16, T])
        dbg_tap(C, "tokc%d" % l, tokc, [128, NT, 32])
        with Scope(k):
            sel = k.sb("selT", [128, 16, 128], F32)
            iotac = k.sb("iotac", [128, CSEL], F32)
            iotap = k.sb("iotap", [128, 3], F32)
            k.dma("sp", [(sel, C.selT_in), (iotac, C.iotac_in), (iotap, C.iotap_in)])
            stg = Stager(C, 2048, n=3)
            Sel = k.sb("Sel", [128, NT, CSEL], BF16)
            SelT2 = [k.sb("SelT%d" % i, [128, 3, T], BF16) for i in range(2)]
            xsT = k.sb("xsT", [128, 8, CSEL], BF16)
            actT = k.sb("actT", [128, 8, 384], BF16)
            k.memset("pool", actT, 0.0)
            ys2 = [k.sb("ys%d" % i, [128, 3, D], BF16) for i in range(2)]
            sg = [k.sb("fsg%d" % i, [128, CSEL], F32) for i in range(2)]
            cbs = [k.sb("fcb%d" % i, [128, 512], F32) for i in range(2)]
            ost = [k.sb("fost%d" % i, [128, D], F32) for i in range(2)]
            it = 0
            nld = [0]

            def load_w(e, Wt, src):
                if "W" in DEVFLAGS and e > 0:
                    return
                for qq in range(2):
                    srcv = src[e, :, qq * 512:(qq + 1) * 512]
                    k.dma("pool", [(Wt[:, :, qq * 512:(qq + 1) * 512], srcv.v(srcv.ap.rearrange("(k p) n -> p k n", p=128)))])
            ne = DEVNEXP
            c0 = 2 * CTX // 16

            def st_sel(e):
                for ti in range(NT):
                    k.ts("dve", Sel[:, ti, :], iotac, tokc[:, ti, e:e + 1], ALU.is_equal,
                         tokc[:, ti, 16 + e:17 + e], ALU.mult)

            def st_gather(e):
                for kk in range(8):
                    pgt = P[kk % 2]
                    for ti in range(NT):
                        k.mm(pgt[:, 0:CSEL], xn_tok[:, ti, kk * 128:(kk + 1) * 128], Sel[:, ti, :],
                             start=(ti == 0), stop=(ti == NT - 1))
                    k.act(xsT[:, kk, 0:c0], pgt[:, 0:c0], AF.Identity, bias=Bc[:, kk:kk + 1, 1], scale=A[:, kk:kk + 1, 1])
                    k.ts("dve", xsT[:, kk, c0:CSEL], pgt[:, c0:CSEL], A[:, kk:kk + 1, 0], ALU.mult, Bc[:, kk:kk + 1, 0], ALU.add)

            def st_gateup(e):
                for f in range(8):
                    b = f % 2
                    pg, pu = P[2 + 2 * b], P[3 + 2 * b]
                    for kk in range(8):
                        k.mm(pg[:, 0:CSEL], Wg[:, kk, f * 128:(f + 1) * 128], xsT[:, kk, :], start=(kk == 0), stop=(kk == 7))
                    for kk in range(8):
                        k.mm(pu[:, 0:CSEL], Wu[:, kk, f * 128:(f + 1) * 128], xsT[:, kk, :], start=(kk == 0), stop=(kk == 7))
                    k.act(sg[b], pg[:, 0:CSEL], AF.Silu)
                    k.tt("dve", actT[:, f, 0:CSEL], sg[b], pu[:, 0:CSEL], ALU.mult)

            def st_down(e):
                ys = ys2[e % 2]
                for ct in range(3):
                    for hh in range(2):
                        po = P[6 + hh]
                        for f in range(8):
                            k.mm(po, actT[:, f, ct * 128:(ct + 1) * 128], Wd[:, f, hh * 512:(hh + 1) * 512],
                                 start=(f == 0), stop=(f == 7))
                        k.copy("dve" if hh == 0 else "act", ys[:, ct, hh * 512:(hh + 1) * 512], po)

            def st_selT(e):
                ST = SelT2[e % 2]
                for bi, (t0, n) in enumerate(TBLK):
                    pp, pc = P[2 + 2 * (bi % 2)], P[3 + 2 * (bi % 2)]
                    k.mm(pp[:, 0:n], sel[:, e, :], posr[:, t0:t0 + n])
                    k.mm(pc[:, 0:n], sel[:, e, :], coefT[:, t0:t0 + n])
                    cb = cbs[bi % 2]
                    k.copy("act", cb[:, 0:n], pc[:, 0:n])
                    for ct in range(3):
                        k.stt(ST[:, ct, t0:t0 + n], pp[:, 0:n], iotap[:, ct:ct + 1], cb[:, 0:n], ALU.is_equal, ALU.mult)

            def st_scatter(e):
                for ti in range(NT):
                    o = ost[ti % 2]
                    for hh in range(2):
                        po = P[4 + (ti % 2) * 2 + hh]
                        n_ = 0
                        for ee in (e - 1, e):
                            for ct in range(3):
                                k.mm(po, SelT2[ee % 2][:, ct, ti * 128:(ti + 1) * 128], ys2[ee % 2][:, ct, hh * 512:(hh + 1) * 512],
                                     start=(n_ == 0), stop=(n_ == 5))
                                n_ += 1
                        k.copy("dve" if hh == 0 else "act", o[:, hh * 512:(hh + 1) * 512], po)
                    k.dma("pool", [(C.facc_t[ti], o)], accum=(e > 1))
            st_sel(0)
            st_gather(0)
            for e in range(ne):
                st_gateup(e)
                if e + 1 < ne:
                    load_w(e + 1, Wg, Lw.w_gate)
                    load_w(e + 1, Wu, Lw.w_up)
                    st_sel(e + 1)
                st_down(e)
                if e + 1 < ne:
                    load_w(e + 1, Wd, Lw.w_down)
                    st_gather(e + 1)
                st_selT(e)
                if e % 2 == 1:
                    st_scatter(e)
    with Scope(k):
        fin = [k.sb("ffin%d" % i, [128, D], F32) for i in range(2)]

        def src(ti, dst):
            f = fin[ti % 2]
            k.dma("pool", [(f, C.facc_t[ti])])
            return [f[:, 0:512], f[:, 512:1024]]
        residual_epilogue(C, src, C.G2b, "ffn%d" % l, final=(l == C.last_layer and "xres" not in C.dbg_names))


def make_in_maps(inp):
    inp = {n: np.asarray(v) for n, v in inp.items()}
    shared = dict(host_consts())
    for l in range(DEPTH):
        for fn in (host_layer_inputs, host_mixer_inputs, host_s5_inputs, host_m2_inputs, host_merge_inputs):
            for n, v in fn(inp, l).items():
                shared["%s%d" % (n, l)] = np.ascontiguousarray(v)
        for n in ("w_router", "w_gate", "w_up", "w_down"):
            shared["%s%d" % (n, l)] = np.ascontiguousarray(inp[n][l])
    cc = col_layout(inp["c_ctx"])
    maps = []
    for b in range(inp["x"].shape[0]):
        m = dict(shared)
        m["x"] = np.ascontiguousarray(inp["x"][b])
        m["ctx"] = np.ascontiguousarray(inp["ctx"][b])
        m["cvec"] = np.ascontiguousarray(np.stack([col_layout(inp["c"][b]), cc], -1))
        maps.append(m)
    return maps


def kernel(**inputs):
    maps = make_in_maps(inputs)
    nc, C = build_program()
    res = run_bass_kernel_spmd(nc, maps, core_ids=list(range(len(maps))))
    return np.stack([np.asarray(r["out"], dtype=np.float32) for r in res.results], 0)
```

```python
import contextlib
import numpy as np
import concourse.bass as bass
import concourse.mybir as mybir
from concourse.bass_utils import run_bass_kernel_spmd

F32 = mybir.dt.float32
BF16 = mybir.dt.bfloat16
ALU = mybir.AluOpType
AF = mybir.ActivationFunctionType
AX = mybir.AxisListType

D = 1024
SEQ = 2048
CTX = 256
T = SEQ + CTX
NT = T // 128
DEPTH = 2
GRID_W = 64
N_IN = 6568
EPS = 1e-6
NEG = -30000.0
DEVSTOP = None
INORDER = ('pool', 'pe')
DEVFLAGS = ''
DEVLAYERS = DEPTH
DEVNEXP = 16


class Buf:
    __slots__ = ("name", "w", "r", "excl", "strictw")

    def __init__(self, name):
        self.name = name
        self.strictw = False
        self.excl = False
        self.w = None
        self.r = {}


class TV:
    __slots__ = ("ap", "bufs")

    def __init__(self, ap, bufs):
        self.ap = ap
        self.bufs = bufs

    def __getitem__(self, idx):
        return TV(self.ap[idx], self.bufs)

    def v(self, ap):
        return TV(ap, self.bufs)

    @property
    def shape(self):
        return self.ap.shape


class Eng:
    def __init__(self, name, eng, sem):
        self.name = name
        self.eng = eng
        self.sem = sem
        self.count = 0
        self.known = {}


class K:
    NDSEM = 20

    def __init__(self, nc, es):
        self.nc = nc
        self.es = es
        self.sems = {}
        self.engs = {}
        for name, eng in (("pe", nc.tensor), ("dve", nc.vector), ("act", nc.scalar),
                          ("pool", nc.gpsimd), ("sp", nc.sync)):
            s = es.enter_context(nc.semaphore("s_" + name))
            self.sems[name] = s
            self.engs[name] = Eng(name, eng, name)
        self.dq = {}
        for q in ("sp", "pool", "act"):
            lst = []
            for i in range(self.NDSEM):
                key = "d_%s_%d" % (q, i)
                self.sems[key] = es.enter_context(nc.semaphore(key))
                lst.append([key, 0])
            self.dq[q] = [lst, 0]
        self.ninstr = 0
        self.nwait = 0
        self.inorder = False
        self.uid = 0

    def sb(self, name, shape, dtype):
        self.uid += 1
        name = "%s_%d" % (name, self.uid)
        t = self.es.enter_context(self.nc.sbuf_tensor(name, list(shape), dtype))
        return TV(t.ap(), [Buf(name)])

    def ps(self, name, shape, dtype=F32):
        self.uid += 1
        name = "%s_%d" % (name, self.uid)
        t = self.es.enter_context(self.nc.psum_tensor(name, list(shape), dtype))
        b = Buf(name)
        b.excl = True
        return TV(t.ap(), [b])

    def dram(self, name, shape, dtype, kind="Internal"):
        t = self.nc.dram_tensor(name, list(shape), dtype, kind=kind)
        return TV(t.ap(), [Buf(name)])

    def split(self, tv, n, sl):
        out = []
        bufs = []
        for i in range(n):
            b = Buf("%s.%d" % (tv.bufs[0].name, i))
            bufs.append(b)
            out.append(TV(tv.ap[sl(i)], [b]))
        tv.bufs = bufs
        return out

    def _need(self, E, ev, waits, strict=False):
        if ev is None:
            return
        sem, val, clock = ev
        if E.known.get(sem, 0) >= val:
            return
        if sem == E.sem and self.inorder and E.name in INORDER and not strict:
            return
        if waits.get(sem, (0, None))[0] < val:
            waits[sem] = (val, clock)

    def _deps(self, E, reads, writes, skip_waw=()):
        waits = {}
        for tv in reads:
            if isinstance(tv, TV):
                for b in tv.bufs:
                    self._need(E, b.w, waits, b.strictw)
                    if b.excl:
                        for sem, (val, clock) in b.r.items():
                            if sem != E.sem:
                                self._need(E, (sem, val, clock), waits)
        for tv in writes:
            for b in tv.bufs:
                if b not in skip_waw:
                    self._need(E, b.w, waits, b.strictw)
                    for sem, (val, clock) in b.r.items():
                        self._need(E, (sem, val, clock), waits)
        for sem, (val, clock) in waits.items():
            if E.known.get(sem, 0) >= val:
                continue
            E.eng.wait_ge(self.sems[sem], val)
            self.nwait += 1
            E.known[sem] = val
            if clock:
                for s2, v2 in clock.items():
                    if E.known.get(s2, 0) < v2:
                        E.known[s2] = v2

    def _mark(self, ev, reads, writes):
        sem, val, clock = ev
        for tv in reads:
            if isinstance(tv, TV):
                for b in tv.bufs:
                    old = b.r.get(sem)
                    if old is None or old[0] < val:
                        b.r[sem] = (val, clock)
        for tv in writes:
            for b in tv.bufs:
                b.w = ev
                b.r = {}
                b.strictw = False

    def ins(self, en, fn, reads, writes, skip_waw=()):
        E = self.engs[en]
        self.inorder = True
        self._deps(E, reads, writes, skip_waw)
        self.inorder = False
        inst = fn(E.eng)
        E.count += 1
        inst.then_inc(self.sems[E.sem], 1)
        clock = dict(E.known)
        if en in INORDER:
            clock[E.sem] = E.count
        ev = (E.sem, E.count, clock)
        self._mark(ev, reads, writes)
        self.ninstr += 1
        return ev

    def dma(self, q, pairs, accum=False):
        if accum:
            q = "pool"
        E = self.engs[q]
        lst, idx = self.dq[q]
        slot = lst[idx % self.NDSEM]
        self.dq[q][1] = idx + 1
        key, total = slot
        reads = [p[1] for p in pairs]
        writes = [p[0] for p in pairs]
        self._deps(E, reads, writes)
        if total > 0 and E.known.get(key, 0) < total:
            E.eng.wait_ge(self.sems[key], total)
            E.known[key] = total
        for o, i in pairs:
            if accum:
                E.eng.dma_start(out=o.ap, in_=i.ap, accum_op=ALU.add).then_inc(self.sems[key], 16)
            else:
                E.eng.dma_start(out=o.ap, in_=i.ap).then_inc(self.sems[key], 16)
            total += 16
        slot[1] = total
        ev = (key, total, dict(E.known))
        self._mark(ev, reads, writes)
        self.ninstr += len(pairs)
        return ev

    def wait_all(self, en, tvs):
        E = self.engs[en]
        self._deps(E, tvs, [])

    def mm(self, out, lhsT, rhs, start=True, stop=True):
        skip = () if start else tuple(out.bufs)
        return self.ins("pe", lambda e: e.matmul(out.ap, lhsT.ap, rhs.ap, start=start, stop=stop),
                        [lhsT, rhs], [out], skip_waw=skip)

    def tr(self, out, in_, ident):
        return self.ins("pe", lambda e: e.transpose(out.ap, in_.ap, ident.ap), [in_, ident], [out])

    def act(self, out, in_, func, bias=None, scale=1.0, accum=None, en="act"):
        kw = {}
        rd = [in_]
        if bias is not None:
            kw["bias"] = bias.ap if isinstance(bias, TV) else bias
            rd.append(bias)
        if isinstance(scale, TV):
            kw["scale"] = scale.ap
            rd.append(scale)
        else:
            kw["scale"] = scale
        wr = [out]
        if accum is not None:
            kw["accum_out"] = accum.ap
            wr.append(accum)
        ev = self.ins("act", lambda e: e.activation(out.ap, in_.ap, func, **kw), rd, wr)
        if accum is not None:
            for b in accum.bufs:
                b.strictw = True
        return ev

    def tt(self, en, out, in0, in1, op):
        return self.ins(en, lambda e: e.tensor_tensor(out=out.ap, in0=in0.ap, in1=in1.ap, op=op),
                        [in0, in1], [out])

    def ts(self, en, out, in0, s1, op0, s2=None, op1=None, accum=None):
        a1 = s1.ap if isinstance(s1, TV) else s1
        a2 = s2.ap if isinstance(s2, TV) else s2
        kw = {}
        wr = [out]
        if op1 is not None:
            kw["op1"] = op1
        if accum is not None:
            kw["accum_out"] = accum.ap
            wr.append(accum)
        return self.ins(en, lambda e: e.tensor_scalar(out=out.ap, in0=in0.ap, scalar1=a1, scalar2=a2,
                                                      op0=op0, **kw), [in0, s1, s2], wr)

    def stt(self, out, in0, sc, in1, op0, op1, en="dve"):
        a = sc.ap if isinstance(sc, TV) else sc
        return self.ins(en, lambda e: e.scalar_tensor_tensor(out=out.ap, in0=in0.ap, scalar=a, in1=in1.ap,
                                                             op0=op0, op1=op1), [in0, sc, in1], [out])

    def copy(self, en, out, in_):
        if en == "act":
            return self.ins("act", lambda e: e.copy(out.ap, in_.ap), [in_], [out])
        return self.ins(en, lambda e: e.tensor_copy(out=out.ap, in_=in_.ap), [in_], [out])

    def recip(self, out, in_):
        return self.ins("dve", lambda e: e.reciprocal(out=out.ap, in_=in_.ap), [in_], [out])

    def memset(self, en, out, val):
        return self.ins(en, lambda e: e.memset(out.ap, val), [], [out])

    def scan(self, out, d0, d1, init, op0, op1):
        a = init.ap if isinstance(init, TV) else init
        return self.ins("dve", lambda e: e.tensor_tensor_scan(out=out.ap, data0=d0.ap, data1=d1.ap, initial=a,
                                                              op0=op0, op1=op1), [d0, d1, init], [out])


def col_layout(v):
    v = np.asarray(v, np.float32)
    return np.ascontiguousarray(v.reshape(-1, 128).T)


def host_consts():
    c = {}
    c["ident"] = np.eye(128, dtype=np.float32)
    c["cosf"], c["sinf"] = rope_tables()
    c["jmat"] = np.ascontiguousarray(np.eye(128, dtype=np.float32)[::-1])
    c["m2msk"] = host_m2_masks()
    c.update(host_ffn_consts())
    c.update(host_ffn_consts2())
    return c


def host_layer_inputs(inp, l):
    o = {}
    o["w_ada"] = np.ascontiguousarray(inp["w_ada"][l])
    o["bada"] = col_layout(inp["b_ada"][l])
    g = np.stack([col_layout(inp[n][l]) for n in ("g_pre_mix", "g_post_mix", "g_pre_ffn", "g_post_ffn")], 1)
    o["gvec"] = np.ascontiguousarray(g)
    o["w_in"] = np.ascontiguousarray(inp["w_in"][l])
    return o


class Ctx:
    pass


def build_program(dbg=()):
    nc = bass.Bass("TRN2", target_bir_lowering=False)
    es = contextlib.ExitStack()
    k = K(nc, es)
    C = Ctx()
    C.nc, C.k, C.es, C.dbg = nc, k, es, {}
    C.dbg_names = dbg

    def din(name, shape, dtype=F32):
        return k.dram(name, shape, dtype, kind="ExternalInput")

    C.x_in = din("x", [SEQ, D])
    C.ctx_in = din("ctx", [CTX, D])
    C.cvec = din("cvec", [128, 8, 2])
    C.ident_in = din("ident", [128, 128])
    C.cosf_in = din("cosf", [128, T], BF16)
    C.jmat_in = din("jmat", [128, 128])
    C.m2msk_in = din("m2msk", [128, 4, 128])
    C.selT_in = din("selT", [128, 16, 128])
    C.iotac_in = din("iotac", [128, CSEL])
    C.iotap_in = din("iotap", [128, 3])
    C.facc = k.dram("facc", [T, D], F32)
    C.facc_t = k.split(C.facc, NT, lambda i: (slice(i * 128, (i + 1) * 128), slice(None)))
    C.sinf_in = din("sinf", [128, T], BF16)
    C.otd = k.dram("otd", [8, 128, T], BF16)
    C.ytd2 = k.dram("ytd2", [8, 128, T], BF16)
    C.ytd = k.dram("ytd", [T, D], BF16)
    C.ytd_t = k.split(C.ytd, NT, lambda i: (slice(i * 128, (i + 1) * 128), slice(None)))
    C.L = []
    for l in range(DEPTH):
        Lw = Ctx()
        Lw.w_ada = din("w_ada%d" % l, [D, 6 * D])
        Lw.bada = din("bada%d" % l, [128, 48])
        Lw.gvec = din("gvec%d" % l, [128, 4, 8])
        Lw.w_in = din("w_in%d" % l, [D, N_IN])
        Lw.wfm = din("wfm%d" % l, [D, NFM])
        Lw.wtm = din("wtm%d" % l, [D, 520])
        Lw.wuq = din("wuq%d" % l, [256, 1024])
        Lw.wukv = din("wukv%d" % l, [128, 768])
        Lw.gmla = din("gmla%d" % l, [128, 3])
        Lw.nab = din("nab%d" % l, [6, 4, 128, 5, 128])
        Lw.s5par = din("s5par%d" % l, [3, 2048])
        Lw.m2vec = din("m2vec%d" % l, [128, 6, 5])
        Lw.wbr = din("wbr%d" % l, [1024, 1024])
        Lw.w_router = din("w_router%d" % l, [D, 16])
        Lw.w_gate = din("w_gate%d" % l, [16, D, D])
        Lw.w_up = din("w_up%d" % l, [16, D, D])
        Lw.w_down = din("w_down%d" % l, [16, D, D])
        Lw.wout = din("wout%d" % l, [1024, 1024])
        Lw.m2row = din("m2row%d" % l, [1, 276])
        Lw.s5BT = din("s5BT%d" % l, [128, 2, 8, 2, 128])
        Lw.s5CT = din("s5CT%d" % l, [128, 2, 8, 2, 128])
        Lw.s5vec = din("s5vec%d" % l, [128, 2, 2])
        Lw.s5wglu = din("s5wglu%d" % l, [256, 256])
        Lw.s5wu = din("s5wu%d" % l, [D, 256])
        C.L.append(Lw)
    C.out = k.dram("out", [SEQ, D], F32, kind="ExternalOutput")
    C.xres = k.dram("xres", [T, D], F32)
    C.xres_t = k.split(C.xres, NT, lambda i: (slice(i * 128, (i + 1) * 128), slice(None)))

    C.ident_f = k.sb("ident_f", [128, 128], F32)
    C.ident_b = k.sb("ident_b", [128, 128], BF16)
    C.ones_f = k.sb("ones_f", [128, 128], F32)
    C.ones_b = k.sb("ones_b", [128, 128], BF16)
    k.dma("sp", [(C.ident_f, C.ident_in)])
    k.copy("dve", C.ident_b, C.ident_f)
    k.memset("dve", C.ones_f, 1.0)
    k.memset("dve", C.ones_b, 1.0)
    C.ps = [k.ps("ps%d" % i, [128, 512], F32) for i in range(8)]

    C.xr = [C.ctx_in[i * 128:(i + 1) * 128, :] if i < 2 else C.x_in[(i - 2) * 128:(i - 1) * 128, :] for i in range(NT)]
    C.out_t = k.split(C.out, NT - 2, lambda i: (slice(i * 128, (i + 1) * 128), slice(None)))
    C.final_done = False
    C.last_layer = DEVLAYERS - 1

    for l in range(DEVLAYERS):
        layer(C, l)

    if not C.final_done:
        with Scope(k):
            stg = [k.sb("op%d" % i, [128, D], F32) for i in range(3)]
            for i in range(2, NT):
                s = stg[i % 3]
                k.dma("sp", [(s, C.xr[i])])
                k.dma("pool", [(C.out_t[i - 2], s)])
    if "xres" in C.dbg_names:
        d = k.dram("dbg_xres", [T, D], F32, kind="ExternalOutput")
        with Scope(k):
            stg = [k.sb("dx%d" % i, [128, D], F32) for i in range(2)]
            for i in range(NT):
                k.dma("sp", [(stg[i % 2], C.xr[i])])
                k.dma("sp", [(d[i * 128:(i + 1) * 128, :], stg[i % 2])])
        C.dbg["xres"] = d
    k.wait_all("sp", [C.out] + [C.dbg[n] for n in C.dbg])
    return nc, C


def dbg_tap(C, name, tv, shape, dtype=F32):
    if name not in C.dbg_names:
        return
    k = C.k
    d = k.dram("dbg_" + name, list(shape), dtype, kind="ExternalOutput")
    k.dma("sp", [(d, tv)])
    C.dbg[name] = d


def barrier(k):
    targets = {}
    for name, E in k.engs.items():
        if E.count:
            targets[E.sem] = E.count
    for q, (lst, _) in k.dq.items():
        for key, total in lst:
            if total:
                targets[key] = total
    for name, E in k.engs.items():
        for sem, val in targets.items():
            if E.known.get(sem, 0) < val:
                E.eng.wait_ge(k.sems[sem], val)
                E.known[sem] = val
                k.nwait += 1


class Scope:
    def __init__(self, k):
        self.k = k

    def __enter__(self):
        self.prev = self.k.es
        self.les = contextlib.ExitStack()
        self.k.es = self.les
        return self

    def __exit__(self, *a):
        barrier(self.k)
        self.k.es = self.prev
        self.les.close()
        return False


def bank(C, i, shape=None, dtype=F32):
    return C.ps[i]


def layer(C, l):
    k = C.k
    Lw = C.L[l]
    C.ctx_out = (l < DEPTH - 1)
    with Scope(k):
        mod = k.sb("mod", [128, 48, 2], F32)
        gv = k.sb("gv", [128, 4, 8], F32)
        k.dma("sp", [(gv, Lw.gvec)])
        with Scope(k):
            sc = k.sb("sc", [128, 8, 2], F32)
            scb = k.sb("scb", [128, 8, 2], BF16)
            bada = k.sb("bada", [128, 48], F32)
            k.dma("sp", [(sc, C.cvec), (bada, Lw.bada)])
            k.act(scb, sc, AF.Silu)
            wst = [k.sb("wada%d" % i, [128, 8, 768], F32) for i in range(2)]
            wbf = [k.sb("wadab%d" % i, [128, 8, 768], BF16) for i in range(2)]
            wv = Lw.w_ada.v(Lw.w_ada.ap.rearrange("(k p) n -> p k n", p=128))
            pm = C.ps[0]
            for jb in range(8):
                w = wst[jb % 2]
                wb_ = wbf[jb % 2]
                k.dma("sp" if jb % 2 == 0 else "pool", [(w, wv[:, :, jb * 768:(jb + 1) * 768])])
                k.copy("act", wb_[:, 0:4, :], w[:, 0:4, :])
                k.copy("dve", wb_[:, 4:8, :], w[:, 4:8, :])
                for jj in range(6):
                    j = jb * 6 + jj
                    for kk in range(8):
                        k.mm(pm[:, j * 2:(j + 1) * 2], wb_[:, kk, jj * 128:(jj + 1) * 128], scb[:, kk, :],
                             start=(kk == 0), stop=(kk == 7))
            pmv = pm.v(pm.ap[:, 0:96].rearrange("p (j v) -> p j v", v=2))
            for v in range(2):
                k.tt("dve", mod[:, :, v], pmv[:, :, v], bada, ALU.add)
        dbg_tap(C, "mod%d" % l, mod, [128, 48, 2])
        A1 = k.sb("A1", [128, 8, 2], F32)
        A2 = k.sb("A2", [128, 8, 2], F32)
        G1c = k.sb("G1c", [128, 8, 2], F32)
        G2c = k.sb("G2c", [128, 8, 2], F32)
        for v in range(2):
            k.ts("dve", A1[:, :, v], mod[:, 8:16, v], 1.0, ALU.add)
            k.tt("dve", A1[:, :, v], A1[:, :, v], gv[:, 0, :], ALU.mult)
            k.ts("dve", A2[:, :, v], mod[:, 32:40, v], 1.0, ALU.add)
            k.tt("dve", A2[:, :, v], A2[:, :, v], gv[:, 2, :], ALU.mult)
            k.tt("dve", G1c[:, :, v], mod[:, 16:24, v], gv[:, 1, :], ALU.mult)
            k.tt("dve", G2c[:, :, v], mod[:, 40:48, v], gv[:, 3, :], ALU.mult)
        C.mod, C.A1, C.A2 = mod, A1, A2
        C.G1c, C.G2c = G1c, G2c
        with Scope(k):
            C.G1b = make_gate_tile(C, G1c, "G1b")
            dbg_tap(C, "G1b%d" % l, C.G1b, [128, 2, D])
            mixer(C, l)
        if "f" not in DEVFLAGS:
            with Scope(k):
                C.G2b = make_gate_tile(C, G2c, "G2b")
                if "D" in DEVFLAGS:
                    ffn_phase(C, l)
                else:
                    ffn_phase_gather(C, l)


def make_gate_tile(C, Gc, name):
    k = C.k
    Gb = k.sb(name, [128, 2, D], F32)
    with Scope(k):
        dg = [k.sb("dg%d" % i, [128, 128], F32) for i in range(2)]
        n = 0
        for v in range(2):
            for half in range(2):
                pb = C.ps[1 + (n % 2)]
                for kk in range(4):
                    kc = half * 4 + kk
                    d = dg[kc % 2]
                    k.ts("dve", d, C.ident_f, Gc[:, kc:kc + 1, v], ALU.mult)
                    k.mm(pb[:, kk * 128:(kk + 1) * 128], C.ones_f, d)
                k.copy("act", Gb[:, v, half * 512:(half + 1) * 512], pb)
                n += 1
    return Gb


def norm_to_hT(C, hT, A, Bc, tag, extra=None):
    k = C.k
    hT_t = k.split(hT, NT, lambda i: (slice(None), slice(None), slice(i * 128, (i + 1) * 128)))
    with Scope(k):
        xin = [k.sb("nx%d" % i, [128, D], F32) for i in range(3)]
        junk = k.sb("njunk", [128, D], BF16)
        xn = [k.sb("nxn%d" % i, [128, D], BF16) for i in range(3)]
        st = [k.sb("nst%d" % i, [128, 2], F32) for i in range(3)]

        def stA(i):
            x = xin[i % 3]
            s = st[i % 3]
            k.dma("sp" if i % 2 == 0 else "pool", [(x, C.xr[i])])
            k.act(junk, x, AF.Square, accum=s[:, 0:1])
            k.act(s[:, 1:2], s[:, 0:1], AF.Sqrt, bias=EPS, scale=1.0 / D)
            k.recip(s[:, 1:2], s[:, 1:2])
            k.ts("dve", xn[i % 3], x, s[:, 1:2], ALU.mult)
            if extra is not None:
                extra(i, x, s[:, 1:2])

        def stB(i):
            v = 1 if i < 2 else 0
            xb = xn[i % 3]
            for hh in range(2):
                pb = C.ps[2 + 2 * (i % 2) + hh]
                pt = pb.v(pb.ap.bitcast(BF16)[:, 0:512].rearrange("p (k t) -> p k t", t=128))
                for kk in range(4):
                    kc = hh * 4 + kk
                    k.tr(pt[:, kk, :], xb[:, kc * 128:(kc + 1) * 128], C.ident_b)
                for kk in range(4):
                    kc = hh * 4 + kk
                    dst = hT_t[i][:, kc, :]
                    if hh == 0:
                        k.act(dst, pt[:, kk, :], AF.Identity, bias=Bc[:, kc:kc + 1, v], scale=A[:, kc:kc + 1, v])
                    else:
                        k.ts("dve", dst, pt[:, kk, :], A[:, kc:kc + 1, v], ALU.mult, Bc[:, kc:kc + 1, v], ALU.add)
        stA(0)
        for i in range(NT):
            if i + 1 < NT:
                stA(i + 1)
            stB(i)


def mixer(C, l):
    k = C.k
    yT = None
    with Scope(k):
        hT = k.sb("hT", [128, 8, T], BF16)
        norm_to_hT(C, hT, C.A1, C.mod[:, 0:8, :], "mix")
        dbg_tap(C, "hT%d" % l, hT, [128, 8, T], BF16)
        if "m" not in DEVFLAGS:
            mla_branch(C, l, hT, C.otd)
        if "n" not in DEVFLAGS:
            na_branch(C, l, hT, C.otd)
        if "s" not in DEVFLAGS:
            s5_branch(C, l, hT, C.otd)
        if "d" not in DEVFLAGS:
            m2_branch(C, l, hT, C.otd)
        if "g" not in DEVFLAGS:
            merge_phase(C, l, hT, yT)
    if "g" not in DEVFLAGS:
        outproj_phase(C, l, yT)


NFM = 19 * 128


def _rot_perm(n):
    p = np.arange(n)
    g, i = p // 16, p % 16
    return g * 16 + np.where(i < 8, i + 8, i - 8)


def host_mixer_inputs(inp, l):
    o = {}
    w = inp["w_in"][l]
    z = np.zeros
    ch = [w[:, 0:128], w[:, 128:256], w[:, 256:384]]
    kr = w[:, 384:416]
    ch.append(np.concatenate([z((D, 64), np.float32), kr, z((D, 32), np.float32)], 1))
    ch.append(np.concatenate([z((D, 64), np.float32), kr[:, _rot_perm(32)], z((D, 32), np.float32)], 1))
    ch += [w[:, 416:544], w[:, 544:672]]
    for h in range(4):
        kh = w[:, 672 + h * 64: 672 + (h + 1) * 64]
        ch.append(np.concatenate([kh, z((D, 64), np.float32)] if h % 2 == 0 else [z((D, 64), np.float32), kh], 1))
    ch += [w[:, 1184:1312], w[:, 1312:1440]]
    ch += [w[:, 1696 + i * 128: 1696 + (i + 1) * 128] for i in range(6)]
    o["wfm"] = np.ascontiguousarray(np.concatenate(ch, 1))
    o["wtm"] = np.ascontiguousarray(np.concatenate([w[:, 928:1184], w[:, 1440:1696], w[:, 2464:2472]], 1))
    wuq = inp["mla_w_uq"][l].reshape(256, 4, 96)
    q = np.zeros((256, 4, 2, 128), np.float32)
    q[:, :, 0, 0:96] = wuq
    q[:, :, 1, 64:96] = wuq[:, :, 64:96][:, :, _rot_perm(32)]
    o["wuq"] = q.reshape(256, 1024)
    wukv = inp["mla_w_ukv"][l].reshape(128, 4, 128)
    kv = np.zeros((128, 768), np.float32)
    for h in range(4):
        kv[:, h * 128: h * 128 + 64] = wukv[:, h, 0:64]
        kv[:, 512 + h * 64: 512 + (h + 1) * 64] = wukv[:, h, 64:128]
    o["wukv"] = kv
    o["gmla"] = np.ascontiguousarray(np.concatenate([col_layout(inp["mla_g_cq"][l]), col_layout(inp["mla_g_ckv"][l])], 1))
    o["nab"] = na_bias_tables(inp["na_rpb"][l])
    return o


def na_variant(m):
    return m if m < 3 else (3 if m <= 13 else m - 10)


def na_kb(m):
    return min(int(np.clip(2 * m - 4, 0, 24)), 22)


def na_bias_tables(rpb):
    out = np.full((6, 4, 5, 128, 128), NEG, np.float32)
    for m in (0, 1, 2, 3, 14, 15):
        var = na_variant(m)
        kb = na_kb(m)
        q = np.arange(128)
        r = 2 * m + q // 64
        qc = q % 64
        rs = np.clip(r - 4, 0, 24)
        cs = np.clip(qc - 8, 0, 48)
        j = np.arange(640)
        krow = kb + j // 64
        kcol = j % 64
        inwin = ((krow[:, None] >= rs[None, :]) & (krow[:, None] < rs[None, :] + 8) &
                 (kcol[:, None] >= cs[None, :]) & (kcol[:, None] < cs[None, :] + 16))
        ro = np.clip(krow[:, None] - r[None, :] + 7, 0, 14)
        co = np.clip(kcol[:, None] - qc[None, :] + 15, 0, 30)
        for h in range(4):
            vals = rpb[h][ro, co]
            out[var, h] = np.where(inwin, vals, NEG).astype(np.float32).reshape(5, 128, 128)
    return np.ascontiguousarray(out.transpose(0, 1, 3, 2, 4))


def rope_tables():
    import ml_dtypes
    cosf = np.ones((128, T), np.float32)
    sinf = np.zeros((128, T), np.float32)
    pos = np.arange(SEQ)
    row, col = pos // GRID_W, pos % GRID_W
    inv = 1.0 / (10000.0 ** (np.arange(0, 16, 2, dtype=np.float32) / 16))
    for i in range(32):
        p = row if i < 16 else col
        ang = p.astype(np.float32) * inv[(i % 16) % 8]
        cosf[64 + i, CTX:] = np.cos(ang)
        sinf[64 + i, CTX:] = np.sin(ang)
    return cosf.astype(ml_dtypes.bfloat16), sinf.astype(ml_dtypes.bfloat16)


TBLK = [(0, 512), (512, 512), (1024, 512), (1536, 512), (2048, 256)]
TBLK_LAT = [(256, 512), (768, 512), (1280, 512), (1792, 512)]


class Stager:
    def __init__(self, C, cols, n=3):
        self.k = C.k

    def load(self, dst, src, kdim=8):
        self.k.dma("pool", [(dst, src.v(src.ap.rearrange("(k p) n -> p k n", p=128)))])


def proj_fm(C, hT, w, t0, n, outp):
    for kk in range(8):
        C.k.mm(outp[:, 0:n], w[:, kk, :], hT[:, kk, t0:t0 + n], start=(kk == 0), stop=(kk == 7))


def rstd_bcast(C, dst, ss_psum, n, dim, tmp):
    k = C.k
    k.act(tmp[:, 0:n], ss_psum[:, 0:n], AF.Sqrt, bias=EPS, scale=1.0 / dim)
    k.recip(dst[:, 0:n], tmp[:, 0:n])


def attn_norm_store(C, po, n, h, dst, rec):
    k = C.k
    if h % 2 == 0:
        k.recip(rec[0:64, 0:n], po[64:128, 0:n])
        k.tt("dve", dst[0:64], po[0:64, 0:n], rec[0:64, 0:n], ALU.mult)
    else:
        k.recip(rec[64:128, 0:n], po[0:64, 0:n])
        k.tt("dve", dst[64:128], po[64:128, 0:n], rec[64:128, 0:n], ALU.mult)


def mla_branch(C, l, hT, otd):
    k = C.k
    Lw = C.L[l]
    scale = 96.0 ** -0.5
    with Scope(k):
        QT = [k.sb("QT%d" % h, [128, T], BF16) for h in range(4)]
        KT = [k.sb("KT%d" % h, [128, T], BF16) for h in range(4)]
        Vaug = k.sb("Vaug", [128, NT, 4, 128], BF16)
        cosf = k.sb("cosf", [128, T], BF16)
        sinf = k.sb("sinf", [128, T], BF16)
        if "A" not in DEVFLAGS:
            k.dma("sp", [(cosf, C.cosf_in), (sinf, C.sinf_in)])
        if "B" not in DEVFLAGS:
            k.memset("pool", Vaug, 1.0)
        with Scope(k):
            wm = k.sb("wm", [128, 8, 640], BF16)
            wuq = k.sb("wuq", [128, 2, 1024], BF16)
            wukv = k.sb("wukv", [128, 1, 768], BF16)
            gm = k.sb("gmla", [128, 3], F32)
            k.dma("sp", [(gm, Lw.gmla)])
            stg = Stager(C, 2048, n=3)
            for j in range(5):
                stg.load(wm[:, :, j * 128:(j + 1) * 128], Lw.wfm[:, j * 128:(j + 1) * 128])
            stg.load(wuq, Lw.wuq, kdim=2)
            stg.load(wukv, Lw.wukv, kdim=1)
            for c in range(2):
                k.ts("dve", wuq[:, c, :], wuq[:, c, :], gm[:, c:c + 1], ALU.mult)
            k.ts("dve", wukv[:, 0, :], wukv[:, 0, :], gm[:, 2:3], ALU.mult)
            v1 = wm.v(wm.ap[:, :, 4 * 128 + 64:4 * 128 + 96].rearrange("p k (g s) -> p k g s", s=16)[:, :, :, 0:8])
            if "C" not in DEVFLAGS:
                k.ts("dve", v1, v1, -1.0, ALU.mult)
            for c in range(2 if "D" not in DEVFLAGS else 0):
                v2 = wuq.v(wuq.ap[:, c, :].rearrange("p (h m x) -> p h m x", h=4, m=2)[:, :, 1, 64:96]
                           .rearrange("p h (g s) -> p h g s", s=16)[:, :, :, 0:8])
                k.ts("dve", v2, v2, -1.0, ALU.mult)
            if DEVSTOP == "mla_w":
                dbg_tap(C, "wm%d" % l, wm, [128, 8, 640], BF16)
                dbg_tap(C, "wuq%d" % l, wuq, [128, 2, 1024], BF16)
                return
            cq = [k.sb("cqb%d" % i, [128, 2, 512], BF16) for i in range(2)]
            sq = [k.sb("sqb%d" % i, [128, 2, 512], BF16) for i in range(2)]
            ckv = [k.sb("ckvb%d" % i, [128, 512], BF16) for i in range(2)]
            skv = [k.sb("skvb%d" % i, [128, 512], BF16) for i in range(2)]
            rq = [k.sb("rq%d" % i, [128, 512], F32) for i in range(2)]
            rkv = [k.sb("rkv%d" % i, [128, 512], F32) for i in range(2)]
            tmp = [k.sb("mt%d" % i, [128, 512], F32) for i in range(4)]
            krr = [k.sb("krr%d" % i, [128, 512], BF16) for i in range(2)]
            rc = [k.sb("rc%d" % i, [128, 2], F32) for i in range(4)]
            P = C.ps
            for bi, (t0, n) in enumerate(TBLK):
                cqb, sqb, ckvb, skvb, rqb, rkvb = cq[bi % 2], sq[bi % 2], ckv[bi % 2], skv[bi % 2], rq[bi % 2], rkv[bi % 2]
                for c in range(2):
                    if 'z' not in DEVFLAGS:
                        proj_fm(C, hT, wm[:, :, c * 128:(c + 1) * 128], t0, n, P[c])
                    if 'x' not in DEVFLAGS:
                        k.copy("dve", cqb[:, c, 0:n], P[c][:, 0:n])
                    if 'y' not in DEVFLAGS:
                        k.act(sqb[:, c, 0:n], P[c][:, 0:n], AF.Square)
                if '1' in DEVFLAGS:
                    continue
                for c in range(2):
                    k.mm(P[2][:, 0:n], C.ones_b, sqb[:, c, 0:n], start=(c == 0), stop=(c == 1))
                rstd_bcast(C, rqb, P[2], n, 256, tmp[0])
                if '2' in DEVFLAGS:
                    continue
                proj_fm(C, hT, wm[:, :, 256:384], t0, n, P[3])
                k.copy("dve", ckvb[:, 0:n], P[3][:, 0:n])
                k.act(skvb[:, 0:n], P[3][:, 0:n], AF.Square)
                k.mm(P[2][:, 0:n], C.ones_b, skvb[:, 0:n])
                rstd_bcast(C, rkvb, P[2], n, 128, tmp[0])
                if '3' in DEVFLAGS:
                    continue
                proj_fm(C, hT, wm[:, :, 384:512], t0, n, P[0])
                proj_fm(C, hT, wm[:, :, 512:640], t0, n, P[1])
                k.tt("dve", tmp[1][:, 0:n], P[0][:, 0:n], cosf[:, t0:t0 + n], ALU.mult)
                k.tt("dve", tmp[2][:, 0:n], P[1][:, 0:n], sinf[:, t0:t0 + n], ALU.mult)
                kb = krr[bi % 2]
                k.tt("pool", kb[:, 0:n], tmp[1][:, 0:n], tmp[2][:, 0:n], ALU.add)
                for h in range(4 if 'H' not in DEVFLAGS else 0):
                    pm, pr = P[4 + (h % 2) * 2], P[5 + (h % 2) * 2]
                    for c in range(2):
                        k.mm(pm[:, 0:n], wuq[:, c, h * 256:h * 256 + 128], cqb[:, c, 0:n], start=(c == 0), stop=(c == 1))
                    for c in range(2):
                        k.mm(pr[:, 0:n], wuq[:, c, h * 256 + 128:h * 256 + 256], cqb[:, c, 0:n], start=(c == 0), stop=(c == 1))
                    ta, tb = tmp[(h % 2) * 2], tmp[(h % 2) * 2 + 1]
                    k.tt("dve", ta[:, 0:n], pm[:, 0:n], cosf[:, t0:t0 + n], ALU.mult)
                    k.tt("dve", tb[:, 0:n], pr[:, 0:n], sinf[:, t0:t0 + n], ALU.mult)
                    k.tt("pool", ta[:, 0:n], ta[:, 0:n], tb[:, 0:n], ALU.add)
                    k.tt("pool", QT[h][:, t0:t0 + n], ta[:, 0:n], rqb[:, 0:n], ALU.mult)
                for h in range(4 if 'G' not in DEVFLAGS else 0):
                    pk = P[h % 2]
                    k.mm(pk[:, 0:n], wukv[:, 0, h * 128:(h + 1) * 128], ckvb[:, 0:n])
                    k.tt("dve", KT[h][0:64, t0:t0 + n], pk[0:64, 0:n], rkvb[0:64, 0:n], ALU.mult)
                    k.copy("pool", KT[h][64:128, t0:t0 + n], kb[64:128, 0:n])
                for j in range(n // 128 if 'F' not in DEVFLAGS else 0):
                    ti = t0 // 128 + j
                    pv, pss = P[2 + (j % 2)], P[4 + (j % 2)]
                    k.mm(pv[:, 0:256], ckvb[:, j * 128:(j + 1) * 128], wukv[:, 0, 512:768])
                    k.mm(pss[:, 0:1], skvb[:, j * 128:(j + 1) * 128], C.ones_b[:, 0:1])
                    r = rc[ti % 4]
                    k.act(r[:, 0:1], pss[:, 0:1], AF.Sqrt, bias=EPS, scale=1.0 / 128)
                    k.recip(r[:, 1:2], r[:, 0:1])
                    pvv = pv.v(pv.ap[:, 0:256].rearrange("p (h d) -> p h d", d=64))
                    k.ts("dve", Vaug[:, ti, 0:4:2, 0:64], pvv[:, 0:4:2, :], r[:, 1:2], ALU.mult)
                    k.ts("dve", Vaug[:, ti, 1:4:2, 64:128], pvv[:, 1:4:2, :], r[:, 1:2], ALU.mult)
        dbg_tap(C, "QT0_%d" % l, QT[0], [128, T], BF16)
        dbg_tap(C, "KT0_%d" % l, KT[0], [128, T], BF16)
        dbg_tap(C, "Vaug%d" % l, Vaug, [128, NT, 4, 128], BF16)
        if DEVSTOP == "mla_proj":
            return
        with Scope(k):
            OT = k.sb("OTmla", [128, 2, T], BF16)
            PT = [k.sb("PT%d" % i, [128, 512], BF16) for i in range(3)]
            rec = [k.sb("rec%d" % i, [128, 512], F32) for i in range(2)]
            P = C.ps
            qblocks = [(256 + 512 * i, 512, list(range(NT))) for i in range(4)] + [(0, 256, [0, 1])]
            steps = []
            nb = 0
            for h in range(4):
                for (q0, n, kts) in qblocks:
                    for ki, kt in enumerate(kts):
                        steps.append((h, q0, n, kt, ki == 0, ki == len(kts) - 1, nb))
                    nb += 1
            NS = 4
            PT = PT + [k.sb("PT3", [128, 512], BF16)]

            def qk(i):
                h, q0, n, kt, first, last, nbi = steps[i]
                k.mm(P[i % NS][:, 0:n], KT[h][:, kt * 128:(kt + 1) * 128], QT[h][:, q0:q0 + n])
                k.act(PT[i % NS][:, 0:n], P[i % NS][:, 0:n], AF.Exp, scale=scale)

            def pv(i):
                h, q0, n, kt, first, last, nbi = steps[i]
                po = P[4 + (nbi % 2)]
                k.mm(po[:, 0:n], Vaug[:, kt, h, :], PT[i % NS][:, 0:n], start=first, stop=last)
                if last:
                    attn_norm_store(C, po, n, h, OT[:, h // 2, q0:q0 + n], rec[nbi % 2])
            LOOK = 2
            for i in range(min(LOOK, len(steps))):
                qk(i)
            for i in range(len(steps)):
                if i + LOOK < len(steps):
                    qk(i + LOOK)
                pv(i)
            dbg_tap(C, "OTmla%d" % l, OT, [128, 2, T], BF16)
            k.dma("sp", [(otd[0:2].v(otd.ap[0:2].rearrange("c p t -> p c t")), OT)])


def na_branch(C, l, hT, otd):
    k = C.k
    Lw = C.L[l]
    scale = 0.125
    P = C.ps
    with Scope(k):
        QnT = k.sb("QnT", [128, 2, T], BF16)
        KmT = [k.sb("KmT%d" % h, [128, T], BF16) for h in range(4)]
        Vaug = k.sb("VaugN", [128, NT, 4, 128], BF16)
        k.memset("pool", Vaug, 1.0)
        with Scope(k):
            wq = k.sb("wq", [128, 8, 256], BF16)
            wk = k.sb("wk", [128, 8, 512], BF16)
            wv = k.sb("wv", [128, 8, 256], BF16)
            stg = Stager(C, 2048, n=3)
            for j in range(2):
                stg.load(wq[:, :, j * 128:(j + 1) * 128], Lw.wfm[:, (5 + j) * 128:(6 + j) * 128])
            for j in range(4):
                stg.load(wk[:, :, j * 128:(j + 1) * 128], Lw.wfm[:, (7 + j) * 128:(8 + j) * 128])
            stg.load(wv, Lw.wtm[:, 0:256])
            for bi, (t0, n) in enumerate(TBLK):
                for c in range(2):
                    proj_fm(C, hT, wq[:, :, c * 128:(c + 1) * 128], t0, n, P[c])
                    k.copy("dve" if c == 0 else "act", QnT[:, c, t0:t0 + n], P[c][:, 0:n])
                for h in range(4):
                    proj_fm(C, hT, wk[:, :, h * 128:(h + 1) * 128], t0, n, P[2 + h])
                    k.copy("dve" if h % 2 == 0 else "act", KmT[h][:, t0:t0 + n], P[2 + h][:, 0:n])
                for j in range(n // 128):
                    ti = t0 // 128 + j
                    pv = P[6 + (j % 2)]
                    for kk in range(8):
                        k.mm(pv[:, 0:256], hT[:, kk, ti * 128:(ti + 1) * 128], wv[:, kk, :], start=(kk == 0), stop=(kk == 7))
                    pvv = pv.v(pv.ap[:, 0:256].rearrange("p (h d) -> p h d", d=64))
                    k.copy("dve", Vaug[:, ti, 0:4:2, 0:64], pvv[:, 0:4:2, :])
                    k.copy("dve", Vaug[:, ti, 1:4:2, 64:128], pvv[:, 1:4:2, :])
        with Scope(k):
            OT = k.sb("OTna", [128, 2, T], BF16)
            nb = [k.sb("nab%d" % i, [128, 5, 128], F32) for i in range(2)]
            nbb = [k.sb("nabb%d" % i, [128, 5, 128], BF16) for i in range(2)]
            PT = [k.sb("nPT%d" % i, [128, 896], BF16) for i in range(2)]
            rec = [k.sb("nrec%d" % i, [128, 256], F32) for i in range(2)]
            items = []
            for h in range(4):
                for m in range(16):
                    items.append((h, m))
                items.append((h, -1))
            state = {"var": None, "nload": 0, "bt": None}

            def stageA(i):
                h, m = items[i]
                s = i % 2
                pa, pb = P[3 * s], P[3 * s + 1]
                pt = PT[s]
                if m < 0:
                    q = QnT[:, h // 2, 0:CTX]
                    for j in range(2):
                        k.mm(pa[:, j * 256:(j + 1) * 256], KmT[h][:, j * 128:(j + 1) * 128], q)
                    k.act(pt[:, 0:512], pa, AF.Exp, scale=scale)
                    return
                var = na_variant(m)
                if (h, var) != state["var"]:
                    bt32 = nb[state["nload"] % 2]
                    state["bt"] = nbb[state["nload"] % 2]
                    state["nload"] += 1
                    k.dma("sp", [(bt32, Lw.nab[var, h])])
                    k.act(state["bt"], bt32, AF.Copy, scale=1.0 / scale)
                    state["var"] = (h, var)
                bt = state["bt"]
                q0 = CTX + m * 128
                kt0 = 2 + na_kb(m) // 2
                q = QnT[:, h // 2, q0:q0 + 128]
                for j in range(4):
                    k.mm(pa[:, j * 128:(j + 1) * 128], KmT[h][:, (kt0 + j) * 128:(kt0 + j + 1) * 128], q, start=True, stop=False)
                    k.mm(pa[:, j * 128:(j + 1) * 128], C.ident_b, bt[:, j, :], start=False, stop=True)
                k.mm(pb[:, 0:128], KmT[h][:, (kt0 + 4) * 128:(kt0 + 5) * 128], q, start=True, stop=False)
                k.mm(pb[:, 0:128], C.ident_b, bt[:, 4, :], start=False, stop=True)
                for j in range(2):
                    k.mm(pb[:, 128 + j * 128:256 + j * 128], KmT[h][:, j * 128:(j + 1) * 128], q)
                k.act(pt[:, 0:512], pa, AF.Exp, scale=scale)
                k.act(pt[:, 512:896], pb[:, 0:384], AF.Exp, scale=scale)

            def stageB(i):
                h, m = items[i]
                s = i % 2
                po = P[3 * s + 2]
                pt = PT[s]
                if m < 0:
                    for j in range(2):
                        k.mm(po[:, 0:256], Vaug[:, j, h, :], pt[:, j * 256:(j + 1) * 256], start=(j == 0), stop=(j == 1))
                    attn_norm_store(C, po, 256, h, OT[:, h // 2, 0:CTX], rec[s])
                    return
                q0 = CTX + m * 128
                kt0 = 2 + na_kb(m) // 2
                kts = [kt0 + j for j in range(5)] + [0, 1]
                for j, kt in enumerate(kts):
                    k.mm(po[:, 0:128], Vaug[:, kt, h, :], pt[:, j * 128:(j + 1) * 128], start=(j == 0), stop=(j == 6))
                attn_norm_store(C, po, 128, h, OT[:, h // 2, q0:q0 + 128], rec[s])
            stageA(0)
            for i in range(len(items)):
                if i + 1 < len(items):
                    stageA(i + 1)
                stageB(i)
            dbg_tap(C, "OTna%d" % l, OT, [128, 2, T], BF16)
            k.dma("sp", [(otd[2:4].v(otd.ap[2:4].rearrange("c p t -> p c t")), OT)])


def host_s5_inputs(inp, l):
    o = {}
    ls = np.repeat(inp["s5_log_step"][l][:, :, None], 64, axis=2)
    o["s5par"] = np.ascontiguousarray(np.stack([inp["s5_a_re"][l].reshape(-1), inp["s5_a_im"][l].reshape(-1),
                                                ls.reshape(-1)], 0).astype(np.float32))
    BT = np.zeros((128, 2, 8, 2, 128), np.float32)
    CT = np.zeros((128, 2, 8, 2, 128), np.float32)
    for d in range(2):
        for g in range(16):
            s, gj = g // 2, g % 2
            r0 = (s % 4) * 32 + gj * 16
            for x, (bn, cn) in enumerate((("s5_b_re", "s5_c_re"), ("s5_b_im", "s5_c_im"))):
                BT[r0:r0 + 16, d, s, x, gj * 64:(gj + 1) * 64] = inp[bn][l][d, g].T
                CT[gj * 64:(gj + 1) * 64, d, s, x, r0:r0 + 16] = inp[cn][l][d, g].T
    o["s5BT"] = BT
    o["s5CT"] = CT
    o["s5vec"] = np.ascontiguousarray(np.stack([col_layout(inp["s5_d"][l]), col_layout(inp["s5_b_glu"][l])], -1))
    o["s5wglu"] = np.ascontiguousarray(inp["s5_w_glu"][l])
    o["s5wu"] = np.ascontiguousarray(inp["w_in"][l][:, 1184:1440])
    return o


S5_ORDER = [list(range(NT)), [1, 0] + list(range(NT - 1, 1, -1))]


def bc(tv, shape, axis):
    return tv.v(tv.ap.unsqueeze(axis).to_broadcast(list(shape)))


def s5_branch(C, l, hT, otd):
    k = C.k
    Lw = C.L[l]
    P = C.ps
    HALF_PI = float(np.pi / 2)
    with Scope(k):
        BbT = k.sb("s5BbT", [128, 2, 8, 2, 128], BF16)
        CTb = k.sb("s5CTb", [128, 2, 8, 2, 128], BF16)
        cos16 = k.sb("s5cos16", [128, 16, 128], BF16)
        sin16 = k.sb("s5sin16", [128, 16, 128], BF16)
        rhob = k.sb("s5rhob", [128, 16, 128], F32)
        eL = k.sb("s5eL", [128, 16, 2], F32)
        vec = k.sb("s5vec", [128, 2, 2], F32)
        wglu = k.sb("s5wglu", [128, 2, 256], BF16)
        wu = k.sb("s5wu", [128, 8, 256], BF16)
        Jm = k.sb("s5J", [128, 128], BF16)
        k.dma("sp", [(vec, Lw.s5vec)])
        with Scope(k):
            stg = Stager(C, 2048, n=2)
            stg.load(wu, Lw.s5wu)
            stg.load(wglu, Lw.s5wglu, kdim=2)
            jf = k.sb("jf", [128, 128], F32)
            k.dma("sp", [(jf, C.jmat_in)])
            k.copy("dve", Jm, jf)
        with Scope(k):
            cosT = k.sb("s5cosT", [128, 16, 128], F32)
            sinT = k.sb("s5sinT", [128, 16, 128], F32)
            par = k.sb("s5par", [128, 3, 2048], F32)
            k.dma("sp", [(par, Lw.s5par.v(Lw.s5par.ap.partition_broadcast(128)))])
            are, aim, ls = par[:, 0, :], par[:, 1, :], par[:, 2, :]
            W = [k.sb("s5w%d" % i, [128, 2048], F32) for i in range(7)]
            step, rho, cth, sth, t1, t2, t3 = W
            k.act(step, ls, AF.Exp)
            k.tt("dve", t1, step, are, ALU.mult)
            k.act(rho, t1, AF.Exp)
            k.tt("dve", t1, step, aim, ALU.mult)
            k.act(sth, t1, AF.Sin, scale=1.0 / 16)
            k.act(cth, t1, AF.Sin, scale=1.0 / 16, bias=HALF_PI)
            for _ in range(4):
                k.act(t1, cth, AF.Square)
                k.act(t2, sth, AF.Square)
                k.tt("dve", t3, cth, sth, ALU.mult)
                k.tt("dve", cth, t1, t2, ALU.subtract)
                k.act(sth, t3, AF.Copy, scale=2.0)
            diag = k.sb("s5diag", [128, 16, 3], F32)
            big = k.sb("s5big", [128, 16, 128], F32)
            for i, src in enumerate((rho, cth, sth)):
                sv = src.v(src.ap.rearrange("p (a m) -> p a m", m=128))
                k.tt("dve", big, sv, bc(C.ident_f, [128, 16, 128], 1), ALU.mult)
                k.ins("dve", lambda e, o=diag[:, :, i], b=big: e.tensor_reduce(out=o.ap, in_=b.ap, axis=AX.X, op=ALU.add),
                      [big], [diag])
            nr, ni = t1, t2
            k.tt("dve", nr, rho, cth, ALU.mult)
            k.ts("dve", nr, nr, -1.0, ALU.add)
            k.tt("pool", ni, rho, sth, ALU.mult)
            den = t3
            k.act(den, are, AF.Square)
            k.act(step, aim, AF.Square)
            k.tt("dve", den, den, step, ALU.add)
            k.recip(den, den)
            cr, ci = cth, sth
            k.tt("dve", cr, nr, are, ALU.mult)
            k.tt("pool", step, ni, aim, ALU.mult)
            k.tt("dve", cr, cr, step, ALU.add)
            k.tt("dve", cr, cr, den, ALU.mult)
            k.tt("dve", ci, ni, are, ALU.mult)
            k.tt("pool", step, nr, aim, ALU.mult)
            k.tt("dve", ci, ci, step, ALU.subtract)
            k.tt("dve", ci, ci, den, ALU.mult)
            btf = k.sb("s5btf", [128, 2, 8, 2, 128], F32)
            k.dma("sp", [(btf, Lw.s5BT)])
            crv = cr.v(cr.ap.rearrange("p (d s m) -> p d s m", d=2, s=8))
            civ = ci.v(ci.ap.rearrange("p (d s m) -> p d s m", d=2, s=8))
            tb1 = rho.v(rho.ap.rearrange("p (d s m) -> p d s m", d=2, s=8))
            tb2 = step.v(step.ap.rearrange("p (d s m) -> p d s m", d=2, s=8))
            k.tt("dve", tb1, crv, btf[:, :, :, 0, :], ALU.mult)
            k.tt("pool", tb2, civ, btf[:, :, :, 1, :], ALU.mult)
            k.tt("dve", BbT[:, :, :, 0, :], tb1, tb2, ALU.subtract)
            k.tt("dve", tb1, crv, btf[:, :, :, 1, :], ALU.mult)
            k.tt("pool", tb2, civ, btf[:, :, :, 0, :], ALU.mult)
            k.tt("dve", BbT[:, :, :, 1, :], tb1, tb2, ALU.add)
            k.dma("sp", [(btf, Lw.s5CT)])
            k.copy("dve", CTb[:, :, :, 0, :], btf[:, :, :, 0, :])
            k.ts("dve", CTb[:, :, :, 1, :], btf[:, :, :, 1, :], -1.0, ALU.mult)
            ck = k.sb("s5ck", [128, 16], F32)
            sk = k.sb("s5sk", [128, 16], F32)
            ta = k.sb("s5ta", [128, 16], F32)
            tb_ = k.sb("s5tb", [128, 16], F32)
            k.copy("dve", ck, diag[:, :, 1])
            k.copy("dve", sk, diag[:, :, 2])
            k.memset("dve", cosT[:, :, 0:1], 1.0)
            k.memset("dve", sinT[:, :, 0:1], 0.0)
            big2 = big[:, :, 0:64]
            big3 = big[:, :, 64:128]
            for kk in range(8):
                w = 1 << kk
                if kk < 7:
                    ckb = bc(ck, [128, 16, w], 2)
                    skb = bc(sk, [128, 16, w], 2)
                    k.tt("dve", big2[:, :, 0:w], cosT[:, :, 0:w], ckb, ALU.mult)
                    k.tt("dve", big3[:, :, 0:w], sinT[:, :, 0:w], skb, ALU.mult)
                    k.tt("dve", cosT[:, :, w:2 * w], big2[:, :, 0:w], big3[:, :, 0:w], ALU.subtract)
                    k.tt("dve", big2[:, :, 0:w], cosT[:, :, 0:w], skb, ALU.mult)
                    k.tt("dve", big3[:, :, 0:w], sinT[:, :, 0:w], ckb, ALU.mult)
                    k.tt("dve", sinT[:, :, w:2 * w], big2[:, :, 0:w], big3[:, :, 0:w], ALU.add)
                    k.tt("dve", ta, ck, ck, ALU.mult)
                    k.tt("dve", tb_, sk, sk, ALU.mult)
                    k.tt("dve", sk, ck, sk, ALU.mult)
                    k.ts("dve", sk, sk, 2.0, ALU.mult)
                    k.tt("dve", ck, ta, tb_, ALU.subtract)
                else:
                    k.copy("dve", eL[:, :, 0], ck)
                    k.copy("dve", eL[:, :, 1], sk)
            k.copy("dve", rhob, bc(diag[:, :, 0], [128, 16, 128], 2))
            k.copy("act", cos16, cosT)
            k.copy("act", sin16, sinT)
        dbg_tap(C, "s5rhob%d" % l, rhob, [128, 16, 128])
        dbg_tap(C, "s5BbT%d" % l, BbT, [128, 2, 8, 2, 128], BF16)
        if DEVSTOP == "s5_par":
            return
        uproc = [k.sb("s5up%d" % d, [128, 2, T], BF16) for d in range(2)]
        unat = k.sb("s5un", [128, 2, T], F32)
        pos = [{c: i for i, c in enumerate(S5_ORDER[d])} for d in range(2)]
        with Scope(k):
            ut = [k.sb("s5ut%d" % i, [128, 256], BF16) for i in range(2)]
            for c in range(NT):
                pu = P[c % 2]
                for kk in range(8):
                    k.mm(pu[:, 0:256], hT[:, kk, c * 128:(c + 1) * 128], wu[:, kk, :], start=(kk == 0), stop=(kk == 7))
                u = ut[c % 2]
                k.copy("act", u, pu[:, 0:256])
                pf = P[2 + (c % 2) * 2]
                pr = P[3 + (c % 2) * 2]
                for q in range(2):
                    k.mm(pf[:, q * 128:(q + 1) * 128], u[:, q * 128:(q + 1) * 128], C.ident_b)
                    k.mm(pr[:, q * 128:(q + 1) * 128], u[:, q * 128:(q + 1) * 128], Jm)
                pfv = pf.v(pf.ap[:, 0:256].rearrange("p (q t) -> p q t", q=2))
                prv = pr.v(pr.ap[:, 0:256].rearrange("p (q t) -> p q t", q=2))
                k.copy("dve", uproc[0][:, :, c * 128:(c + 1) * 128], pfv)
                k.copy("act", unat[:, :, c * 128:(c + 1) * 128], pfv)
                i1 = pos[1][c]
                k.copy("dve", uproc[1][:, :, i1 * 128:(i1 + 1) * 128], prv)
        dbg_tap(C, "s5up1_%d" % l, uproc[1], [128, 2, T], BF16)
        yf = k.sb("s5yf", [128, 2, T], F32)
        with Scope(k):
            BQ = k.sb("s5BQ", [128, 8, 2, 512], F32)
            bq = [[None, None] for _ in range(8)]
            subs = k.split(BQ, 16, lambda i: (slice(None), i // 2, i % 2, slice(None)))
            for i in range(16):
                bq[i // 2][i % 2] = subs[i]
            G16 = k.sb("s5G16", [128, 8, 2, 512], BF16)
            g16 = [[None, None] for _ in range(8)]
            subs16 = k.split(G16, 16, lambda i: (slice(None), i // 2, i % 2, slice(None)))
            for i in range(16):
                g16[i // 2][i % 2] = subs16[i]
            tm = [k.sb("s5tm%d" % i, [128, 512], BF16) for i in range(8)]
            p16 = [k.sb("s5p16%d" % i, [128, 512], BF16) for i in range(4)]

            hre = [k.sb("s5hre%d" % i, [128, 512], BF16) for i in range(2)]
            him = [k.sb("s5him%d" % i, [128, 512], BF16) for i in range(2)]
            ini = [k.sb("s5ini%d" % i, [128, 8, 2], F32) for i in range(2)]
            tp_ = [k.sb("s5tp%d" % i, [128, 8], F32) for i in range(4)]
            ytr = [k.sb("s5ytr%d" % i, [128, 256], BF16) for i in range(2)]
            it = 0
            nchunk = 0
            for d in range(2):
                k.memset("pool", ini[nchunk % 2], 0.0)
                cL = eL[:, d * 8:(d + 1) * 8, 0]
                sL = eL[:, d * 8:(d + 1) * 8, 1]
                for bi, (t0, n) in enumerate(TBLK):
                    nch = n // 128

                    def v3(tv):
                        return tv.v(tv.ap[:, 0:n].rearrange("p (c j) -> p c j", j=128))
                    for s in range(8):
                        q = s // 4
                        sd = d * 8 + s
                        pre, pim = P[2 * (s % 2)], P[2 * (s % 2) + 1]
                        k.mm(pre[:, 0:n], BbT[:, d, s, 0, :], uproc[d][:, q, t0:t0 + n])
                        k.mm(pim[:, 0:n], BbT[:, d, s, 1, :], uproc[d][:, q, t0:t0 + n])
                        cb = bc(cos16[:, sd, :], [128, nch, 128], 1)
                        sb_ = bc(sin16[:, sd, :], [128, nch, 128], 1)
                        t = tm[(s % 2) * 4:(s % 2) * 4 + 4]
                        r16, i16 = p16[(s % 2) * 2], p16[(s % 2) * 2 + 1]
                        k.copy("act", r16[:, 0:n], pre[:, 0:n])
                        k.copy("act", i16[:, 0:n], pim[:, 0:n])
                        k.tt("dve", v3(t[0]), v3(r16), cb, ALU.mult)
                        k.tt("dve", v3(t[1]), v3(r16), sb_, ALU.mult)
                        k.tt("dve", v3(t[2]), v3(i16), sb_, ALU.mult)
                        k.tt("dve", v3(t[3]), v3(i16), cb, ALU.mult)
                        k.tt("pool", bq[s][0][:, 0:n], t[0][:, 0:n], t[2][:, 0:n], ALU.add)
                        k.tt("pool", bq[s][1][:, 0:n], t[3][:, 0:n], t[1][:, 0:n], ALU.subtract)
                    for j in range(nch):
                        cur, nxt = ini[nchunk % 2], ini[(nchunk + 1) % 2]
                        nchunk += 1
                        sl = slice(j * 128, (j + 1) * 128)
                        for s in range(8):
                            sd = d * 8 + s
                            for x in range(2):
                                k.scan(g16[s][x][:, sl], rhob[:, sd, :], bq[s][x][:, sl], cur[:, s, x:x + 1], ALU.mult, ALU.add)
                        last = j * 128 + 127
                        cr_ = G16[:, :, 0, last]
                        ci_ = G16[:, :, 1, last]
                        k.tt("pool", tp_[0], cr_, cL, ALU.mult)
                        k.tt("pool", tp_[1], ci_, sL, ALU.mult)
                        k.tt("pool", nxt[:, :, 0], tp_[0], tp_[1], ALU.subtract)
                        k.tt("pool", tp_[2], cr_, sL, ALU.mult)
                        k.tt("pool", tp_[3], ci_, cL, ALU.mult)
                        k.tt("pool", nxt[:, :, 1], tp_[2], tp_[3], ALU.add)
                    if d == 0:
                        py = [P[4], P[5]]
                    else:
                        py = [P[4 + j] for j in range(nch)]
                    for s in range(8):
                        q = s // 4
                        sd = d * 8 + s
                        b = it % 2
                        it += 1
                        cb = bc(cos16[:, sd, :], [128, nch, 128], 1)
                        sb_ = bc(sin16[:, sd, :], [128, nch, 128], 1)
                        t = tm[(s % 2) * 4:(s % 2) * 4 + 4]
                        k.tt("dve", v3(t[0]), v3(g16[s][0]), cb, ALU.mult)
                        k.tt("pool", v3(t[1]), v3(g16[s][1]), sb_, ALU.mult)
                        k.tt("dve", v3(t[2]), v3(g16[s][0]), sb_, ALU.mult)
                        k.tt("pool", v3(t[3]), v3(g16[s][1]), cb, ALU.mult)
                        k.tt("dve", hre[b][:, 0:n], t[0][:, 0:n], t[1][:, 0:n], ALU.subtract)
                        k.tt("dve", him[b][:, 0:n], t[2][:, 0:n], t[3][:, 0:n], ALU.add)
                        first, lastq = (s % 4 == 0), (s % 4 == 3)
                        if d == 0:
                            k.mm(py[q][:, 0:n], CTb[:, d, s, 0, :], hre[b][:, 0:n], start=first, stop=False)
                            k.mm(py[q][:, 0:n], CTb[:, d, s, 1, :], him[b][:, 0:n], start=False, stop=lastq)
                        else:
                            for j in range(nch):
                                sl = slice(j * 128, (j + 1) * 128)
                                k.mm(py[j][:, q * 128:(q + 1) * 128], hre[b][:, sl], CTb[:, d, s, 0, :], start=first, stop=False)
                                k.mm(py[j][:, q * 128:(q + 1) * 128], him[b][:, sl], CTb[:, d, s, 1, :], start=False, stop=lastq)
                        if lastq:
                            if d == 0:
                                k.copy("act", yf[:, q, t0:t0 + n], py[q][:, 0:n])
                            elif q == 1:
                                for j in range(nch):
                                    c = S5_ORDER[1][t0 // 128 + j]
                                    yt = ytr[j % 2]
                                    k.copy("act", yt, py[j][:, 0:256])
                                    pz = P[2 * (j % 2)]
                                    for qq in range(2):
                                        k.mm(pz[:, qq * 128:(qq + 1) * 128], yt[:, qq * 128:(qq + 1) * 128], Jm)
                                    pzv = pz.v(pz.ap[:, 0:256].rearrange("p (q t) -> p q t", q=2))
                                    ysl = yf[:, :, c * 128:(c + 1) * 128]
                                    k.tt("dve", ysl, ysl, pzv, ALU.add)
        for q in range(2):
            k.stt(unat[:, q, :], unat[:, q, :], vec[:, q, 0:1], yf[:, q, :], ALU.mult, ALU.add)
        dbg_tap(C, "s5y%d" % l, unat, [128, 2, T])
        with Scope(k):
            OT = k.sb("OTs5", [128, 2, T], BF16)
            zb = [k.sb("s5z%d" % i, [128, 2, 512], BF16) for i in range(2)]
            g1 = [k.sb("s5g%d" % i, [128, 512], F32) for i in range(4)]
            for bi, (t0, n) in enumerate(TBLK):
                z = zb[bi % 2]
                for q in range(2):
                    y = unat[:, q, t0:t0 + n]
                    a, b_ = g1[q * 2], g1[q * 2 + 1]
                    k.tt("pool", a[:, 0:n], y, y, ALU.mult)
                    k.ts("dve", a[:, 0:n], a[:, 0:n], 0.044715, ALU.mult, 1.0, ALU.add)
                    k.tt("dve", a[:, 0:n], a[:, 0:n], y, ALU.mult)
                    k.act(b_[:, 0:n], a[:, 0:n], AF.Sigmoid, scale=1.5957691216057308)
                    k.tt("dve", z[:, q, 0:n], y, b_[:, 0:n], ALU.mult)
                for q in range(2):
                    pg = P[q]
                    for c in range(2):
                        k.mm(pg[:, 0:n], wglu[:, c, q * 128:(q + 1) * 128], z[:, c, 0:n], start=(c == 0), stop=(c == 1))
                    sg = g1[q * 2]
                    k.act(sg[:, 0:n], pg[:, 0:n], AF.Sigmoid, bias=vec[:, q, 1:2])
                    k.tt("dve", OT[:, q, t0:t0 + n], z[:, q, 0:n], sg[:, 0:n], ALU.mult)
            dbg_tap(C, "OTs5%d" % l, OT, [128, 2, T], BF16)
            k.dma("sp", [(otd[4:6].v(otd.ap[4:6].rearrange("c p t -> p c t")), OT)])


def host_m2_inputs(inp, l):
    o = {}
    cw = inp["m2_conv_w"][l]
    v = np.zeros((128, 6, 5), np.float32)
    for kk in range(4):
        v[:, :, kk] = col_layout(cw[kk])
    v[:, :, 4] = col_layout(inp["m2_conv_b"][l])
    o["m2vec"] = v
    o["m2row"] = np.ascontiguousarray(np.concatenate([inp["m2_a_log"][l].reshape(-1), inp["m2_dt_bias"][l].reshape(-1),
                                                      inp["m2_d"][l].reshape(-1), inp["m2_g_norm"][l].reshape(-1)])[None, :].astype(np.float32))
    return o


def host_m2_masks():
    i = np.arange(128)
    le = (i[:, None] <= i[None, :]).astype(np.float32)
    ge = (i[:, None] >= i[None, :]).astype(np.float32)
    su = (i[:, None] > i[None, :]).astype(np.float32)
    sl = (i[:, None] < i[None, :]).astype(np.float32)
    return np.ascontiguousarray(np.stack([le, ge, su, sl], 0).transpose(1, 0, 2))


def m2_branch(C, l, hT, otd):
    k = C.k
    Lw = C.L[l]
    P = C.ps
    with Scope(k):
        xc = k.sb("m2xc", [128, 6, T], BF16)
        xtok = k.sb("m2xtok", [128, NT, 512], BF16)
        zs = k.sb("m2zs", [128, NT, 256], BF16)
        dt = k.sb("m2dt", [128, NT, 8], F32)
        dA = k.sb("m2dA", [128, NT, 8], F32)
        ysum = k.sb("m2ys", [128, NT, 256], F32)
        row = k.sb("m2row", [128, 276], F32)
        msk = k.sb("m2msk", [128, 4, 128], F32)
        mskb = k.sb("m2mskb", [128, 2, 128], BF16)
        k.dma("sp", [(row, Lw.m2row.v(Lw.m2row.ap.partition_broadcast(128))), (msk, C.m2msk_in)])
        k.copy("dve", mskb, msk[:, 0:2, :])
        LE, GE, SU, SL = msk[:, 0, :], msk[:, 1, :], msk[:, 2, :], msk[:, 3, :]
        aneg = k.sb("m2aneg", [128, 8], F32)
        k.act(aneg, row[:, 0:8], AF.Exp)
        k.ts("dve", aneg, aneg, -1.0, ALU.mult)
        with Scope(k):
            wx = k.sb("m2wx", [128, 8, 768], BF16)
            wz = k.sb("m2wz", [128, 8, 264], BF16)
            cv = k.sb("m2cv", [128, 6, 5], F32)
            k.dma("sp", [(cv, Lw.m2vec)])
            with Scope(k):
                stg = Stager(C, 2112, n=2)
                for j in range(6):
                    stg.load(wx[:, :, j * 128:(j + 1) * 128], Lw.wfm[:, (13 + j) * 128:(14 + j) * 128])
                stg.load(wz, Lw.wtm[:, 256:520])
            xpre = k.sb("m2xpre", [128, 6, T], BF16)
            for bi, (t0, n) in enumerate(TBLK):
                for c6 in range(6):
                    pp = P[c6 % 4]
                    proj_fm(C, hT, wx[:, :, c6 * 128:(c6 + 1) * 128], t0, n, pp)
                    k.copy("dve" if c6 % 2 == 0 else "act", xpre[:, c6, t0:t0 + n], pp[:, 0:n])
            acc = [k.sb("m2acc%d" % i, [128, T], F32) for i in range(2)]
            for c6 in range(6):
                a = acc[c6 % 2]
                en = "dve"
                k.ts(en, a, xpre[:, c6, :], cv[:, c6, 2:3], ALU.mult, cv[:, c6, 4:5], ALU.add)
                for (lo, hi) in ((0, CTX), (CTX, T)):
                    k.stt(a[:, lo + 2:hi], xpre[:, c6, lo:hi - 2], cv[:, c6, 0:1], a[:, lo + 2:hi], ALU.mult, ALU.add)
                    k.stt(a[:, lo + 1:hi], xpre[:, c6, lo:hi - 1], cv[:, c6, 1:2], a[:, lo + 1:hi], ALU.mult, ALU.add)
                    k.stt(a[:, lo:hi - 1], xpre[:, c6, lo + 1:hi], cv[:, c6, 3:4], a[:, lo:hi - 1], ALU.mult, ALU.add)
                k.act(xc[:, c6, :], a, AF.Silu)
            tmpd = [k.sb("m2tmpd%d" % i, [128, 8], F32) for i in range(2)]
            for ti in range(NT):
                pz = P[4 + (ti % 2)]
                for kk in range(8):
                    k.mm(pz[:, 0:264], hT[:, kk, ti * 128:(ti + 1) * 128], wz[:, kk, :], start=(kk == 0), stop=(kk == 7))
                k.act(zs[:, ti, :], pz[:, 0:256], AF.Silu)
                td = tmpd[ti % 2]
                k.tt("dve", td, pz[:, 256:264], row[:, 8:16], ALU.add)
                k.act(td, td, AF.Exp)
                k.act(dt[:, ti, :], td, AF.Ln, bias=1.0)
            k.tt("dve", dA, dt, bc(aneg, [128, NT, 8], 1), ALU.mult)
            for ti in range(NT):
                pt = P[6 + (ti % 2)]
                ptv = pt.v(pt.ap.bitcast(BF16)[:, 0:512].rearrange("p (c t) -> p c t", t=128))
                for c4 in range(4):
                    k.tr(ptv[:, c4, :], xc[:, c4, ti * 128:(ti + 1) * 128], C.ident_b)
                k.copy("dve" if ti % 2 == 0 else "act", xtok[:, ti, :], pt.v(pt.ap.bitcast(BF16)[:, 0:512]))
        dbg_tap(C, "m2xc%d" % l, xc, [128, 6, T], BF16)
        dbg_tap(C, "m2dt%d" % l, dt, [128, NT, 8])
        dsk = row[:, 16:20]
        k.tt("dve", ysum.v(ysum.ap.rearrange("p t (h d) -> p t h d", h=4)),
             xtok.v(xtok.ap[:, :, 0:256].rearrange("p t (h d) -> p t h d", h=4)),
             row.v(row.ap[:, 16:20].unsqueeze(1).unsqueeze(3).to_broadcast([128, NT, 4, 64])), ALU.mult)
        with Scope(k):
            Sst = k.sb("m2S", [128, 8, 64], F32)
            Sbf = k.sb("m2Sb", [128, 8, 64], BF16)
            k.memset("dve", Sst, 0.0)
            k.memset("dve", Sbf, 0.0)
            GTm = [k.sb("m2GT%d" % i, [128, 2, 128], F32) for i in range(2)]
            lhs = [k.sb("m2lhs%d" % i, [128, 128], F32) for i in range(2)]
            ex = [k.sb("m2ex%d" % i, [128, 128], F32) for i in range(2)]
            MT = [k.sb("m2MT%d" % i, [128, 128], BF16) for i in range(2)]
            xd = [k.sb("m2xd%d" % i, [128, 4, 64], BF16) for i in range(2)]
            xw = [k.sb("m2xw%d" % i, [128, 4, 64], BF16) for i in range(2)]
            yo = [k.sb("m2yo%d" % i, [128, 4, 64], F32) for i in range(2)]
            sc8 = [k.sb("m2sc%d" % i, [128, 4, 8], F32) for i in range(2)]
            it = 0
            n2 = 0
            units = [(ci, d) for ci in range(NT) for d in range(2)]

            def pro_heads(ci, d, mid=None):
                dsl = slice(d * 4, (d + 1) * 4)
                c = S5_ORDER[d][ci]
                cs = slice(c * 128, (c + 1) * 128)
                b2 = d
                pc = P[0]
                k.mm(pc[:, 0:8], C.ones_f, dA[:, c, :])
                k.mm(pc[:, 8:16], LE if d == 0 else GE, dA[:, c, :])
                sc = sc8[b2]
                k.copy("act", sc[:, 0, :], pc[:, 0:8])
                k.act(sc[:, 1, :], pc[:, 0:8], AF.Exp)
                k.act(sc[:, 2, :], pc[:, 8:16], AF.Exp)
                k.tt("dve", sc[:, 3, :], sc[:, 0, :], pc[:, 8:16], ALU.subtract)
                k.act(sc[:, 3, :], sc[:, 3, :], AF.Exp)
                gt = GTm[b2]
                for g in range(2):
                    k.mm(P[1][:, g * 128:(g + 1) * 128], xc[:, 2 + g, cs], xc[:, 4 + g, cs])
                pgv = P[1].v(P[1].ap[:, 0:256].rearrange("p (g l) -> p g l", g=2))
                k.tt("dve", gt, pgv, bc(msk[:, d, :], [128, 2, 128], 1), ALU.mult)
                xv = xtok.v(xtok.ap[:, c, 0:256].rearrange("p (h e) -> p h e", h=4))
                k.tt("dve", xd[b2], xv, bc(dt[:, c, dsl], [128, 4, 64], 2), ALU.mult)
                k.tt("pool", xw[b2], xd[b2], bc(sc[:, 3, dsl], [128, 4, 64], 2), ALU.mult)
                PY, PS = P[4 + 2 * b2], P[5 + 2 * b2]
                for h in range(4):
                    g = h // 2
                    dh = d * 4 + h
                    b = h % 2
                    k.act(lhs[b], SU if d == 0 else SL, AF.Copy, scale=dA[:, c, dh:dh + 1])
                    ps_ = P[2 + b]
                    k.mm(ps_[:, 0:128], lhs[b], LE if d == 0 else GE)
                    k.act(ex[b], ps_[:, 0:128], AF.Exp)
                    k.tt("dve", MT[b], ex[b], gt[:, g, :], ALU.mult)
                    k.mm(PY[:, h * 64:(h + 1) * 64], MT[b], xd[b2][:, h, :])
                    k.mm(PY[:, 256 + h * 64:256 + (h + 1) * 64], xc[:, 4 + g, cs], Sbf[:, dh, :])
                    k.mm(PS[:, h * 64:(h + 1) * 64], xtok[:, c, 256 + g * 128:256 + (g + 1) * 128], xw[b2][:, h, :])
                    if h == 1 and mid is not None:
                        mid()

            def epi(ci, d):
                dsl = slice(d * 4, (d + 1) * 4)
                c = S5_ORDER[d][ci]
                b2 = d
                sc = sc8[b2]
                PY, PS = P[4 + 2 * b2], P[5 + 2 * b2]
                pyo = PY.v(PY.ap[:, 256:512].rearrange("p (h e) -> p h e", h=4))
                k.tt("dve", yo[b2], pyo, bc(sc[:, 2, dsl], [128, 4, 64], 2), ALU.mult)
                ysl = ysum[:, c, :]
                k.tt("dve", ysl, ysl, PY[:, 0:256], ALU.add)
                k.tt("pool", ysl, ysl, yo[b2].v(yo[b2].ap.rearrange("p h e -> p (h e)")), ALU.add)
                Sd = Sst[:, dsl, :]
                k.tt("pool", Sd, Sd, bc(sc[:, 1, dsl], [128, 4, 64], 2), ALU.mult)
                k.tt("dve", Sd, Sd, PS.v(PS.ap[:, 0:256].rearrange("p (h e) -> p h e", h=4)), ALU.add)
                k.copy("act", Sbf[:, dsl, :], Sd)
            for i, (ci, d) in enumerate(units):
                pro_heads(ci, d)
                if i >= 1:
                    epi(*units[i - 1])
            epi(*units[-1])
        dbg_tap(C, "m2ys%d" % l, ysum, [128, NT, 256])
        with Scope(k):
            OT = k.sb("OTm2", [128, 2, T], BF16)
            yg = [k.sb("m2yg%d" % i, [128, 256], F32) for i in range(2)]
            yb = [k.sb("m2yb%d" % i, [128, 256], BF16) for i in range(2)]
            junk = k.sb("m2junk", [128, 256], BF16)
            st = [k.sb("m2st%d" % i, [128, 2], F32) for i in range(2)]
            for ti in range(NT):
                y, s = yg[ti % 2], st[ti % 2]
                k.tt("dve", y, ysum[:, ti, :], zs[:, ti, :], ALU.mult)
                k.act(junk, y, AF.Square, accum=s[:, 0:1])
                k.act(s[:, 1:2], s[:, 0:1], AF.Sqrt, bias=EPS, scale=1.0 / 256)
                k.recip(s[:, 1:2], s[:, 1:2])
                k.stt(yb[ti % 2], y, s[:, 1:2], row[:, 20:276], ALU.mult, ALU.mult)
                pt = P[6 + (ti % 2)]
                ptv = pt.v(pt.ap.bitcast(BF16)[:, 0:256].rearrange("p (c t) -> p c t", t=128))
                for q in range(2):
                    k.tr(ptv[:, q, :], yb[ti % 2][:, q * 128:(q + 1) * 128], C.ident_b)
                k.copy("act", OT[:, :, ti * 128:(ti + 1) * 128], ptv)
            dbg_tap(C, "OTm2%d" % l, OT, [128, 2, T], BF16)
            k.dma("sp", [(otd[6:8].v(otd.ap[6:8].rearrange("c p t -> p c t")), OT)])


def host_merge_inputs(inp, l):
    o = {}
    o["wbr"] = np.ascontiguousarray(inp["w_branch"][l].reshape(1024, 1024))
    o["wout"] = np.ascontiguousarray(inp["w_out"][l])
    return o


def merge_phase(C, l, hT, yT):
    k = C.k
    Lw = C.L[l]
    P = C.ps
    with Scope(k):
        OTall = k.sb("OTall", [128, 8, T], BF16)
        k.dma("sp", [(OTall, C.otd.v(C.otd.ap.rearrange("c p t -> p c t")))])
        wb = k.sb("wb", [128, 8, 1024], BF16)
        wg = k.sb("wg", [128, 8, 4, 512], BF16)
        stg = Stager(C, 4096, n=2)
        for hh in range(2):
            stg.load(wb[:, :, hh * 512:(hh + 1) * 512], Lw.wbr[:, hh * 512:(hh + 1) * 512])
        sg = [k.sb("msg%d" % i, [128, 512], F32) for i in range(2)]
        ya = [k.sb("mya%d" % i, [128, 512], F32) for i in range(2)]
        yo = [k.sb("myo%d" % i, [128, 512], BF16) for i in range(2)]
        it = 0
        nq = 0
        for half in range(2):
            for j in range(4):
                c0 = 2472 + j * 1024 + half * 512
                stg.load(wg[:, :, j, :], Lw.w_in[:, c0:c0 + 512])
            for n4 in range(4):
                nch = half * 4 + n4
                for (t0, n) in (TBLK if C.ctx_out else TBLK_LAT):
                    y = ya[nq % 2]
                    nq += 1
                    for j in range(4):
                        b = it % 2
                        it += 1
                        pg, pb = P[2 * b], P[2 * b + 1]
                        for kk in range(8):
                            k.mm(pg[:, 0:n], wg[:, kk, j, n4 * 128:(n4 + 1) * 128], hT[:, kk, t0:t0 + n],
                                 start=(kk == 0), stop=(kk == 7))
                        for c in range(2):
                            k.mm(pb[:, 0:n], wb[:, 2 * j + c, nch * 128:(nch + 1) * 128], OTall[:, 2 * j + c, t0:t0 + n],
                                 start=(c == 0), stop=(c == 1))
                        k.act(sg[b][:, 0:n], pg[:, 0:n], AF.Sigmoid)
                        if j == 0:
                            k.tt("dve", y[:, 0:n], sg[b][:, 0:n], pb[:, 0:n], ALU.mult)
                        else:
                            k.tt("dve", sg[b][:, 0:n], sg[b][:, 0:n], pb[:, 0:n], ALU.mult)
                            k.tt("pool", y[:, 0:n], y[:, 0:n], sg[b][:, 0:n], ALU.add)
                    o = yo[nq % 2]
                    k.copy("act", o[:, 0:n], y[:, 0:n])
                    k.dma("sp", [(C.ytd2[nch, :, t0:t0 + n], o[:, 0:n])])


def outproj_phase(C, l, yT):
    k = C.k
    Lw = C.L[l]
    with Scope(k):
        wout = k.sb("wout", [128, 8, 1024], BF16)
        yT = k.sb("yT", [128, 8, T], BF16)
        k.dma("sp", [(yT, C.ytd2.v(C.ytd2.ap.rearrange("c p t -> p c t")))])
        stg = Stager(C, 4096, n=2)
        for hh in range(2):
            stg.load(wout[:, :, hh * 512:(hh + 1) * 512], Lw.wout[:, hh * 512:(hh + 1) * 512])

        def src(ti, dst):
            for hh in range(2):
                for kk in range(8):
                    k.mm(dst[hh], yT[:, kk, ti * 128:(ti + 1) * 128], wout[:, kk, hh * 512:(hh + 1) * 512],
                         start=(kk == 0), stop=(kk == 7))
        residual_epilogue(C, src, C.G1b, "mix%d" % l)


def residual_epilogue(C, src, Gb, tag, final=False):
    k = C.k
    P = C.ps
    with Scope(k):
        xt = [k.sb("ex%d" % i, [128, D], F32) for i in range(2)]
        ft = [k.sb("ef%d" % i, [128, D], F32) for i in range(2)]
        junk = k.sb("ejunk", [128, 512], BF16)
        st = [k.sb("est%d" % i, [128, 4], F32) for i in range(2)]
        for ti in range(2 if (final or not C.ctx_out) else 0, NT):
            v = 1 if ti < 2 else 0
            b = ti % 2
            dst = [P[2 * b], P[2 * b + 1]]
            k.dma("sp", [(xt[b], C.xr[ti])])
            r_ = src(ti, dst)
            if r_ is not None:
                dst = r_
            s = st[b]
            for hh in range(2):
                k.act(junk, dst[hh], AF.Square, accum=s[:, hh:hh + 1])
            k.tt("dve", s[:, 2:3], s[:, 0:1], s[:, 1:2], ALU.add)
            k.act(s[:, 3:4], s[:, 2:3], AF.Sqrt, bias=EPS, scale=1.0 / D)
            k.recip(s[:, 3:4], s[:, 3:4])
            for hh in range(2):
                sl = slice(hh * 512, (hh + 1) * 512)
                k.stt(ft[b][:, sl], dst[hh], s[:, 3:4], Gb[:, v, sl], ALU.mult, ALU.mult)
                k.tt("pool", xt[b][:, sl], xt[b][:, sl], ft[b][:, sl], ALU.add)
            if final:
                k.dma("sp", [(C.out_t[ti - 2], xt[b])])
            else:
                k.dma("sp", [(C.xres_t[ti], xt[b])])
                C.xr[ti] = C.xres_t[ti]
        if final:
            C.final_done = True


def host_ffn_consts():
    sel = np.zeros((128, 16, 128), np.float32)
    for e in range(16):
        sel[e, e, :] = 1.0
    return {"selT": sel}


def ffn_phase(C, l, inp_names=None):
    k = C.k
    Lw = C.L[l]
    P = C.ps
    with Scope(k):
        h2T = k.sb("h2T", [128, 8, T], BF16)
        coefT = k.sb("coefT", [128, T], F32)
        k.memset("pool", coefT, 0.0)
        with Scope(k):
            wr = k.sb("wr", [128, 8, 16], F32)
            k.dma("sp", [(wr, Lw.w_router.v(Lw.w_router.ap.rearrange("(k p) e -> p k e", p=128)))])
            xnf = [k.sb("rxnf%d" % i, [128, D], F32) for i in range(3)]
            h32 = [k.sb("rh32%d" % i, [128, 8, 128], F32) for i in range(3)]
            sm = [k.sb("rsm%d" % i, [128, 20], F32) for i in range(2)]
            aff = [k.sb("raff%d" % i, [128, 16], F32) for i in range(2)]
            A, Bc = C.A2, C.mod[:, 24:32, :]

            def router(i, x, rstd):
                v = 1 if i < 2 else 0
                b = i % 2
                k.ts("pool", xnf[b], x, rstd, ALU.mult)
                for hh in range(2):
                    pt = P[4 + hh]
                    for kk in range(4):
                        kc = hh * 4 + kk
                        k.tr(pt[:, kk * 128:(kk + 1) * 128], xnf[b][:, kc * 128:(kc + 1) * 128], C.ident_f)
                    for kk in range(4):
                        kc = hh * 4 + kk
                        k.ts("dve", h32[b][:, kc, :], pt[:, kk * 128:(kk + 1) * 128], A[:, kc:kc + 1, v], ALU.mult,
                             Bc[:, kc:kc + 1, v], ALU.add)
                pl = P[6]
                for kk in range(8):
                    k.mm(pl[:, 0:16], h32[b][:, kk, :], wr[:, kk, :], start=(kk == 0), stop=(kk == 7))
                s = sm[b]
                k.ins("dve", lambda e: e.reduce_max(out=s[:, 0:1].ap, in_=pl[:, 0:16].ap, axis=AX.X), [pl], [s])
                k.ts("dve", s[:, 1:2], s[:, 0:1], -1.0, ALU.mult)
                k.act(aff[b], pl[:, 0:16], AF.Exp, bias=s[:, 1:2], accum=s[:, 2:3])
                k.recip(s[:, 3:4], s[:, 2:3])
                k.ts("dve", aff[b], aff[b], s[:, 3:4], ALU.mult)
                pa = P[7]
                k.tr(pa[0:16, 0:128], aff[b], C.ident_f)
                k.copy("act", coefT[0:16, i * 128:(i + 1) * 128], pa[0:16, 0:128])
            norm_to_hT(C, h2T, A, Bc, "ffn", extra=router)
            dbg_tap(C, "aff%d" % l, coefT[0:16, :], [16, T])
            work = k.sb("rwork", [16, T], F32)
            m8 = k.sb("rm8", [16, 8], F32)
            k.copy("dve", work, coefT[0:16, :])
            for (lo, hi, cap) in ((0, CTX, 2 * CTX // 16), (CTX, T, 2 * SEQ // 16)):
                wv_ = work[:, lo:hi]
                for r in range(cap // 8):
                    k.ins("dve", lambda e, w=wv_: e.max(out=m8.ap, in_=w.ap), [wv_], [m8])
                    if r < cap // 8 - 1:
                        k.ins("dve", lambda e, w=wv_: e.match_replace(out=w.ap, in_to_replace=m8.ap, in_values=w.ap,
                                                                     imm_value=-1.0), [m8, wv_], [wv_])
                msk = k.sb("rmsk", [16, hi - lo], F32)
                k.ts("dve", msk, coefT[0:16, lo:hi], m8[:, 7:8], ALU.is_ge)
                k.tt("dve", coefT[0:16, lo:hi], coefT[0:16, lo:hi], msk, ALU.mult)
        dbg_tap(C, "coefT%d" % l, coefT[0:16, :], [16, T])
        dbg_tap(C, "h2T%d" % l, h2T, [128, 8, T], BF16)
        if DEVSTOP == "router":
            return
        with Scope(k):
            sel = k.sb("selT", [128, 16, 128], F32)
            k.dma("sp", [(sel, C.selT_in)])
            Wg = k.sb("Wg", [128, 8, 1024], BF16)
            Wu = k.sb("Wu", [128, 8, 1024], BF16)
            Wd = k.sb("Wd", [128, 8, 1024], BF16)
            stg = Stager(C, 4096, n=2)
            cbs = [k.sb("fcb%d" % i, [128, 512], F32) for i in range(2)]
            actT = [k.sb("factT%d" % i, [128, 8, 512], BF16) for i in range(2)]
            sg = [k.sb("fsg%d" % i, [128, 512], F32) for i in range(2)]
            ost = [k.sb("fost%d" % i, [128, D], F32) for i in range(2)]
            it = 0
            nexp = DEVNEXP
            for e in range(nexp):
                for (Wt, src) in ((Wg, Lw.w_gate), (Wu, Lw.w_up), (Wd, Lw.w_down)):
                    for hh in range(2):
                        stg.load(Wt[:, :, hh * 512:(hh + 1) * 512], src[e, :, hh * 512:(hh + 1) * 512])
                for bi, (t0, n) in enumerate(TBLK):
                    cb = cbs[bi % 2]
                    at = actT[bi % 2]
                    pc = P[6]
                    k.mm(pc[:, 0:n], sel[:, e, :], coefT[:, t0:t0 + n])
                    k.copy("act", cb[:, 0:n], pc[:, 0:n])
                    for f in range(8):
                        b = it % 2
                        it += 1
                        pg, pu = P[2 * b], P[2 * b + 1]
                        proj_fm(C, h2T, Wg[:, :, f * 128:(f + 1) * 128], t0, n, pg)
                        proj_fm(C, h2T, Wu[:, :, f * 128:(f + 1) * 128], t0, n, pu)
                        k.act(sg[b][:, 0:n], pg[:, 0:n], AF.Silu)
                        k.tt("dve", sg[b][:, 0:n], sg[b][:, 0:n], pu[:, 0:n], ALU.mult)
                        k.tt("pool", at[:, f, 0:n], sg[b][:, 0:n], cb[:, 0:n], ALU.mult)
                    for j in range(n // 128):
                        ti = t0 // 128 + j
                        o = ost[ti % 2]
                        for hh in range(2):
                            po = P[4 + hh]
                            for f in range(8):
                                k.mm(po, at[:, f, j * 128:(j + 1) * 128], Wd[:, f, hh * 512:(hh + 1) * 512],
                                     start=(f == 0), stop=(f == 7))
                            k.copy("act" if hh == 0 else "dve", o[:, hh * 512:(hh + 1) * 512], po)
                        k.dma("pool", [(C.facc_t[ti], o)], accum=(e > 0))
    with Scope(k):
        fin = [k.sb("ffin%d" % i, [128, D], F32) for i in range(2)]

        def src(ti, dst):
            f = fin[ti % 2]
            k.dma("pool", [(f, C.facc_t[ti])])
            return [f[:, 0:512], f[:, 512:1024]]
        residual_epilogue(C, src, C.G2b, "ffn%d" % l, final=(l == C.last_layer and "xres" not in C.dbg_names))


CSEL = 2 * CTX // 16 + 2 * SEQ // 16


def host_ffn_consts2():
    ic = np.broadcast_to(np.arange(CSEL, dtype=np.float32)[None, :], (128, CSEL))
    ip = np.arange(128, dtype=np.float32)[:, None] + 128.0 * np.arange(3, dtype=np.float32)[None, :]
    return {"iotac": np.ascontiguousarray(ic), "iotap": np.ascontiguousarray(ip)}


def ffn_phase_gather(C, l):
    k = C.k
    Lw = C.L[l]
    P = C.ps
    A, Bc = C.A2, C.mod[:, 24:32, :]
    with Scope(k):
        xn_tok = k.sb("xn_tok", [128, NT, D], BF16)
        coefT = k.sb("coefT", [128, T], F32)
        posr = k.sb("posr", [128, T], F32)
        tokc = k.sb("tokc", [128, NT, 32], F32)
        k.memset("pool", coefT, 0.0)
        k.memset("pool", posr, 0.0)
        with Scope(k):
            wr = k.sb("wr", [128, 8, 16], F32)
            k.dma("sp", [(wr, Lw.w_router.v(Lw.w_router.ap.rearrange("(k p) e -> p k e", p=128)))])
            xin = [k.sb("gx%d" % i, [128, D], F32) for i in range(3)]
            junk = k.sb("gjunk", [128, D], BF16)
            st = [k.sb("gst%d" % i, [128, 2], F32) for i in range(3)]
            xnf = [k.sb("rxnf%d" % i, [128, D], F32) for i in range(3)]
            h32 = [k.sb("rh32%d" % i, [128, 8, 128], F32) for i in range(3)]
            sm = [k.sb("rsm%d" % i, [128, 20], F32) for i in range(2)]
            aff = [k.sb("raff%d" % i, [128, 16], F32) for i in range(2)]
            def stA(i):
                x, s_ = xin[i % 3], st[i % 3]
                k.dma("sp", [(x, C.xr[i])])
                k.act(junk, x, AF.Square, accum=s_[:, 0:1])
                k.act(s_[:, 1:2], s_[:, 0:1], AF.Sqrt, bias=EPS, scale=1.0 / D)
                k.recip(s_[:, 1:2], s_[:, 1:2])
                k.ts("dve", xn_tok[:, i, :], x, s_[:, 1:2], ALU.mult)
                k.act(xnf[i % 3], x, AF.Identity, scale=s_[:, 1:2])

            def stB(i):
                v = 1 if i < 2 else 0
                b = i % 3
                for hh in range(2):
                    pt = P[4 + hh]
                    for kk in range(4):
                        kc = hh * 4 + kk
                        k.tr(pt[:, kk * 128:(kk + 1) * 128], xnf[b][:, kc * 128:(kc + 1) * 128], C.ident_f)
                    for kk in range(4):
                        kc = hh * 4 + kk
                        if hh == 0:
                            k.ts("dve", h32[b][:, kc, :], pt[:, kk * 128:(kk + 1) * 128], A[:, kc:kc + 1, v], ALU.mult,
                                 Bc[:, kc:kc + 1, v], ALU.add)
                        else:
                            k.act(h32[b][:, kc, :], pt[:, kk * 128:(kk + 1) * 128], AF.Identity,
                                  bias=Bc[:, kc:kc + 1, v], scale=A[:, kc:kc + 1, v])
                pl = P[6 + (i % 2)]
                for kk in range(8):
                    k.mm(pl[:, 0:16], h32[b][:, kk, :], wr[:, kk, :], start=(kk == 0), stop=(kk == 7))

            def stC(i):
                b = i % 2
                pl = P[6 + (i % 2)]
                sv = sm[b]
                k.ins("dve", lambda e, sv=sv, pl=pl: e.reduce_max(out=sv[:, 0:1].ap, in_=pl[:, 0:16].ap, axis=AX.X), [pl], [sv])
                k.ts("dve", sv[:, 1:2], sv[:, 0:1], -1.0, ALU.mult)
                k.act(aff[b], pl[:, 0:16], AF.Exp, bias=sv[:, 1:2], accum=sv[:, 2:3])
                k.recip(sv[:, 3:4], sv[:, 2:3])
                k.ts("dve", aff[b], aff[b], sv[:, 3:4], ALU.mult)
                pa = P[2 + (i % 2)]
                k.tr(pa[0:16, 0:128], aff[b], C.ident_f)
                k.copy("act", coefT[0:16, i * 128:(i + 1) * 128], pa[0:16, 0:128])
            tiles = list(range(NT)) if C.ctx_out else list(range(2, NT))
            if not C.ctx_out:
                k.memset("pool", xn_tok[:, 0:2, :], 0.0)
            for step in range(len(tiles) + 2):
                if step < len(tiles):
                    stA(tiles[step])
                if 1 <= step <= len(tiles):
                    stB(tiles[step - 1])
                if step >= 2:
                    stC(tiles[step - 2])
            work = k.sb("rwork", [16, T], F32)
            msk = k.sb("rmsk", [16, T], F32)
            m8 = k.sb("rm8", [16, 8], F32)
            k.copy("dve", work, coefT[0:16, :])
            if not C.ctx_out:
                k.memset("dve", msk[:, 0:CTX], 0.0)
            for (lo, hi, cap, off) in ((0, CTX, 2 * CTX // 16, 0), (CTX, T, 2 * SEQ // 16, 2 * CTX // 16)):
                if lo == 0 and not C.ctx_out:
                    continue
                wv_ = work[:, lo:hi]
                for r in range(cap // 8):
                    k.ins("dve", lambda e, w=wv_: e.max(out=m8.ap, in_=w.ap), [wv_], [m8])
                    if r < cap // 8 - 1:
                        k.ins("dve", lambda e, w=wv_: e.match_replace(out=w.ap, in_to_replace=m8.ap, in_values=w.ap,
                                                                     imm_value=-1.0), [m8, wv_], [wv_])
                k.ts("dve", msk[:, lo:hi], coefT[0:16, lo:hi], m8[:, 7:8], ALU.is_ge)
                k.tt("dve", coefT[0:16, lo:hi], coefT[0:16, lo:hi], msk[:, lo:hi], ALU.mult)
                k.scan(posr[0:16, lo:hi], msk[:, lo:hi], msk[:, lo:hi], 0.0, ALU.add, ALU.max)
                k.ts("dve", posr[0:16, lo:hi], posr[0:16, lo:hi], float(off - 1), ALU.add)
            if not C.ctx_out:
                k.memset("dve", tokc[:, 0:2, :], 0.0)
            for ti in (range(NT) if C.ctx_out else range(2, NT)):
                pa = P[6 + (ti % 2)]
                k.tr(pa[:, 0:16], posr[0:16, ti * 128:(ti + 1) * 128], C.ident_f[0:16, 0:16])
                k.tr(pa[:, 16:32], msk[:, ti * 128:(ti + 1) * 128], C.ident_f[0:16, 0:16])
                k.copy("act", tokc[:, ti, :], pa[:, 0:32])
        dbg_tap(C, "coefT%d" % l, coefT[0:16, :], [16, T])
        dbg_tap(C, "posr%d" % l, posr[0:16, :], [16, T])
        dbg_tap(C, "tokc%d" % l, tokc, [128, NT, 32])
        with Scope(k):
            sel = k.sb("selT", [128, 16, 128], F32)
            iotac = k.sb("iotac", [128, CSEL], F32)
            iotap = k.sb("iotap", [128, 3], F32)
            k.dma("sp", [(sel, C.selT_in), (iotac, C.iotac_in), (iotap, C.iotap_in)])
            Wg = k.sb("Wg", [128, 8, 1024], BF16)
            Wu = k.sb("Wu", [128, 8, 1024], BF16)
            Wd = k.sb("Wd", [128, 8, 1024], BF16)
            stg = Stager(C, 2048, n=3)
            Sel = k.sb("Sel", [128, NT, CSEL], BF16)
            SelT2 = [k.sb("SelT%d" % i, [128, 3, T], BF16) for i in range(2)]
            xsT = k.sb("xsT", [128, 8, CSEL], BF16)
            actT = k.sb("actT", [128, 8, 384], BF16)
            k.memset("pool", actT, 0.0)
            ys2 = [k.sb("ys%d" % i, [128, 3, D], BF16) for i in range(2)]
            sg = [k.sb("fsg%d" % i, [128, CSEL], F32) for i in range(2)]
            cbs = [k.sb("fcb%d" % i, [128, 512], F32) for i in range(2)]
            ost = [k.sb("fost%d" % i, [128, D], F32) for i in range(2)]
            it = 0
            nld = [0]

            def load_w(e, Wt, src):
                if "W" in DEVFLAGS and e > 0:
                    return
                for qq in range(2):
                    srcv = src[e, :, qq * 512:(qq + 1) * 512]
                    k.dma("pool", [(Wt[:, :, qq * 512:(qq + 1) * 512], srcv.v(srcv.ap.rearrange("(k p) n -> p k n", p=128)))])
            load_w(0, Wg, Lw.w_gate)
            load_w(0, Wu, Lw.w_up)
            load_w(0, Wd, Lw.w_down)
            ne = DEVNEXP
            c0 = 2 * CTX // 16

            def st_sel(e):
                for ti in range(NT):
                    k.ts("dve", Sel[:, ti, :], iotac, tokc[:, ti, e:e + 1], ALU.is_equal,
                         tokc[:, ti, 16 + e:17 + e], ALU.mult)

            def st_gather(e):
                for kk in range(8):
                    pgt = P[kk % 2]
                    for ti in range(NT):
                        k.mm(pgt[:, 0:CSEL], xn_tok[:, ti, kk * 128:(kk + 1) * 128], Sel[:, ti, :],
                             start=(ti == 0), stop=(ti == NT - 1))
                    k.act(xsT[:, kk, 0:c0], pgt[:, 0:c0], AF.Identity, bias=Bc[:, kk:kk + 1, 1], scale=A[:, kk:kk + 1, 1])
                    k.ts("dve", xsT[:, kk, c0:CSEL], pgt[:, c0:CSEL], A[:, kk:kk + 1, 0], ALU.mult, Bc[:, kk:kk + 1, 0], ALU.add)

            def st_gateup(e):
                for f in range(8):
                    b = f % 2
                    pg, pu = P[2 + 2 * b], P[3 + 2 * b]
                    for kk in range(8):
                        k.mm(pg[:, 0:CSEL], Wg[:, kk, f * 128:(f + 1) * 128], xsT[:, kk, :], start=(kk == 0), stop=(kk == 7))
                    for kk in range(8):
                        k.mm(pu[:, 0:CSEL], Wu[:, kk, f * 128:(f + 1) * 128], xsT[:, kk, :], start=(kk == 0), stop=(kk == 7))
                    k.act(sg[b], pg[:, 0:CSEL], AF.Silu)
                    k.tt("dve", actT[:, f, 0:CSEL], sg[b], pu[:, 0:CSEL], ALU.mult)

            def st_down(e):
                ys = ys2[e % 2]
                for ct in range(3):
                    for hh in range(2):
                        po = P[6 + hh]
                        for f in range(8):
                            k.mm(po, actT[:, f, ct * 128:(ct + 1) * 128], Wd[:, f, hh * 512:(hh + 1) * 512],
                                 start=(f == 0), stop=(f == 7))
                        k.copy("dve" if hh == 0 else "act", ys[:, ct, hh * 512:(hh + 1) * 512], po)

            def st_selT(e):
                ST = SelT2[e % 2]
                for bi, (t0, n) in enumerate(TBLK):
                    pp, pc = P[2 + 2 * (bi % 2)], P[3 + 2 * (bi % 2)]
                    k.mm(pp[:, 0:n], sel[:, e, :], posr[:, t0:t0 + n])
                    k.mm(pc[:, 0:n], sel[:, e, :], coefT[:, t0:t0 + n])
                    cb = cbs[bi % 2]
                    k.copy("act", cb[:, 0:n], pc[:, 0:n])
                    for ct in range(3):
                        k.stt(ST[:, ct, t0:t0 + n], pp[:, 0:n], iotap[:, ct:ct + 1], cb[:, 0:n], ALU.is_equal, ALU.mult)

            def st_scatter(e):
                for ti in range(NT):
                    o = ost[ti % 2]
                    for hh in range(2):
                        po = P[4 + (ti % 2) * 2 + hh]
                        n_ = 0
                        for ee in (e - 1, e):
                            for ct in range(3):
                                k.mm(po, SelT2[ee % 2][:, ct, ti * 128:(ti + 1) * 128], ys2[ee % 2][:, ct, hh * 512:(hh + 1) * 512],
                                     start=(n_ == 0), stop=(n_ == 5))
                                n_ += 1
                        k.copy("dve" if hh == 0 else "act", o[:, hh * 512:(hh + 1) * 512], po)
                    k.dma("pool", [(C.facc_t[ti], o)], accum=(e > 1))
            st_sel(0)
            st_gather(0)
            for e in range(ne):
                st_gateup(e)
                if e + 1 < ne:
                    load_w(e + 1, Wg, Lw.w_gate)
                    load_w(e + 1, Wu, Lw.w_up)
                    st_sel(e + 1)
                st_down(e)
                if e + 1 < ne:
                    load_w(e + 1, Wd, Lw.w_down)
                    st_gather(e + 1)
                st_selT(e)
                if e % 2 == 1:
                    st_scatter(e)
    with Scope(k):
        fin = [k.sb("ffin%d" % i, [128, D], F32) for i in range(2)]

        def src(ti, dst):
            f = fin[ti % 2]
            k.dma("pool", [(f, C.facc_t[ti])])
            return [f[:, 0:512], f[:, 512:1024]]
        residual_epilogue(C, src, C.G2b, "ffn%d" % l, final=(l == C.last_layer and "xres" not in C.dbg_names))


def make_in_maps(inp):
    inp = {n: np.asarray(v) for n, v in inp.items()}
    shared = dict(host_consts())
    for l in range(DEPTH):
        for fn in (host_layer_inputs, host_mixer_inputs, host_s5_inputs, host_m2_inputs, host_merge_inputs):
            for n, v in fn(inp, l).items():
                shared["%s%d" % (n, l)] = np.ascontiguousarray(v)
        for n in ("w_router", "w_gate", "w_up", "w_down"):
            shared["%s%d" % (n, l)] = np.ascontiguousarray(inp[n][l])
    cc = col_layout(inp["c_ctx"])
    maps = []
    for b in range(inp["x"].shape[0]):
        m = dict(shared)
        m["x"] = np.ascontiguousarray(inp["x"][b])
        m["ctx"] = np.ascontiguousarray(inp["ctx"][b])
        m["cvec"] = np.ascontiguousarray(np.stack([col_layout(inp["c"][b]), cc], -1))
        maps.append(m)
    return maps


def kernel(**inputs):
    maps = make_in_maps(inputs)
    nc, C = build_program()
    res = run_bass_kernel_spmd(nc, maps, core_ids=list(range(len(maps))))
    return np.stack([np.asarray(r["out"], dtype=np.float32) for r in res.results], 0)
```

```python
import contextlib
import numpy as np
import concourse.bass as bass
import concourse.mybir as mybir
from concourse.bass_utils import run_bass_kernel_spmd

F32 = mybir.dt.float32
BF16 = mybir.dt.bfloat16
ALU = mybir.AluOpType
AF = mybir.ActivationFunctionType
AX = mybir.AxisListType

D = 1024
SEQ = 2048
CTX = 256
T = SEQ + CTX
NT = T // 128
DEPTH = 2
GRID_W = 64
N_IN = 6568
EPS = 1e-6
NEG = -30000.0
DEVSTOP = None
INORDER = ('pe',)
DEVFLAGS = ''
DEVLAYERS = DEPTH
DEVNEXP = 16


class Buf:
    __slots__ = ("name", "w", "r", "excl", "strictw")

    def __init__(self, name):
        self.name = name
        self.strictw = False
        self.excl = False
        self.w = None
        self.r = {}


class TV:
    __slots__ = ("ap", "bufs")

    def __init__(self, ap, bufs):
        self.ap = ap
        self.bufs = bufs

    def __getitem__(self, idx):
        return TV(self.ap[idx], self.bufs)

    def v(self, ap):
        return TV(ap, self.bufs)

    @property
    def shape(self):
        return self.ap.shape


class Eng:
    def __init__(self, name, eng, sem):
        self.name = name
        self.eng = eng
        self.sem = sem
        self.count = 0
        self.known = {}


class K:
    NDSEM = 20

    def __init__(self, nc, es):
        self.nc = nc
        self.es = es
        self.sems = {}
        self.engs = {}
        for name, eng in (("pe", nc.tensor), ("dve", nc.vector), ("act", nc.scalar),
                          ("pool", nc.gpsimd), ("sp", nc.sync)):
            s = es.enter_context(nc.semaphore("s_" + name))
            self.sems[name] = s
            self.engs[name] = Eng(name, eng, name)
        self.dq = {}
        for q in ("sp", "pool", "act"):
            lst = []
            for i in range(self.NDSEM):
                key = "d_%s_%d" % (q, i)
                self.sems[key] = es.enter_context(nc.semaphore(key))
                lst.append([key, 0])
            self.dq[q] = [lst, 0]
        self.ninstr = 0
        self.nwait = 0
        self.inorder = False
        self.uid = 0

    def sb(self, name, shape, dtype):
        self.uid += 1
        name = "%s_%d" % (name, self.uid)
        t = self.es.enter_context(self.nc.sbuf_tensor(name, list(shape), dtype))
        return TV(t.ap(), [Buf(name)])

    def ps(self, name, shape, dtype=F32):
        self.uid += 1
        name = "%s_%d" % (name, self.uid)
        t = self.es.enter_context(self.nc.psum_tensor(name, list(shape), dtype))
        b = Buf(name)
        b.excl = True
        return TV(t.ap(), [b])

    def dram(self, name, shape, dtype, kind="Internal"):
        t = self.nc.dram_tensor(name, list(shape), dtype, kind=kind)
        return TV(t.ap(), [Buf(name)])

    def split(self, tv, n, sl):
        out = []
        bufs = []
        for i in range(n):
            b = Buf("%s.%d" % (tv.bufs[0].name, i))
            bufs.append(b)
            out.append(TV(tv.ap[sl(i)], [b]))
        tv.bufs = bufs
        return out

    def _need(self, E, ev, waits, strict=False):
        if ev is None:
            return
        sem, val, clock = ev
        if E.known.get(sem, 0) >= val:
            return
        if sem == E.sem and self.inorder and E.name in INORDER and not strict:
            return
        if waits.get(sem, (0, None))[0] < val:
            waits[sem] = (val, clock)

    def _deps(self, E, reads, writes, skip_waw=()):
        waits = {}
        for tv in reads:
            if isinstance(tv, TV):
                for b in tv.bufs:
                    self._need(E, b.w, waits, b.strictw)
                    if b.excl:
                        for sem, (val, clock) in b.r.items():
                            if sem != E.sem:
                                self._need(E, (sem, val, clock), waits)
        for tv in writes:
            for b in tv.bufs:
                if b not in skip_waw:
                    self._need(E, b.w, waits, b.strictw)
                    for sem, (val, clock) in b.r.items():
                        self._need(E, (sem, val, clock), waits)
        for sem, (val, clock) in waits.items():
            if E.known.get(sem, 0) >= val:
                continue
            E.eng.wait_ge(self.sems[sem], val)
            self.nwait += 1
            E.known[sem] = val
            if clock:
                for s2, v2 in clock.items():
                    if E.known.get(s2, 0) < v2:
                        E.known[s2] = v2

    def _mark(self, ev, reads, writes):
        sem, val, clock = ev
        for tv in reads:
            if isinstance(tv, TV):
                for b in tv.bufs:
                    old = b.r.get(sem)
                    if old is None or old[0] < val:
                        b.r[sem] = (val, clock)
        for tv in writes:
            for b in tv.bufs:
                b.w = ev
                b.r = {}
                b.strictw = False

    def ins(self, en, fn, reads, writes, skip_waw=()):
        E = self.engs[en]
        self.inorder = True
        self._deps(E, reads, writes, skip_waw)
        self.inorder = False
        inst = fn(E.eng)
        E.count += 1
        inst.then_inc(self.sems[E.sem], 1)
        clock = dict(E.known)
        if en in INORDER:
            clock[E.sem] = E.count
        ev = (E.sem, E.count, clock)
        self._mark(ev, reads, writes)
        self.ninstr += 1
        return ev

    def dma(self, q, pairs, accum=False):
        if accum:
            q = "pool"
        E = self.engs[q]
        lst, idx = self.dq[q]
        slot = lst[idx % self.NDSEM]
        self.dq[q][1] = idx + 1
        key, total = slot
        reads = [p[1] for p in pairs]
        writes = [p[0] for p in pairs]
        self._deps(E, reads, writes)
        if total > 0 and E.known.get(key, 0) < total:
            E.eng.wait_ge(self.sems[key], total)
            E.known[key] = total
        for o, i in pairs:
            if accum:
                E.eng.dma_start(out=o.ap, in_=i.ap, accum_op=ALU.add).then_inc(self.sems[key], 16)
            else:
                E.eng.dma_start(out=o.ap, in_=i.ap).then_inc(self.sems[key], 16)
            total += 16
        slot[1] = total
        ev = (key, total, dict(E.known))
        self._mark(ev, reads, writes)
        self.ninstr += len(pairs)
        return ev

    def wait_all(self, en, tvs):
        E = self.engs[en]
        self._deps(E, tvs, [])

    def mm(self, out, lhsT, rhs, start=True, stop=True):
        skip = () if start else tuple(out.bufs)
        return self.ins("pe", lambda e: e.matmul(out.ap, lhsT.ap, rhs.ap, start=start, stop=stop),
                        [lhsT, rhs], [out], skip_waw=skip)

    def tr(self, out, in_, ident):
        return self.ins("pe", lambda e: e.transpose(out.ap, in_.ap, ident.ap), [in_, ident], [out])

    def act(self, out, in_, func, bias=None, scale=1.0, accum=None, en="act"):
        kw = {}
        rd = [in_]
        if bias is not None:
            kw["bias"] = bias.ap if isinstance(bias, TV) else bias
            rd.append(bias)
        if isinstance(scale, TV):
            kw["scale"] = scale.ap
            rd.append(scale)
        else:
            kw["scale"] = scale
        wr = [out]
        if accum is not None:
            kw["accum_out"] = accum.ap
            wr.append(accum)
        ev = self.ins("act", lambda e: e.activation(out.ap, in_.ap, func, **kw), rd, wr)
        if accum is not None:
            for b in accum.bufs:
                b.strictw = True
        return ev

    def tt(self, en, out, in0, in1, op):
        return self.ins(en, lambda e: e.tensor_tensor(out=out.ap, in0=in0.ap, in1=in1.ap, op=op),
                        [in0, in1], [out])

    def ts(self, en, out, in0, s1, op0, s2=None, op1=None, accum=None):
        a1 = s1.ap if isinstance(s1, TV) else s1
        a2 = s2.ap if isinstance(s2, TV) else s2
        kw = {}
        wr = [out]
        if op1 is not None:
            kw["op1"] = op1
        if accum is not None:
            kw["accum_out"] = accum.ap
            wr.append(accum)
        return self.ins(en, lambda e: e.tensor_scalar(out=out.ap, in0=in0.ap, scalar1=a1, scalar2=a2,
                                                      op0=op0, **kw), [in0, s1, s2], wr)

    def stt(self, out, in0, sc, in1, op0, op1, en="dve"):
        a = sc.ap if isinstance(sc, TV) else sc
        return self.ins(en, lambda e: e.scalar_tensor_tensor(out=out.ap, in0=in0.ap, scalar=a, in1=in1.ap,
                                                             op0=op0, op1=op1), [in0, sc, in1], [out])

    def copy(self, en, out, in_):
        if en == "act":
            return self.ins("act", lambda e: e.copy(out.ap, in_.ap), [in_], [out])
        return self.ins(en, lambda e: e.tensor_copy(out=out.ap, in_=in_.ap), [in_], [out])

    def recip(self, out, in_):
        return self.ins("dve", lambda e: e.reciprocal(out=out.ap, in_=in_.ap), [in_], [out])

    def memset(self, en, out, val):
        return self.ins(en, lambda e: e.memset(out.ap, val), [], [out])

    def scan(self, out, d0, d1, init, op0, op1):
        a = init.ap if isinstance(init, TV) else init
        return self.ins("dve", lambda e: e.tensor_tensor_scan(out=out.ap, data0=d0.ap, data1=d1.ap, initial=a,
                                                              op0=op0, op1=op1), [d0, d1, init], [out])


def col_layout(v):
    v = np.asarray(v, np.float32)
    return np.ascontiguousarray(v.reshape(-1, 128).T)


def host_consts():
    c = {}
    c["ident"] = np.eye(128, dtype=np.float32)
    c["cosf"], c["sinf"] = rope_tables()
    c["jmat"] = np.ascontiguousarray(np.eye(128, dtype=np.float32)[::-1])
    c["m2msk"] = host_m2_masks()
    c.update(host_ffn_consts())
    c.update(host_ffn_consts2())
    return c


def host_layer_inputs(inp, l):
    o = {}
    o["w_ada"] = np.ascontiguousarray(inp["w_ada"][l])
    o["bada"] = col_layout(inp["b_ada"][l])
    g = np.stack([col_layout(inp[n][l]) for n in ("g_pre_mix", "g_post_mix", "g_pre_ffn", "g_post_ffn")], 1)
    o["gvec"] = np.ascontiguousarray(g)
    o["w_in"] = np.ascontiguousarray(inp["w_in"][l])
    return o


class Ctx:
    pass


def build_program(dbg=()):
    nc = bass.Bass("TRN2", target_bir_lowering=False)
    es = contextlib.ExitStack()
    k = K(nc, es)
    C = Ctx()
    C.nc, C.k, C.es, C.dbg = nc, k, es, {}
    C.dbg_names = dbg

    def din(name, shape, dtype=F32):
        return k.dram(name, shape, dtype, kind="ExternalInput")

    C.x_in = din("x", [SEQ, D])
    C.ctx_in = din("ctx", [CTX, D])
    C.cvec = din("cvec", [128, 8, 2])
    C.ident_in = din("ident", [128, 128])
    C.cosf_in = din("cosf", [128, T], BF16)
    C.jmat_in = din("jmat", [128, 128])
    C.m2msk_in = din("m2msk", [128, 4, 128])
    C.selT_in = din("selT", [128, 16, 128])
    C.iotac_in = din("iotac", [128, CSEL])
    C.iotap_in = din("iotap", [128, 3])
    C.facc = k.dram("facc", [T, D], F32)
    C.facc_t = k.split(C.facc, NT, lambda i: (slice(i * 128, (i + 1) * 128), slice(None)))
    C.sinf_in = din("sinf", [128, T], BF16)
    C.otd = k.dram("otd", [8, 128, T], BF16)
    C.ytd2 = k.dram("ytd2", [8, 128, T], BF16)
    C.ytd = k.dram("ytd", [T, D], BF16)
    C.ytd_t = k.split(C.ytd, NT, lambda i: (slice(i * 128, (i + 1) * 128), slice(None)))
    C.L = []
    for l in range(DEPTH):
        Lw = Ctx()
        Lw.w_ada = din("w_ada%d" % l, [D, 6 * D])
        Lw.bada = din("bada%d" % l, [128, 48])
        Lw.gvec = din("gvec%d" % l, [128, 4, 8])
        Lw.w_in = din("w_in%d" % l, [D, N_IN])
        Lw.wfm = din("wfm%d" % l, [D, NFM])
        Lw.wtm = din("wtm%d" % l, [D, 520])
        Lw.wuq = din("wuq%d" % l, [256, 1024])
        Lw.wukv = din("wukv%d" % l, [128, 768])
        Lw.gmla = din("gmla%d" % l, [128, 3])
        Lw.nab = din("nab%d" % l, [6, 4, 128, 5, 128])
        Lw.s5par = din("s5par%d" % l, [3, 2048])
        Lw.m2vec = din("m2vec%d" % l, [128, 6, 5])
        Lw.wbr = din("wbr%d" % l, [1024, 1024])
        Lw.w_router = din("w_router%d" % l, [D, 16])
        Lw.w_gate = din("w_gate%d" % l, [16, D, D])
        Lw.w_up = din("w_up%d" % l, [16, D, D])
        Lw.w_down = din("w_down%d" % l, [16, D, D])
        Lw.wout = din("wout%d" % l, [1024, 1024])
        Lw.m2row = din("m2row%d" % l, [1, 276])
        Lw.s5BT = din("s5BT%d" % l, [128, 2, 8, 2, 128])
        Lw.s5CT = din("s5CT%d" % l, [128, 2, 8, 2, 128])
        Lw.s5vec = din("s5vec%d" % l, [128, 2, 2])
        Lw.s5wglu = din("s5wglu%d" % l, [256, 256])
        Lw.s5wu = din("s5wu%d" % l, [D, 256])
        C.L.append(Lw)
    C.out = k.dram("out", [SEQ, D], F32, kind="ExternalOutput")
    C.xres = k.dram("xres", [T, D], F32)
    C.xres_t = k.split(C.xres, NT, lambda i: (slice(i * 128, (i + 1) * 128), slice(None)))

    C.ident_f = k.sb("ident_f", [128, 128], F32)
    C.ident_b = k.sb("ident_b", [128, 128], BF16)
    C.ones_f = k.sb("ones_f", [128, 128], F32)
    C.ones_b = k.sb("ones_b", [128, 128], BF16)
    k.dma("sp", [(C.ident_f, C.ident_in)])
    k.copy("dve", C.ident_b, C.ident_f)
    k.memset("dve", C.ones_f, 1.0)
    k.memset("dve", C.ones_b, 1.0)
    C.ps = [k.ps("ps%d" % i, [128, 512], F32) for i in range(8)]

    C.xr = [C.ctx_in[i * 128:(i + 1) * 128, :] if i < 2 else C.x_in[(i - 2) * 128:(i - 1) * 128, :] for i in range(NT)]
    C.out_t = k.split(C.out, NT - 2, lambda i: (slice(i * 128, (i + 1) * 128), slice(None)))
    C.final_done = False
    C.last_layer = DEVLAYERS - 1

    for l in range(DEVLAYERS):
        layer(C, l)

    if not C.final_done:
        with Scope(k):
            stg = [k.sb("op%d" % i, [128, D], F32) for i in range(3)]
            for i in range(2, NT):
                s = stg[i % 3]
                k.dma("sp", [(s, C.xr[i])])
                k.dma("pool", [(C.out_t[i - 2], s)])
    if "xres" in C.dbg_names:
        d = k.dram("dbg_xres", [T, D], F32, kind="ExternalOutput")
        with Scope(k):
            stg = [k.sb("dx%d" % i, [128, D], F32) for i in range(2)]
            for i in range(NT):
                k.dma("sp", [(stg[i % 2], C.xr[i])])
                k.dma("sp", [(d[i * 128:(i + 1) * 128, :], stg[i % 2])])
        C.dbg["xres"] = d
    k.wait_all("sp", [C.out] + [C.dbg[n] for n in C.dbg])
    return nc, C


def dbg_tap(C, name, tv, shape, dtype=F32):
    if name not in C.dbg_names:
        return
    k = C.k
    d = k.dram("dbg_" + name, list(shape), dtype, kind="ExternalOutput")
    k.dma("sp", [(d, tv)])
    C.dbg[name] = d


def barrier(k):
    targets = {}
    for name, E in k.engs.items():
        if E.count:
            targets[E.sem] = E.count
    for q, (lst, _) in k.dq.items():
        for key, total in lst:
            if total:
                targets[key] = total
    for name, E in k.engs.items():
        for sem, val in targets.items():
            if E.known.get(sem, 0) < val:
                E.eng.wait_ge(k.sems[sem], val)
                E.known[sem] = val
                k.nwait += 1


class Scope:
    def __init__(self, k):
        self.k = k

    def __enter__(self):
        self.prev = self.k.es
        self.les = contextlib.ExitStack()
        self.k.es = self.les
        return self

    def __exit__(self, *a):
        barrier(self.k)
        self.k.es = self.prev
        self.les.close()
        return False


def bank(C, i, shape=None, dtype=F32):
    return C.ps[i]


def layer(C, l):
    k = C.k
    Lw = C.L[l]
    C.ctx_out = (l < DEPTH - 1)
    with Scope(k):
        mod = k.sb("mod", [128, 48, 2], F32)
        gv = k.sb("gv", [128, 4, 8], F32)
        k.dma("sp", [(gv, Lw.gvec)])
        with Scope(k):
            sc = k.sb("sc", [128, 8, 2], F32)
            scb = k.sb("scb", [128, 8, 2], BF16)
            bada = k.sb("bada", [128, 48], F32)
            k.dma("sp", [(sc, C.cvec), (bada, Lw.bada)])
            k.act(scb, sc, AF.Silu)
            wst = [k.sb("wada%d" % i, [128, 8, 768], F32) for i in range(2)]
            wbf = [k.sb("wadab%d" % i, [128, 8, 768], BF16) for i in range(2)]
            wv = Lw.w_ada.v(Lw.w_ada.ap.rearrange("(k p) n -> p k n", p=128))
            pm = C.ps[0]
            for jb in range(8):
                w = wst[jb % 2]
                wb_ = wbf[jb % 2]
                k.dma("sp" if jb % 2 == 0 else "pool", [(w, wv[:, :, jb * 768:(jb + 1) * 768])])
                k.copy("act", wb_[:, 0:4, :], w[:, 0:4, :])
                k.copy("dve", wb_[:, 4:8, :], w[:, 4:8, :])
                for jj in range(6):
                    j = jb * 6 + jj
                    for kk in range(8):
                        k.mm(pm[:, j * 2:(j + 1) * 2], wb_[:, kk, jj * 128:(jj + 1) * 128], scb[:, kk, :],
                             start=(kk == 0), stop=(kk == 7))
            pmv = pm.v(pm.ap[:, 0:96].rearrange("p (j v) -> p j v", v=2))
            for v in range(2):
                k.tt("dve", mod[:, :, v], pmv[:, :, v], bada, ALU.add)
        dbg_tap(C, "mod%d" % l, mod, [128, 48, 2])
        A1 = k.sb("A1", [128, 8, 2], F32)
        A2 = k.sb("A2", [128, 8, 2], F32)
        G1c = k.sb("G1c", [128, 8, 2], F32)
        G2c = k.sb("G2c", [128, 8, 2], F32)
        for v in range(2):
            k.ts("dve", A1[:, :, v], mod[:, 8:16, v], 1.0, ALU.add)
            k.tt("dve", A1[:, :, v], A1[:, :, v], gv[:, 0, :], ALU.mult)
            k.ts("dve", A2[:, :, v], mod[:, 32:40, v], 1.0, ALU.add)
            k.tt("dve", A2[:, :, v], A2[:, :, v], gv[:, 2, :], ALU.mult)
            k.tt("dve", G1c[:, :, v], mod[:, 16:24, v], gv[:, 1, :], ALU.mult)
            k.tt("dve", G2c[:, :, v], mod[:, 40:48, v], gv[:, 3, :], ALU.mult)
        C.mod, C.A1, C.A2 = mod, A1, A2
        C.G1c, C.G2c = G1c, G2c
        with Scope(k):
            C.G1b = make_gate_tile(C, G1c, "G1b")
            dbg_tap(C, "G1b%d" % l, C.G1b, [128, 2, D])
            mixer(C, l)
        if "f" not in DEVFLAGS:
            with Scope(k):
                C.G2b = make_gate_tile(C, G2c, "G2b")
                if "D" in DEVFLAGS:
                    ffn_phase(C, l)
                else:
                    ffn_phase_gather(C, l)


def make_gate_tile(C, Gc, name):
    k = C.k
    Gb = k.sb(name, [128, 2, D], F32)
    with Scope(k):
        dg = [k.sb("dg%d" % i, [128, 128], F32) for i in range(2)]
        n = 0
        for v in range(2):
            for half in range(2):
                pb = C.ps[1 + (n % 2)]
                for kk in range(4):
                    kc = half * 4 + kk
                    d = dg[kc % 2]
                    k.ts("dve", d, C.ident_f, Gc[:, kc:kc + 1, v], ALU.mult)
                    k.mm(pb[:, kk * 128:(kk + 1) * 128], C.ones_f, d)
                k.copy("act", Gb[:, v, half * 512:(half + 1) * 512], pb)
                n += 1
    return Gb


def norm_to_hT(C, hT, A, Bc, tag, extra=None):
    k = C.k
    hT_t = k.split(hT, NT, lambda i: (slice(None), slice(None), slice(i * 128, (i + 1) * 128)))
    with Scope(k):
        xin = [k.sb("nx%d" % i, [128, D], F32) for i in range(3)]
        junk = k.sb("njunk", [128, D], BF16)
        xn = [k.sb("nxn%d" % i, [128, D], BF16) for i in range(3)]
        st = [k.sb("nst%d" % i, [128, 2], F32) for i in range(3)]

        def stA(i):
            x = xin[i % 3]
            s = st[i % 3]
            k.dma("sp" if i % 2 == 0 else "pool", [(x, C.xr[i])])
            k.act(junk, x, AF.Square, accum=s[:, 0:1])
            k.act(s[:, 1:2], s[:, 0:1], AF.Sqrt, bias=EPS, scale=1.0 / D)
            k.recip(s[:, 1:2], s[:, 1:2])
            k.ts("dve", xn[i % 3], x, s[:, 1:2], ALU.mult)
            if extra is not None:
                extra(i, x, s[:, 1:2])

        def stB(i):
            v = 1 if i < 2 else 0
            xb = xn[i % 3]
            for hh in range(2):
                pb = C.ps[2 + 2 * (i % 2) + hh]
                pt = pb.v(pb.ap.bitcast(BF16)[:, 0:512].rearrange("p (k t) -> p k t", t=128))
                for kk in range(4):
                    kc = hh * 4 + kk
                    k.tr(pt[:, kk, :], xb[:, kc * 128:(kc + 1) * 128], C.ident_b)
                for kk in range(4):
                    kc = hh * 4 + kk
                    dst = hT_t[i][:, kc, :]
                    if hh == 0:
                        k.act(dst, pt[:, kk, :], AF.Identity, bias=Bc[:, kc:kc + 1, v], scale=A[:, kc:kc + 1, v])
                    else:
                        k.ts("dve", dst, pt[:, kk, :], A[:, kc:kc + 1, v], ALU.mult, Bc[:, kc:kc + 1, v], ALU.add)
        stA(0)
        for i in range(NT):
            if i + 1 < NT:
                stA(i + 1)
            stB(i)


def mixer(C, l):
    k = C.k
    yT = None
    with Scope(k):
        hT = k.sb("hT", [128, 8, T], BF16)
        norm_to_hT(C, hT, C.A1, C.mod[:, 0:8, :], "mix")
        dbg_tap(C, "hT%d" % l, hT, [128, 8, T], BF16)
        if "m" not in DEVFLAGS:
            mla_branch(C, l, hT, C.otd)
        if "n" not in DEVFLAGS:
            na_branch(C, l, hT, C.otd)
        if "s" not in DEVFLAGS:
            s5_branch(C, l, hT, C.otd)
        if "d" not in DEVFLAGS:
            m2_branch(C, l, hT, C.otd)
        if "g" not in DEVFLAGS:
            merge_phase(C, l, hT, yT)
    if "g" not in DEVFLAGS:
        outproj_phase(C, l, yT)


NFM = 19 * 128


def _rot_perm(n):
    p = np.arange(n)
    g, i = p // 16, p % 16
    return g * 16 + np.where(i < 8, i + 8, i - 8)


def host_mixer_inputs(inp, l):
    o = {}
    w = inp["w_in"][l]
    z = np.zeros
    ch = [w[:, 0:128], w[:, 128:256], w[:, 256:384]]
    kr = w[:, 384:416]
    ch.append(np.concatenate([z((D, 64), np.float32), kr, z((D, 32), np.float32)], 1))
    ch.append(np.concatenate([z((D, 64), np.float32), kr[:, _rot_perm(32)], z((D, 32), np.float32)], 1))
    ch += [w[:, 416:544], w[:, 544:672]]
    for h in range(4):
        kh = w[:, 672 + h * 64: 672 + (h + 1) * 64]
        ch.append(np.concatenate([kh, z((D, 64), np.float32)] if h % 2 == 0 else [z((D, 64), np.float32), kh], 1))
    ch += [w[:, 1184:1312], w[:, 1312:1440]]
    ch += [w[:, 1696 + i * 128: 1696 + (i + 1) * 128] for i in range(6)]
    o["wfm"] = np.ascontiguousarray(np.concatenate(ch, 1))
    o["wtm"] = np.ascontiguousarray(np.concatenate([w[:, 928:1184], w[:, 1440:1696], w[:, 2464:2472]], 1))
    wuq = inp["mla_w_uq"][l].reshape(256, 4, 96)
    q = np.zeros((256, 4, 2, 128), np.float32)
    q[:, :, 0, 0:96] = wuq
    q[:, :, 1, 64:96] = wuq[:, :, 64:96][:, :, _rot_perm(32)]
    o["wuq"] = q.reshape(256, 1024)
    wukv = inp["mla_w_ukv"][l].reshape(128, 4, 128)
    kv = np.zeros((128, 768), np.float32)
    for h in range(4):
        kv[:, h * 128: h * 128 + 64] = wukv[:, h, 0:64]
        kv[:, 512 + h * 64: 512 + (h + 1) * 64] = wukv[:, h, 64:128]
    o["wukv"] = kv
    o["gmla"] = np.ascontiguousarray(np.concatenate([col_layout(inp["mla_g_cq"][l]), col_layout(inp["mla_g_ckv"][l])], 1))
    o["nab"] = na_bias_tables(inp["na_rpb"][l])
    return o


def na_variant(m):
    return m if m < 3 else (3 if m <= 13 else m - 10)


def na_kb(m):
    return min(int(np.clip(2 * m - 4, 0, 24)), 22)


def na_bias_tables(rpb):
    out = np.full((6, 4, 5, 128, 128), NEG, np.float32)
    for m in (0, 1, 2, 3, 14, 15):
        var = na_variant(m)
        kb = na_kb(m)
        q = np.arange(128)
        r = 2 * m + q // 64
        qc = q % 64
        rs = np.clip(r - 4, 0, 24)
        cs = np.clip(qc - 8, 0, 48)
        j = np.arange(640)
        krow = kb + j // 64
        kcol = j % 64
        inwin = ((krow[:, None] >= rs[None, :]) & (krow[:, None] < rs[None, :] + 8) &
                 (kcol[:, None] >= cs[None, :]) & (kcol[:, None] < cs[None, :] + 16))
        ro = np.clip(krow[:, None] - r[None, :] + 7, 0, 14)
        co = np.clip(kcol[:, None] - qc[None, :] + 15, 0, 30)
        for h in range(4):
            vals = rpb[h][ro, co]
            out[var, h] = np.where(inwin, vals, NEG).astype(np.float32).reshape(5, 128, 128)
    return np.ascontiguousarray(out.transpose(0, 1, 3, 2, 4))


def rope_tables():
    import ml_dtypes
    cosf = np.ones((128, T), np.float32)
    sinf = np.zeros((128, T), np.float32)
    pos = np.arange(SEQ)
    row, col = pos // GRID_W, pos % GRID_W
    inv = 1.0 / (10000.0 ** (np.arange(0, 16, 2, dtype=np.float32) / 16))
    for i in range(32):
        p = row if i < 16 else col
        ang = p.astype(np.float32) * inv[(i % 16) % 8]
        cosf[64 + i, CTX:] = np.cos(ang)
        sinf[64 + i, CTX:] = np.sin(ang)
    return cosf.astype(ml_dtypes.bfloat16), sinf.astype(ml_dtypes.bfloat16)


TBLK = [(0, 512), (512, 512), (1024, 512), (1536, 512), (2048, 256)]
TBLK_LAT = [(256, 512), (768, 512), (1280, 512), (1792, 512)]


class Stager:
    def __init__(self, C, cols, n=3):
        self.k = C.k

    def load(self, dst, src, kdim=8):
        self.k.dma("pool", [(dst, src.v(src.ap.rearrange("(k p) n -> p k n", p=128)))])


def proj_fm(C, hT, w, t0, n, outp):
    for kk in range(8):
        C.k.mm(outp[:, 0:n], w[:, kk, :], hT[:, kk, t0:t0 + n], start=(kk == 0), stop=(kk == 7))


def rstd_bcast(C, dst, ss_psum, n, dim, tmp):
    k = C.k
    k.act(tmp[:, 0:n], ss_psum[:, 0:n], AF.Sqrt, bias=EPS, scale=1.0 / dim)
    k.recip(dst[:, 0:n], tmp[:, 0:n])


def attn_norm_store(C, po, n, h, dst, rec):
    k = C.k
    if h % 2 == 0:
        k.recip(rec[0:64, 0:n], po[64:128, 0:n])
        k.tt("dve", dst[0:64], po[0:64, 0:n], rec[0:64, 0:n], ALU.mult)
    else:
        k.recip(rec[64:128, 0:n], po[0:64, 0:n])
        k.tt("dve", dst[64:128], po[64:128, 0:n], rec[64:128, 0:n], ALU.mult)


def mla_branch(C, l, hT, otd):
    k = C.k
    Lw = C.L[l]
    scale = 96.0 ** -0.5
    with Scope(k):
        QT = [k.sb("QT%d" % h, [128, T], BF16) for h in range(4)]
        KT = [k.sb("KT%d" % h, [128, T], BF16) for h in range(4)]
        Vaug = k.sb("Vaug", [128, NT, 4, 128], BF16)
        cosf = k.sb("cosf", [128, T], BF16)
        sinf = k.sb("sinf", [128, T], BF16)
        if "A" not in DEVFLAGS:
            k.dma("sp", [(cosf, C.cosf_in), (sinf, C.sinf_in)])
        if "B" not in DEVFLAGS:
            k.memset("pool", Vaug, 1.0)
        with Scope(k):
            wm = k.sb("wm", [128, 8, 640], BF16)
            wuq = k.sb("wuq", [128, 2, 1024], BF16)
            wukv = k.sb("wukv", [128, 1, 768], BF16)
            gm = k.sb("gmla", [128, 3], F32)
            k.dma("sp", [(gm, Lw.gmla)])
            stg = Stager(C, 2048, n=3)
            for j in range(5):
                stg.load(wm[:, :, j * 128:(j + 1) * 128], Lw.wfm[:, j * 128:(j + 1) * 128])
            stg.load(wuq, Lw.wuq, kdim=2)
            stg.load(wukv, Lw.wukv, kdim=1)
            for c in range(2):
                k.ts("dve", wuq[:, c, :], wuq[:, c, :], gm[:, c:c + 1], ALU.mult)
            k.ts("dve", wukv[:, 0, :], wukv[:, 0, :], gm[:, 2:3], ALU.mult)
            v1 = wm.v(wm.ap[:, :, 4 * 128 + 64:4 * 128 + 96].rearrange("p k (g s) -> p k g s", s=16)[:, :, :, 0:8])
            if "C" not in DEVFLAGS:
                k.ts("dve", v1, v1, -1.0, ALU.mult)
            for c in range(2 if "D" not in DEVFLAGS else 0):
                v2 = wuq.v(wuq.ap[:, c, :].rearrange("p (h m x) -> p h m x", h=4, m=2)[:, :, 1, 64:96]
                           .rearrange("p h (g s) -> p h g s", s=16)[:, :, :, 0:8])
                k.ts("dve", v2, v2, -1.0, ALU.mult)
            if DEVSTOP == "mla_w":
                dbg_tap(C, "wm%d" % l, wm, [128, 8, 640], BF16)
                dbg_tap(C, "wuq%d" % l, wuq, [128, 2, 1024], BF16)
                return
            cq = [k.sb("cqb%d" % i, [128, 2, 512], BF16) for i in range(2)]
            sq = [k.sb("sqb%d" % i, [128, 2, 512], BF16) for i in range(2)]
            ckv = [k.sb("ckvb%d" % i, [128, 512], BF16) for i in range(2)]
            skv = [k.sb("skvb%d" % i, [128, 512], BF16) for i in range(2)]
            rq = [k.sb("rq%d" % i, [128, 512], F32) for i in range(2)]
            rkv = [k.sb("rkv%d" % i, [128, 512], F32) for i in range(2)]
            tmp = [k.sb("mt%d" % i, [128, 512], F32) for i in range(4)]
            krr = [k.sb("krr%d" % i, [128, 512], BF16) for i in range(2)]
            rc = [k.sb("rc%d" % i, [128, 2], F32) for i in range(4)]
            P = C.ps
            for bi, (t0, n) in enumerate(TBLK):
                cqb, sqb, ckvb, skvb, rqb, rkvb = cq[bi % 2], sq[bi % 2], ckv[bi % 2], skv[bi % 2], rq[bi % 2], rkv[bi % 2]
                for c in range(2):
                    if 'z' not in DEVFLAGS:
                        proj_fm(C, hT, wm[:, :, c * 128:(c + 1) * 128], t0, n, P[c])
                    if 'x' not in DEVFLAGS:
                        k.copy("dve", cqb[:, c, 0:n], P[c][:, 0:n])
                    if 'y' not in DEVFLAGS:
                        k.act(sqb[:, c, 0:n], P[c][:, 0:n], AF.Square)
                if '1' in DEVFLAGS:
                    continue
                for c in range(2):
                    k.mm(P[2][:, 0:n], C.ones_b, sqb[:, c, 0:n], start=(c == 0), stop=(c == 1))
                rstd_bcast(C, rqb, P[2], n, 256, tmp[0])
                if '2' in DEVFLAGS:
                    continue
                proj_fm(C, hT, wm[:, :, 256:384], t0, n, P[3])
                k.copy("dve", ckvb[:, 0:n], P[3][:, 0:n])
                k.act(skvb[:, 0:n], P[3][:, 0:n], AF.Square)
                k.mm(P[2][:, 0:n], C.ones_b, skvb[:, 0:n])
                rstd_bcast(C, rkvb, P[2], n, 128, tmp[0])
                if '3' in DEVFLAGS:
                    continue
                proj_fm(C, hT, wm[:, :, 384:512], t0, n, P[0])
                proj_fm(C, hT, wm[:, :, 512:640], t0, n, P[1])
                k.tt("dve", tmp[1][:, 0:n], P[0][:, 0:n], cosf[:, t0:t0 + n], ALU.mult)
                k.tt("dve", tmp[2][:, 0:n], P[1][:, 0:n], sinf[:, t0:t0 + n], ALU.mult)
                kb = krr[bi % 2]
                k.tt("pool", kb[:, 0:n], tmp[1][:, 0:n], tmp[2][:, 0:n], ALU.add)
                for h in range(4 if 'H' not in DEVFLAGS else 0):
                    pm, pr = P[4 + (h % 2) * 2], P[5 + (h % 2) * 2]
                    for c in range(2):
                        k.mm(pm[:, 0:n], wuq[:, c, h * 256:h * 256 + 128], cqb[:, c, 0:n], start=(c == 0), stop=(c == 1))
                    for c in range(2):
                        k.mm(pr[:, 0:n], wuq[:, c, h * 256 + 128:h * 256 + 256], cqb[:, c, 0:n], start=(c == 0), stop=(c == 1))
                    ta, tb = tmp[(h % 2) * 2], tmp[(h % 2) * 2 + 1]
                    k.tt("dve", ta[:, 0:n], pm[:, 0:n], cosf[:, t0:t0 + n], ALU.mult)
                    k.tt("dve", tb[:, 0:n], pr[:, 0:n], sinf[:, t0:t0 + n], ALU.mult)
                    k.tt("pool", ta[:, 0:n], ta[:, 0:n], tb[:, 0:n], ALU.add)
                    k.tt("pool", QT[h][:, t0:t0 + n], ta[:, 0:n], rqb[:, 0:n], ALU.mult)
                for h in range(4 if 'G' not in DEVFLAGS else 0):
                    pk = P[h % 2]
                    k.mm(pk[:, 0:n], wukv[:, 0, h * 128:(h + 1) * 128], ckvb[:, 0:n])
                    k.tt("dve", KT[h][0:64, t0:t0 + n], pk[0:64, 0:n], rkvb[0:64, 0:n], ALU.mult)
                    k.copy("pool", KT[h][64:128, t0:t0 + n], kb[64:128, 0:n])
                for j in range(n // 128 if 'F' not in DEVFLAGS else 0):
                    ti = t0 // 128 + j
                    pv, pss = P[2 + (j % 2)], P[4 + (j % 2)]
                    k.mm(pv[:, 0:256], ckvb[:, j * 128:(j + 1) * 128], wukv[:, 0, 512:768])
                    k.mm(pss[:, 0:1], skvb[:, j * 128:(j + 1) * 128], C.ones_b[:, 0:1])
                    r = rc[ti % 4]
                    k.act(r[:, 0:1], pss[:, 0:1], AF.Sqrt, bias=EPS, scale=1.0 / 128)
                    k.recip(r[:, 1:2], r[:, 0:1])
                    pvv = pv.v(pv.ap[:, 0:256].rearrange("p (h d) -> p h d", d=64))
                    k.ts("dve", Vaug[:, ti, 0:4:2, 0:64], pvv[:, 0:4:2, :], r[:, 1:2], ALU.mult)
                    k.ts("dve", Vaug[:, ti, 1:4:2, 64:128], pvv[:, 1:4:2, :], r[:, 1:2], ALU.mult)
        dbg_tap(C, "QT0_%d" % l, QT[0], [128, T], BF16)
        dbg_tap(C, "KT0_%d" % l, KT[0], [128, T], BF16)
        dbg_tap(C, "Vaug%d" % l, Vaug, [128, NT, 4, 128], BF16)
        if DEVSTOP == "mla_proj":
            return
        with Scope(k):
            OT = k.sb("OTmla", [128, 2, T], BF16)
            PT = [k.sb("PT%d" % i, [128, 512], BF16) for i in range(3)]
            rec = [k.sb("rec%d" % i, [128, 512], F32) for i in range(2)]
            P = C.ps
            qblocks = [(256 + 512 * i, 512, list(range(NT))) for i in range(4)] + [(0, 256, [0, 1])]
            steps = []
            nb = 0
            for h in range(4):
                for (q0, n, kts) in qblocks:
                    for ki, kt in enumerate(kts):
                        steps.append((h, q0, n, kt, ki == 0, ki == len(kts) - 1, nb))
                    nb += 1
            NS = 4
            PT = PT + [k.sb("PT3", [128, 512], BF16)]

            def qk(i):
                h, q0, n, kt, first, last, nbi = steps[i]
                k.mm(P[i % NS][:, 0:n], KT[h][:, kt * 128:(kt + 1) * 128], QT[h][:, q0:q0 + n])
                k.act(PT[i % NS][:, 0:n], P[i % NS][:, 0:n], AF.Exp, scale=scale)

            def pv(i):
                h, q0, n, kt, first, last, nbi = steps[i]
                po = P[4 + (nbi % 2)]
                k.mm(po[:, 0:n], Vaug[:, kt, h, :], PT[i % NS][:, 0:n], start=first, stop=last)
                if last:
                    attn_norm_store(C, po, n, h, OT[:, h // 2, q0:q0 + n], rec[nbi % 2])
            LOOK = 2
            for i in range(min(LOOK, len(steps))):
                qk(i)
            for i in range(len(steps)):
                if i + LOOK < len(steps):
                    qk(i + LOOK)
                pv(i)
            dbg_tap(C, "OTmla%d" % l, OT, [128, 2, T], BF16)
            k.dma("sp", [(otd[0:2].v(otd.ap[0:2].rearrange("c p t -> p c t")), OT)])


def na_branch(C, l, hT, otd):
    k = C.k
    Lw = C.L[l]
    scale = 0.125
    P = C.ps
    with Scope(k):
        QnT = k.sb("QnT", [128, 2, T], BF16)
        KmT = [k.sb("KmT%d" % h, [128, T], BF16) for h in range(4)]
        Vaug = k.sb("VaugN", [128, NT, 4, 128], BF16)
        k.memset("pool", Vaug, 1.0)
        with Scope(k):
            wq = k.sb("wq", [128, 8, 256], BF16)
            wk = k.sb("wk", [128, 8, 512], BF16)
            wv = k.sb("wv", [128, 8, 256], BF16)
            stg = Stager(C, 2048, n=3)
            for j in range(2):
                stg.load(wq[:, :, j * 128:(j + 1) * 128], Lw.wfm[:, (5 + j) * 128:(6 + j) * 128])
            for j in range(4):
                stg.load(wk[:, :, j * 128:(j + 1) * 128], Lw.wfm[:, (7 + j) * 128:(8 + j) * 128])
            stg.load(wv, Lw.wtm[:, 0:256])
            for bi, (t0, n) in enumerate(TBLK):
                for c in range(2):
                    proj_fm(C, hT, wq[:, :, c * 128:(c + 1) * 128], t0, n, P[c])
                    k.copy("dve" if c == 0 else "act", QnT[:, c, t0:t0 + n], P[c][:, 0:n])
                for h in range(4):
                    proj_fm(C, hT, wk[:, :, h * 128:(h + 1) * 128], t0, n, P[2 + h])
                    k.copy("dve" if h % 2 == 0 else "act", KmT[h][:, t0:t0 + n], P[2 + h][:, 0:n])
                for j in range(n // 128):
                    ti = t0 // 128 + j
                    pv = P[6 + (j % 2)]
                    for kk in range(8):
                        k.mm(pv[:, 0:256], hT[:, kk, ti * 128:(ti + 1) * 128], wv[:, kk, :], start=(kk == 0), stop=(kk == 7))
                    pvv = pv.v(pv.ap[:, 0:256].rearrange("p (h d) -> p h d", d=64))
                    k.copy("dve", Vaug[:, ti, 0:4:2, 0:64], pvv[:, 0:4:2, :])
                    k.copy("dve", Vaug[:, ti, 1:4:2, 64:128], pvv[:, 1:4:2, :])
        with Scope(k):
            OT = k.sb("OTna", [128, 2, T], BF16)
            nb = [k.sb("nab%d" % i, [128, 5, 128], F32) for i in range(2)]
            nbb = [k.sb("nabb%d" % i, [128, 5, 128], BF16) for i in range(2)]
            PT = [k.sb("nPT%d" % i, [128, 896], BF16) for i in range(2)]
            rec = [k.sb("nrec%d" % i, [128, 256], F32) for i in range(2)]
            items = []
            for h in range(4):
                for m in range(16):
                    items.append((h, m))
                items.append((h, -1))
            state = {"var": None, "nload": 0, "bt": None}

            def stageA(i):
                h, m = items[i]
                s = i % 2
                pa, pb = P[3 * s], P[3 * s + 1]
                pt = PT[s]
                if m < 0:
                    q = QnT[:, h // 2, 0:CTX]
                    for j in range(2):
                        k.mm(pa[:, j * 256:(j + 1) * 256], KmT[h][:, j * 128:(j + 1) * 128], q)
                    k.act(pt[:, 0:512], pa, AF.Exp, scale=scale)
                    return
                var = na_variant(m)
                if (h, var) != state["var"]:
                    bt32 = nb[state["nload"] % 2]
                    state["bt"] = nbb[state["nload"] % 2]
                    state["nload"] += 1
                    k.dma("sp", [(bt32, Lw.nab[var, h])])
                    k.act(state["bt"], bt32, AF.Copy, scale=1.0 / scale)
                    state["var"] = (h, var)
                bt = state["bt"]
                q0 = CTX + m * 128
                kt0 = 2 + na_kb(m) // 2
                q = QnT[:, h // 2, q0:q0 + 128]
                for j in range(4):
                    k.mm(pa[:, j * 128:(j + 1) * 128], KmT[h][:, (kt0 + j) * 128:(kt0 + j + 1) * 128], q, start=True, stop=False)
                    k.mm(pa[:, j * 128:(j + 1) * 128], C.ident_b, bt[:, j, :], start=False, stop=True)
                k.mm(pb[:, 0:128], KmT[h][:, (kt0 + 4) * 128:(kt0 + 5) * 128], q, start=True, stop=False)
                k.mm(pb[:, 0:128], C.ident_b, bt[:, 4, :], start=False, stop=True)
                for j in range(2):
                    k.mm(pb[:, 128 + j * 128:256 + j * 128], KmT[h][:, j * 128:(j + 1) * 128], q)
                k.act(pt[:, 0:512], pa, AF.Exp, scale=scale)
                k.act(pt[:, 512:896], pb[:, 0:384], AF.Exp, scale=scale)

            def stageB(i):
                h, m = items[i]
                s = i % 2
                po = P[3 * s + 2]
                pt = PT[s]
                if m < 0:
                    for j in range(2):
                        k.mm(po[:, 0:256], Vaug[:, j, h, :], pt[:, j * 256:(j + 1) * 256], start=(j == 0), stop=(j == 1))
                    attn_norm_store(C, po, 256, h, OT[:, h // 2, 0:CTX], rec[s])
                    return
                q0 = CTX + m * 128
                kt0 = 2 + na_kb(m) // 2
                kts = [kt0 + j for j in range(5)] + [0, 1]
                for j, kt in enumerate(kts):
                    k.mm(po[:, 0:128], Vaug[:, kt, h, :], pt[:, j * 128:(j + 1) * 128], start=(j == 0), stop=(j == 6))
                attn_norm_store(C, po, 128, h, OT[:, h // 2, q0:q0 + 128], rec[s])
            stageA(0)
            for i in range(len(items)):
                if i + 1 < len(items):
                    stageA(i + 1)
                stageB(i)
            dbg_tap(C, "OTna%d" % l, OT, [128, 2, T], BF16)
            k.dma("sp", [(otd[2:4].v(otd.ap[2:4].rearrange("c p t -> p c t")), OT)])


def host_s5_inputs(inp, l):
    o = {}
    ls = np.repeat(inp["s5_log_step"][l][:, :, None], 64, axis=2)
    o["s5par"] = np.ascontiguousarray(np.stack([inp["s5_a_re"][l].reshape(-1), inp["s5_a_im"][l].reshape(-1),
                                                ls.reshape(-1)], 0).astype(np.float32))
    BT = np.zeros((128, 2, 8, 2, 128), np.float32)
    CT = np.zeros((128, 2, 8, 2, 128), np.float32)
    for d in range(2):
        for g in range(16):
            s, gj = g // 2, g % 2
            r0 = (s % 4) * 32 + gj * 16
            for x, (bn, cn) in enumerate((("s5_b_re", "s5_c_re"), ("s5_b_im", "s5_c_im"))):
                BT[r0:r0 + 16, d, s, x, gj * 64:(gj + 1) * 64] = inp[bn][l][d, g].T
                CT[gj * 64:(gj + 1) * 64, d, s, x, r0:r0 + 16] = inp[cn][l][d, g].T
    o["s5BT"] = BT
    o["s5CT"] = CT
    o["s5vec"] = np.ascontiguousarray(np.stack([col_layout(inp["s5_d"][l]), col_layout(inp["s5_b_glu"][l])], -1))
    o["s5wglu"] = np.ascontiguousarray(inp["s5_w_glu"][l])
    o["s5wu"] = np.ascontiguousarray(inp["w_in"][l][:, 1184:1440])
    return o


S5_ORDER = [list(range(NT)), [1, 0] + list(range(NT - 1, 1, -1))]


def bc(tv, shape, axis):
    return tv.v(tv.ap.unsqueeze(axis).to_broadcast(list(shape)))


def s5_branch(C, l, hT, otd):
    k = C.k
    Lw = C.L[l]
    P = C.ps
    HALF_PI = float(np.pi / 2)
    with Scope(k):
        BbT = k.sb("s5BbT", [128, 2, 8, 2, 128], BF16)
        CTb = k.sb("s5CTb", [128, 2, 8, 2, 128], BF16)
        cos16 = k.sb("s5cos16", [128, 16, 128], BF16)
        sin16 = k.sb("s5sin16", [128, 16, 128], BF16)
        rhob = k.sb("s5rhob", [128, 16, 128], F32)
        eL = k.sb("s5eL", [128, 16, 2], F32)
        vec = k.sb("s5vec", [128, 2, 2], F32)
        wglu = k.sb("s5wglu", [128, 2, 256], BF16)
        wu = k.sb("s5wu", [128, 8, 256], BF16)
        Jm = k.sb("s5J", [128, 128], BF16)
        k.dma("sp", [(vec, Lw.s5vec)])
        with Scope(k):
            stg = Stager(C, 2048, n=2)
            stg.load(wu, Lw.s5wu)
            stg.load(wglu, Lw.s5wglu, kdim=2)
            jf = k.sb("jf", [128, 128], F32)
            k.dma("sp", [(jf, C.jmat_in)])
            k.copy("dve", Jm, jf)
        with Scope(k):
            cosT = k.sb("s5cosT", [128, 16, 128], F32)
            sinT = k.sb("s5sinT", [128, 16, 128], F32)
            par = k.sb("s5par", [128, 3, 2048], F32)
            k.dma("sp", [(par, Lw.s5par.v(Lw.s5par.ap.partition_broadcast(128)))])
            are, aim, ls = par[:, 0, :], par[:, 1, :], par[:, 2, :]
            W = [k.sb("s5w%d" % i, [128, 2048], F32) for i in range(7)]
            step, rho, cth, sth, t1, t2, t3 = W
            k.act(step, ls, AF.Exp)
            k.tt("dve", t1, step, are, ALU.mult)
            k.act(rho, t1, AF.Exp)
            k.tt("dve", t1, step, aim, ALU.mult)
            k.act(sth, t1, AF.Sin, scale=1.0 / 16)
            k.act(cth, t1, AF.Sin, scale=1.0 / 16, bias=HALF_PI)
            for _ in range(4):
                k.act(t1, cth, AF.Square)
                k.act(t2, sth, AF.Square)
                k.tt("dve", t3, cth, sth, ALU.mult)
                k.tt("dve", cth, t1, t2, ALU.subtract)
                k.act(sth, t3, AF.Copy, scale=2.0)
            diag = k.sb("s5diag", [128, 16, 3], F32)
            big = k.sb("s5big", [128, 16, 128], F32)
            for i, src in enumerate((rho, cth, sth)):
                sv = src.v(src.ap.rearrange("p (a m) -> p a m", m=128))
                k.tt("dve", big, sv, bc(C.ident_f, [128, 16, 128], 1), ALU.mult)
                k.ins("dve", lambda e, o=diag[:, :, i], b=big: e.tensor_reduce(out=o.ap, in_=b.ap, axis=AX.X, op=ALU.add),
                      [big], [diag])
            nr, ni = t1, t2
            k.tt("dve", nr, rho, cth, ALU.mult)
            k.ts("dve", nr, nr, -1.0, ALU.add)
            k.tt("pool", ni, rho, sth, ALU.mult)
            den = t3
            k.act(den, are, AF.Square)
            k.act(step, aim, AF.Square)
            k.tt("dve", den, den, step, ALU.add)
            k.recip(den, den)
            cr, ci = cth, sth
            k.tt("dve", cr, nr, are, ALU.mult)
            k.tt("pool", step, ni, aim, ALU.mult)
            k.tt("dve", cr, cr, step, ALU.add)
            k.tt("dve", cr, cr, den, ALU.mult)
            k.tt("dve", ci, ni, are, ALU.mult)
            k.tt("pool", step, nr, aim, ALU.mult)
            k.tt("dve", ci, ci, step, ALU.subtract)
            k.tt("dve", ci, ci, den, ALU.mult)
            btf = k.sb("s5btf", [128, 2, 8, 2, 128], F32)
            k.dma("sp", [(btf, Lw.s5BT)])
            crv = cr.v(cr.ap.rearrange("p (d s m) -> p d s m", d=2, s=8))
            civ = ci.v(ci.ap.rearrange("p (d s m) -> p d s m", d=2, s=8))
            tb1 = rho.v(rho.ap.rearrange("p (d s m) -> p d s m", d=2, s=8))
            tb2 = step.v(step.ap.rearrange("p (d s m) -> p d s m", d=2, s=8))
            k.tt("dve", tb1, crv, btf[:, :, :, 0, :], ALU.mult)
            k.tt("pool", tb2, civ, btf[:, :, :, 1, :], ALU.mult)
            k.tt("dve", BbT[:, :, :, 0, :], tb1, tb2, ALU.subtract)
            k.tt("dve", tb1, crv, btf[:, :, :, 1, :], ALU.mult)
            k.tt("pool", tb2, civ, btf[:, :, :, 0, :], ALU.mult)
            k.tt("dve", BbT[:, :, :, 1, :], tb1, tb2, ALU.add)
            k.dma("sp", [(btf, Lw.s5CT)])
            k.copy("dve", CTb[:, :, :, 0, :], btf[:, :, :, 0, :])
            k.ts("dve", CTb[:, :, :, 1, :], btf[:, :, :, 1, :], -1.0, ALU.mult)
            ck = k.sb("s5ck", [128, 16], F32)
            sk = k.sb("s5sk", [128, 16], F32)
            ta = k.sb("s5ta", [128, 16], F32)
            tb_ = k.sb("s5tb", [128, 16], F32)
            k.copy("dve", ck, diag[:, :, 1])
            k.copy("dve", sk, diag[:, :, 2])
            k.memset("dve", cosT[:, :, 0:1], 1.0)
            k.memset("dve", sinT[:, :, 0:1], 0.0)
            big2 = big[:, :, 0:64]
            big3 = big[:, :, 64:128]
            for kk in range(8):
                w = 1 << kk
                if kk < 7:
                    ckb = bc(ck, [128, 16, w], 2)
                    skb = bc(sk, [128, 16, w], 2)
                    k.tt("dve", big2[:, :, 0:w], cosT[:, :, 0:w], ckb, ALU.mult)
                    k.tt("dve", big3[:, :, 0:w], sinT[:, :, 0:w], skb, ALU.mult)
                    k.tt("dve", cosT[:, :, w:2 * w], big2[:, :, 0:w], big3[:, :, 0:w], ALU.subtract)
                    k.tt("dve", big2[:, :, 0:w], cosT[:, :, 0:w], skb, ALU.mult)
                    k.tt("dve", big3[:, :, 0:w], sinT[:, :, 0:w], ckb, ALU.mult)
                    k.tt("dve", sinT[:, :, w:2 * w], big2[:, :, 0:w], big3[:, :, 0:w], ALU.add)
                    k.tt("dve", ta, ck, ck, ALU.mult)
                    k.tt("dve", tb_, sk, sk, ALU.mult)
                    k.tt("dve", sk, ck, sk, ALU.mult)
                    k.ts("dve", sk, sk, 2.0, ALU.mult)
                    k.tt("dve", ck, ta, tb_, ALU.subtract)
                else:
                    k.copy("dve", eL[:, :, 0], ck)
                    k.copy("dve", eL[:, :, 1], sk)
            k.copy("dve", rhob, bc(diag[:, :, 0], [128, 16, 128], 2))
            k.copy("act", cos16, cosT)
            k.copy("act", sin16, sinT)
        dbg_tap(C, "s5rhob%d" % l, rhob, [128, 16, 128])
        dbg_tap(C, "s5BbT%d" % l, BbT, [128, 2, 8, 2, 128], BF16)
        if DEVSTOP == "s5_par":
            return
        uproc = [k.sb("s5up%d" % d, [128, 2, T], BF16) for d in range(2)]
        unat = k.sb("s5un", [128, 2, T], F32)
        pos = [{c: i for i, c in enumerate(S5_ORDER[d])} for d in range(2)]
        with Scope(k):
            ut = [k.sb("s5ut%d" % i, [128, 256], BF16) for i in range(2)]
            for c in range(NT):
                pu = P[c % 2]
                for kk in range(8):
                    k.mm(pu[:, 0:256], hT[:, kk, c * 128:(c + 1) * 128], wu[:, kk, :], start=(kk == 0), stop=(kk == 7))
                u = ut[c % 2]
                k.copy("act", u, pu[:, 0:256])
                pf = P[2 + (c % 2) * 2]
                pr = P[3 + (c % 2) * 2]
                for q in range(2):
                    k.mm(pf[:, q * 128:(q + 1) * 128], u[:, q * 128:(q + 1) * 128], C.ident_b)
                    k.mm(pr[:, q * 128:(q + 1) * 128], u[:, q * 128:(q + 1) * 128], Jm)
                pfv = pf.v(pf.ap[:, 0:256].rearrange("p (q t) -> p q t", q=2))
                prv = pr.v(pr.ap[:, 0:256].rearrange("p (q t) -> p q t", q=2))
                k.copy("dve", uproc[0][:, :, c * 128:(c + 1) * 128], pfv)
                k.copy("act", unat[:, :, c * 128:(c + 1) * 128], pfv)
                i1 = pos[1][c]
                k.copy("dve", uproc[1][:, :, i1 * 128:(i1 + 1) * 128], prv)
        dbg_tap(C, "s5up1_%d" % l, uproc[1], [128, 2, T], BF16)
        yf = k.sb("s5yf", [128, 2, T], F32)
        with Scope(k):
            BQ = k.sb("s5BQ", [128, 8, 2, 512], F32)
            bq = [[None, None] for _ in range(8)]
            subs = k.split(BQ, 16, lambda i: (slice(None), i // 2, i % 2, slice(None)))
            for i in range(16):
                bq[i // 2][i % 2] = subs[i]
            G16 = k.sb("s5G16", [128, 8, 2, 512], BF16)
            g16 = [[None, None] for _ in range(8)]
            subs16 = k.split(G16, 16, lambda i: (slice(None), i // 2, i % 2, slice(None)))
            for i in range(16):
                g16[i // 2][i % 2] = subs16[i]
            tm = [k.sb("s5tm%d" % i, [128, 512], BF16) for i in range(8)]
            p16 = [k.sb("s5p16%d" % i, [128, 512], BF16) for i in range(4)]

            hre = [k.sb("s5hre%d" % i, [128, 512], BF16) for i in range(2)]
            him = [k.sb("s5him%d" % i, [128, 512], BF16) for i in range(2)]
            ini = [k.sb("s5ini%d" % i, [128, 8, 2], F32) for i in range(2)]
            tp_ = [k.sb("s5tp%d" % i, [128, 8], F32) for i in range(4)]
            ytr = [k.sb("s5ytr%d" % i, [128, 256], BF16) for i in range(2)]
            it = 0
            nchunk = 0
            for d in range(2):
                k.memset("pool", ini[nchunk % 2], 0.0)
                cL = eL[:, d * 8:(d + 1) * 8, 0]
                sL = eL[:, d * 8:(d + 1) * 8, 1]
                for bi, (t0, n) in enumerate(TBLK):
                    nch = n // 128

                    def v3(tv):
                        return tv.v(tv.ap[:, 0:n].rearrange("p (c j) -> p c j", j=128))
                    for s in range(8):
                        q = s // 4
                        sd = d * 8 + s
                        pre, pim = P[2 * (s % 2)], P[2 * (s % 2) + 1]
                        k.mm(pre[:, 0:n], BbT[:, d, s, 0, :], uproc[d][:, q, t0:t0 + n])
                        k.mm(pim[:, 0:n], BbT[:, d, s, 1, :], uproc[d][:, q, t0:t0 + n])
                        cb = bc(cos16[:, sd, :], [128, nch, 128], 1)
                        sb_ = bc(sin16[:, sd, :], [128, nch, 128], 1)
                        t = tm[(s % 2) * 4:(s % 2) * 4 + 4]
                        r16, i16 = p16[(s % 2) * 2], p16[(s % 2) * 2 + 1]
                        k.copy("act", r16[:, 0:n], pre[:, 0:n])
                        k.copy("act", i16[:, 0:n], pim[:, 0:n])
                        k.tt("dve", v3(t[0]), v3(r16), cb, ALU.mult)
                        k.tt("dve", v3(t[1]), v3(r16), sb_, ALU.mult)
                        k.tt("dve", v3(t[2]), v3(i16), sb_, ALU.mult)
                        k.tt("dve", v3(t[3]), v3(i16), cb, ALU.mult)
                        k.tt("pool", bq[s][0][:, 0:n], t[0][:, 0:n], t[2][:, 0:n], ALU.add)
                        k.tt("pool", bq[s][1][:, 0:n], t[3][:, 0:n], t[1][:, 0:n], ALU.subtract)
                    for j in range(nch):
                        cur, nxt = ini[nchunk % 2], ini[(nchunk + 1) % 2]
                        nchunk += 1
                        sl = slice(j * 128, (j + 1) * 128)
                        for s in range(8):
                            sd = d * 8 + s
                            for x in range(2):
                                k.scan(g16[s][x][:, sl], rhob[:, sd, :], bq[s][x][:, sl], cur[:, s, x:x + 1], ALU.mult, ALU.add)
                        last = j * 128 + 127
                        cr_ = G16[:, :, 0, last]
                        ci_ = G16[:, :, 1, last]
                        k.tt("pool", tp_[0], cr_, cL, ALU.mult)
                        k.tt("pool", tp_[1], ci_, sL, ALU.mult)
                        k.tt("pool", nxt[:, :, 0], tp_[0], tp_[1], ALU.subtract)
                        k.tt("pool", tp_[2], cr_, sL, ALU.mult)
                        k.tt("pool", tp_[3], ci_, cL, ALU.mult)
                        k.tt("pool", nxt[:, :, 1], tp_[2], tp_[3], ALU.add)
                    if d == 0:
                        py = [P[4], P[5]]
                    else:
                        py = [P[4 + j] for j in range(nch)]
                    for s in range(8):
                        q = s // 4
                        sd = d * 8 + s
                        b = it % 2
                        it += 1
                        cb = bc(cos16[:, sd, :], [128, nch, 128], 1)
                        sb_ = bc(sin16[:, sd, :], [128, nch, 128], 1)
                        t = tm[(s % 2) * 4:(s % 2) * 4 + 4]
                        k.tt("dve", v3(t[0]), v3(g16[s][0]), cb, ALU.mult)
                        k.tt("pool", v3(t[1]), v3(g16[s][1]), sb_, ALU.mult)
                        k.tt("dve", v3(t[2]), v3(g16[s][0]), sb_, ALU.mult)
                        k.tt("pool", v3(t[3]), v3(g16[s][1]), cb, ALU.mult)
                        k.tt("dve", hre[b][:, 0:n], t[0][:, 0:n], t[1][:, 0:n], ALU.subtract)
                        k.tt("dve", him[b][:, 0:n], t[2][:, 0:n], t[3][:, 0:n], ALU.add)
                        first, lastq = (s % 4 == 0), (s % 4 == 3)
                        if d == 0:
                            k.mm(py[q][:, 0:n], CTb[:, d, s, 0, :], hre[b][:, 0:n], start=first, stop=False)
                            k.mm(py[q][:, 0:n], CTb[:, d, s, 1, :], him[b][:, 0:n], start=False, stop=lastq)
                        else:
                            for j in range(nch):
                                sl = slice(j * 128, (j + 1) * 128)
                                k.mm(py[j][:, q * 128:(q + 1) * 128], hre[b][:, sl], CTb[:, d, s, 0, :], start=first, stop=False)
                                k.mm(py[j][:, q * 128:(q + 1) * 128], him[b][:, sl], CTb[:, d, s, 1, :], start=False, stop=lastq)
                        if lastq:
                            if d == 0:
                                k.copy("act", yf[:, q, t0:t0 + n], py[q][:, 0:n])
                            elif q == 1:
                                for j in range(nch):
                                    c = S5_ORDER[1][t0 // 128 + j]
                                    yt = ytr[j % 2]
                                    k.copy("act", yt, py[j][:, 0:256])
                                    pz = P[2 * (j % 2)]
                                    for qq in range(2):
                                        k.mm(pz[:, qq * 128:(qq + 1) * 128], yt[:, qq * 128:(qq + 1) * 128], Jm)
                                    pzv = pz.v(pz.ap[:, 0:256].rearrange("p (q t) -> p q t", q=2))
                                    ysl = yf[:, :, c * 128:(c + 1) * 128]
                                    k.tt("dve", ysl, ysl, pzv, ALU.add)
        for q in range(2):
            k.stt(unat[:, q, :], unat[:, q, :], vec[:, q, 0:1], yf[:, q, :], ALU.mult, ALU.add)
        dbg_tap(C, "s5y%d" % l, unat, [128, 2, T])
        with Scope(k):
            OT = k.sb("OTs5", [128, 2, T], BF16)
            zb = [k.sb("s5z%d" % i, [128, 2, 512], BF16) for i in range(2)]
            g1 = [k.sb("s5g%d" % i, [128, 512], F32) for i in range(4)]
            for bi, (t0, n) in enumerate(TBLK):
                z = zb[bi % 2]
                for q in range(2):
                    y = unat[:, q, t0:t0 + n]
                    a, b_ = g1[q * 2], g1[q * 2 + 1]
                    k.tt("pool", a[:, 0:n], y, y, ALU.mult)
                    k.ts("dve", a[:, 0:n], a[:, 0:n], 0.044715, ALU.mult, 1.0, ALU.add)
                    k.tt("dve", a[:, 0:n], a[:, 0:n], y, ALU.mult)
                    k.act(b_[:, 0:n], a[:, 0:n], AF.Sigmoid, scale=1.5957691216057308)
                    k.tt("dve", z[:, q, 0:n], y, b_[:, 0:n], ALU.mult)
                for q in range(2):
                    pg = P[q]
                    for c in range(2):
                        k.mm(pg[:, 0:n], wglu[:, c, q * 128:(q + 1) * 128], z[:, c, 0:n], start=(c == 0), stop=(c == 1))
                    sg = g1[q * 2]
                    k.act(sg[:, 0:n], pg[:, 0:n], AF.Sigmoid, bias=vec[:, q, 1:2])
                    k.tt("dve", OT[:, q, t0:t0 + n], z[:, q, 0:n], sg[:, 0:n], ALU.mult)
            dbg_tap(C, "OTs5%d" % l, OT, [128, 2, T], BF16)
            k.dma("sp", [(otd[4:6].v(otd.ap[4:6].rearrange("c p t -> p c t")), OT)])


def host_m2_inputs(inp, l):
    o = {}
    cw = inp["m2_conv_w"][l]
    v = np.zeros((128, 6, 5), np.float32)
    for kk in range(4):
        v[:, :, kk] = col_layout(cw[kk])
    v[:, :, 4] = col_layout(inp["m2_conv_b"][l])
    o["m2vec"] = v
    o["m2row"] = np.ascontiguousarray(np.concatenate([inp["m2_a_log"][l].reshape(-1), inp["m2_dt_bias"][l].reshape(-1),
                                                      inp["m2_d"][l].reshape(-1), inp["m2_g_norm"][l].reshape(-1)])[None, :].astype(np.float32))
    return o


def host_m2_masks():
    i = np.arange(128)
    le = (i[:, None] <= i[None, :]).astype(np.float32)
    ge = (i[:, None] >= i[None, :]).astype(np.float32)
    su = (i[:, None] > i[None, :]).astype(np.float32)
    sl = (i[:, None] < i[None, :]).astype(np.float32)
    return np.ascontiguousarray(np.stack([le, ge, su, sl], 0).transpose(1, 0, 2))


def m2_branch(C, l, hT, otd):
    k = C.k
    Lw = C.L[l]
    P = C.ps
    with Scope(k):
        xc = k.sb("m2xc", [128, 6, T], BF16)
        xtok = k.sb("m2xtok", [128, NT, 512], BF16)
        zs = k.sb("m2zs", [128, NT, 256], BF16)
        dt = k.sb("m2dt", [128, NT, 8], F32)
        dA = k.sb("m2dA", [128, NT, 8], F32)
        ysum = k.sb("m2ys", [128, NT, 256], F32)
        row = k.sb("m2row", [128, 276], F32)
        msk = k.sb("m2msk", [128, 4, 128], F32)
        mskb = k.sb("m2mskb", [128, 2, 128], BF16)
        k.dma("sp", [(row, Lw.m2row.v(Lw.m2row.ap.partition_broadcast(128))), (msk, C.m2msk_in)])
        k.copy("dve", mskb, msk[:, 0:2, :])
        LE, GE, SU, SL = msk[:, 0, :], msk[:, 1, :], msk[:, 2, :], msk[:, 3, :]
        aneg = k.sb("m2aneg", [128, 8], F32)
        k.act(aneg, row[:, 0:8], AF.Exp)
        k.ts("dve", aneg, aneg, -1.0, ALU.mult)
        with Scope(k):
            wx = k.sb("m2wx", [128, 8, 768], BF16)
            wz = k.sb("m2wz", [128, 8, 264], BF16)
            cv = k.sb("m2cv", [128, 6, 5], F32)
            k.dma("sp", [(cv, Lw.m2vec)])
            with Scope(k):
                stg = Stager(C, 2112, n=2)
                for j in range(6):
                    stg.load(wx[:, :, j * 128:(j + 1) * 128], Lw.wfm[:, (13 + j) * 128:(14 + j) * 128])
                stg.load(wz, Lw.wtm[:, 256:520])
            xpre = k.sb("m2xpre", [128, 6, T], BF16)
            for bi, (t0, n) in enumerate(TBLK):
                for c6 in range(6):
                    pp = P[c6 % 4]
                    proj_fm(C, hT, wx[:, :, c6 * 128:(c6 + 1) * 128], t0, n, pp)
                    k.copy("dve" if c6 % 2 == 0 else "act", xpre[:, c6, t0:t0 + n], pp[:, 0:n])
            acc = [k.sb("m2acc%d" % i, [128, T], F32) for i in range(2)]
            for c6 in range(6):
                a = acc[c6 % 2]
                en = "dve"
                k.ts(en, a, xpre[:, c6, :], cv[:, c6, 2:3], ALU.mult, cv[:, c6, 4:5], ALU.add)
                for (lo, hi) in ((0, CTX), (CTX, T)):
                    k.stt(a[:, lo + 2:hi], xpre[:, c6, lo:hi - 2], cv[:, c6, 0:1], a[:, lo + 2:hi], ALU.mult, ALU.add)
                    k.stt(a[:, lo + 1:hi], xpre[:, c6, lo:hi - 1], cv[:, c6, 1:2], a[:, lo + 1:hi], ALU.mult, ALU.add)
                    k.stt(a[:, lo:hi - 1], xpre[:, c6, lo + 1:hi], cv[:, c6, 3:4], a[:, lo:hi - 1], ALU.mult, ALU.add)
                k.act(xc[:, c6, :], a, AF.Silu)
            tmpd = [k.sb("m2tmpd%d" % i, [128, 8], F32) for i in range(2)]
            for ti in range(NT):
                pz = P[4 + (ti % 2)]
                for kk in range(8):
                    k.mm(pz[:, 0:264], hT[:, kk, ti * 128:(ti + 1) * 128], wz[:, kk, :], start=(kk == 0), stop=(kk == 7))
                k.act(zs[:, ti, :], pz[:, 0:256], AF.Silu)
                td = tmpd[ti % 2]
                k.tt("dve", td, pz[:, 256:264], row[:, 8:16], ALU.add)
                k.act(td, td, AF.Exp)
                k.act(dt[:, ti, :], td, AF.Ln, bias=1.0)
            k.tt("dve", dA, dt, bc(aneg, [128, NT, 8], 1), ALU.mult)
            for ti in range(NT):
                pt = P[6 + (ti % 2)]
                ptv = pt.v(pt.ap.bitcast(BF16)[:, 0:512].rearrange("p (c t) -> p c t", t=128))
                for c4 in range(4):
                    k.tr(ptv[:, c4, :], xc[:, c4, ti * 128:(ti + 1) * 128], C.ident_b)
                k.copy("dve" if ti % 2 == 0 else "act", xtok[:, ti, :], pt.v(pt.ap.bitcast(BF16)[:, 0:512]))
        dbg_tap(C, "m2xc%d" % l, xc, [128, 6, T], BF16)
        dbg_tap(C, "m2dt%d" % l, dt, [128, NT, 8])
        dsk = row[:, 16:20]
        k.tt("dve", ysum.v(ysum.ap.rearrange("p t (h d) -> p t h d", h=4)),
             xtok.v(xtok.ap[:, :, 0:256].rearrange("p t (h d) -> p t h d", h=4)),
             row.v(row.ap[:, 16:20].unsqueeze(1).unsqueeze(3).to_broadcast([128, NT, 4, 64])), ALU.mult)
        with Scope(k):
            Sst = k.sb("m2S", [128, 8, 64], F32)
            Sbf = k.sb("m2Sb", [128, 8, 64], BF16)
            k.memset("dve", Sst, 0.0)
            k.memset("dve", Sbf, 0.0)
            GTm = [k.sb("m2GT%d" % i, [128, 2, 128], F32) for i in range(2)]
            lhs = [k.sb("m2lhs%d" % i, [128, 128], F32) for i in range(2)]
            ex = [k.sb("m2ex%d" % i, [128, 128], F32) for i in range(2)]
            MT = [k.sb("m2MT%d" % i, [128, 128], BF16) for i in range(2)]
            xd = [k.sb("m2xd%d" % i, [128, 4, 64], BF16) for i in range(2)]
            xw = [k.sb("m2xw%d" % i, [128, 4, 64], BF16) for i in range(2)]
            yo = [k.sb("m2yo%d" % i, [128, 4, 64], F32) for i in range(2)]
            sc8 = [k.sb("m2sc%d" % i, [128, 4, 8], F32) for i in range(2)]
            it = 0
            n2 = 0
            units = [(ci, d) for ci in range(NT) for d in range(2)]

            def pro_heads(ci, d, mid=None):
                dsl = slice(d * 4, (d + 1) * 4)
                c = S5_ORDER[d][ci]
                cs = slice(c * 128, (c + 1) * 128)
                b2 = d
                pc = P[0]
                k.mm(pc[:, 0:8], C.ones_f, dA[:, c, :])
                k.mm(pc[:, 8:16], LE if d == 0 else GE, dA[:, c, :])
                sc = sc8[b2]
                k.copy("act", sc[:, 0, :], pc[:, 0:8])
                k.act(sc[:, 1, :], pc[:, 0:8], AF.Exp)
                k.act(sc[:, 2, :], pc[:, 8:16], AF.Exp)
                k.tt("dve", sc[:, 3, :], sc[:, 0, :], pc[:, 8:16], ALU.subtract)
                k.act(sc[:, 3, :], sc[:, 3, :], AF.Exp)
                gt = GTm[b2]
                for g in range(2):
                    k.mm(P[1][:, g * 128:(g + 1) * 128], xc[:, 2 + g, cs], xc[:, 4 + g, cs])
                pgv = P[1].v(P[1].ap[:, 0:256].rearrange("p (g l) -> p g l", g=2))
                k.tt("dve", gt, pgv, bc(msk[:, d, :], [128, 2, 128], 1), ALU.mult)
                xv = xtok.v(xtok.ap[:, c, 0:256].rearrange("p (h e) -> p h e", h=4))
                k.tt("dve", xd[b2], xv, bc(dt[:, c, dsl], [128, 4, 64], 2), ALU.mult)
                k.tt("pool", xw[b2], xd[b2], bc(sc[:, 3, dsl], [128, 4, 64], 2), ALU.mult)
                PY, PS = P[4 + 2 * b2], P[5 + 2 * b2]
                for h in range(4):
                    g = h // 2
                    dh = d * 4 + h
                    b = h % 2
                    k.act(lhs[b], SU if d == 0 else SL, AF.Copy, scale=dA[:, c, dh:dh + 1])
                    ps_ = P[2 + b]
                    k.mm(ps_[:, 0:128], lhs[b], LE if d == 0 else GE)
                    k.act(ex[b], ps_[:, 0:128], AF.Exp)
                    k.tt("dve", MT[b], ex[b], gt[:, g, :], ALU.mult)
                    k.mm(PY[:, h * 64:(h + 1) * 64], MT[b], xd[b2][:, h, :])
                    k.mm(PY[:, 256 + h * 64:256 + (h + 1) * 64], xc[:, 4 + g, cs], Sbf[:, dh, :])
                    k.mm(PS[:, h * 64:(h + 1) * 64], xtok[:, c, 256 + g * 128:256 + (g + 1) * 128], xw[b2][:, h, :])
                    if h == 1 and mid is not None:
                        mid()

            def epi(ci, d):
                dsl = slice(d * 4, (d + 1) * 4)
                c = S5_ORDER[d][ci]
                b2 = d
                sc = sc8[b2]
                PY, PS = P[4 + 2 * b2], P[5 + 2 * b2]
                pyo = PY.v(PY.ap[:, 256:512].rearrange("p (h e) -> p h e", h=4))
                k.tt("dve", yo[b2], pyo, bc(sc[:, 2, dsl], [128, 4, 64], 2), ALU.mult)
                ysl = ysum[:, c, :]
                k.tt("dve", ysl, ysl, PY[:, 0:256], ALU.add)
                k.tt("pool", ysl, ysl, yo[b2].v(yo[b2].ap.rearrange("p h e -> p (h e)")), ALU.add)
                Sd = Sst[:, dsl, :]
                k.tt("pool", Sd, Sd, bc(sc[:, 1, dsl], [128, 4, 64], 2), ALU.mult)
                k.tt("dve", Sd, Sd, PS.v(PS.ap[:, 0:256].rearrange("p (h e) -> p h e", h=4)), ALU.add)
                k.copy("act", Sbf[:, dsl, :], Sd)
            for i, (ci, d) in enumerate(units):
                pro_heads(ci, d)
                if i >= 1:
                    epi(*units[i - 1])
            epi(*units[-1])
        dbg_tap(C, "m2ys%d" % l, ysum, [128, NT, 256])
        with Scope(k):
            OT = k.sb("OTm2", [128, 2, T], BF16)
            yg = [k.sb("m2yg%d" % i, [128, 256], F32) for i in range(2)]
            yb = [k.sb("m2yb%d" % i, [128, 256], BF16) for i in range(2)]
            junk = k.sb("m2junk", [128, 256], BF16)
            st = [k.sb("m2st%d" % i, [128, 2], F32) for i in range(2)]
            for ti in range(NT):
                y, s = yg[ti % 2], st[ti % 2]
                k.tt("dve", y, ysum[:, ti, :], zs[:, ti, :], ALU.mult)
                k.act(junk, y, AF.Square, accum=s[:, 0:1])
                k.act(s[:, 1:2], s[:, 0:1], AF.Sqrt, bias=EPS, scale=1.0 / 256)
                k.recip(s[:, 1:2], s[:, 1:2])
                k.stt(yb[ti % 2], y, s[:, 1:2], row[:, 20:276], ALU.mult, ALU.mult)
                pt = P[6 + (ti % 2)]
                ptv = pt.v(pt.ap.bitcast(BF16)[:, 0:256].rearrange("p (c t) -> p c t", t=128))
                for q in range(2):
                    k.tr(ptv[:, q, :], yb[ti % 2][:, q * 128:(q + 1) * 128], C.ident_b)
                k.copy("act", OT[:, :, ti * 128:(ti + 1) * 128], ptv)
            dbg_tap(C, "OTm2%d" % l, OT, [128, 2, T], BF16)
            k.dma("sp", [(otd[6:8].v(otd.ap[6:8].rearrange("c p t -> p c t")), OT)])


def host_merge_inputs(inp, l):
    o = {}
    o["wbr"] = np.ascontiguousarray(inp["w_branch"][l].reshape(1024, 1024))
    o["wout"] = np.ascontiguousarray(inp["w_out"][l])
    return o


def merge_phase(C, l, hT, yT):
    k = C.k
    Lw = C.L[l]
    P = C.ps
    with Scope(k):
        OTall = k.sb("OTall", [128, 8, T], BF16)
        k.dma("sp", [(OTall, C.otd.v(C.otd.ap.rearrange("c p t -> p c t")))])
        wb = k.sb("wb", [128, 8, 1024], BF16)
        wg = k.sb("wg", [128, 8, 4, 512], BF16)
        stg = Stager(C, 4096, n=2)
        for hh in range(2):
            stg.load(wb[:, :, hh * 512:(hh + 1) * 512], Lw.wbr[:, hh * 512:(hh + 1) * 512])
        sg = [k.sb("msg%d" % i, [128, 512], F32) for i in range(2)]
        ya = [k.sb("mya%d" % i, [128, 512], F32) for i in range(2)]
        yo = [k.sb("myo%d" % i, [128, 512], BF16) for i in range(2)]
        it = 0
        nq = 0
        for half in range(2):
            for j in range(4):
                c0 = 2472 + j * 1024 + half * 512
                stg.load(wg[:, :, j, :], Lw.w_in[:, c0:c0 + 512])
            for n4 in range(4):
                nch = half * 4 + n4
                for (t0, n) in (TBLK if C.ctx_out else TBLK_LAT):
                    y = ya[nq % 2]
                    nq += 1
                    for j in range(4):
                        b = it % 2
                        it += 1
                        pg, pb = P[2 * b], P[2 * b + 1]
                        for kk in range(8):
                            k.mm(pg[:, 0:n], wg[:, kk, j, n4 * 128:(n4 + 1) * 128], hT[:, kk, t0:t0 + n],
                                 start=(kk == 0), stop=(kk == 7))
                        for c in range(2):
                            k.mm(pb[:, 0:n], wb[:, 2 * j + c, nch * 128:(nch + 1) * 128], OTall[:, 2 * j + c, t0:t0 + n],
                                 start=(c == 0), stop=(c == 1))
                        k.act(sg[b][:, 0:n], pg[:, 0:n], AF.Sigmoid)
                        if j == 0:
                            k.tt("dve", y[:, 0:n], sg[b][:, 0:n], pb[:, 0:n], ALU.mult)
                        else:
                            k.tt("dve", sg[b][:, 0:n], sg[b][:, 0:n], pb[:, 0:n], ALU.mult)
                            k.tt("pool", y[:, 0:n], y[:, 0:n], sg[b][:, 0:n], ALU.add)
                    o = yo[nq % 2]
                    k.copy("act", o[:, 0:n], y[:, 0:n])
                    k.dma("sp", [(C.ytd2[nch, :, t0:t0 + n], o[:, 0:n])])


def outproj_phase(C, l, yT):
    k = C.k
    Lw = C.L[l]
    with Scope(k):
        wout = k.sb("wout", [128, 8, 1024], BF16)
        yT = k.sb("yT", [128, 8, T], BF16)
        k.dma("sp", [(yT, C.ytd2.v(C.ytd2.ap.rearrange("c p t -> p c t")))])
        stg = Stager(C, 4096, n=2)
        for hh in range(2):
            stg.load(wout[:, :, hh * 512:(hh + 1) * 512], Lw.wout[:, hh * 512:(hh + 1) * 512])

        def src(ti, dst):
            for hh in range(2):
                for kk in range(8):
                    k.mm(dst[hh], yT[:, kk, ti * 128:(ti + 1) * 128], wout[:, kk, hh * 512:(hh + 1) * 512],
                         start=(kk == 0), stop=(kk == 7))
        residual_epilogue(C, src, C.G1b, "mix%d" % l)


def residual_epilogue(C, src, Gb, tag, final=False):
    k = C.k
    P = C.ps
    with Scope(k):
        xt = [k.sb("ex%d" % i, [128, D], F32) for i in range(2)]
        ft = [k.sb("ef%d" % i, [128, D], F32) for i in range(2)]
        junk = k.sb("ejunk", [128, 512], BF16)
        st = [k.sb("est%d" % i, [128, 4], F32) for i in range(2)]
        for ti in range(2 if (final or not C.ctx_out) else 0, NT):
            v = 1 if ti < 2 else 0
            b = ti % 2
            dst = [P[2 * b], P[2 * b + 1]]
            k.dma("sp", [(xt[b], C.xr[ti])])
            r_ = src(ti, dst)
            if r_ is not None:
                dst = r_
            s = st[b]
            for hh in range(2):
                k.act(junk, dst[hh], AF.Square, accum=s[:, hh:hh + 1])
            k.tt("dve", s[:, 2:3], s[:, 0:1], s[:, 1:2], ALU.add)
            k.act(s[:, 3:4], s[:, 2:3], AF.Sqrt, bias=EPS, scale=1.0 / D)
            k.recip(s[:, 3:4], s[:, 3:4])
            for hh in range(2):
                sl = slice(hh * 512, (hh + 1) * 512)
                k.stt(ft[b][:, sl], dst[hh], s[:, 3:4], Gb[:, v, sl], ALU.mult, ALU.mult)
                k.tt("pool", xt[b][:, sl], xt[b][:, sl], ft[b][:, sl], ALU.add)
            if final:
                k.dma("sp", [(C.out_t[ti - 2], xt[b])])
            else:
                k.dma("sp", [(C.xres_t[ti], xt[b])])
                C.xr[ti] = C.xres_t[ti]
        if final:
            C.final_done = True


def host_ffn_consts():
    sel = np.zeros((128, 16, 128), np.float32)
    for e in range(16):
        sel[e, e, :] = 1.0
    return {"selT": sel}


def ffn_phase(C, l, inp_names=None):
    k = C.k
    Lw = C.L[l]
    P = C.ps
    with Scope(k):
        h2T = k.sb("h2T", [128, 8, T], BF16)
        coefT = k.sb("coefT", [128, T], F32)
        k.memset("pool", coefT, 0.0)
        with Scope(k):
            wr = k.sb("wr", [128, 8, 16], F32)
            k.dma("sp", [(wr, Lw.w_router.v(Lw.w_router.ap.rearrange("(k p) e -> p k e", p=128)))])
            xnf = [k.sb("rxnf%d" % i, [128, D], F32) for i in range(3)]
            h32 = [k.sb("rh32%d" % i, [128, 8, 128], F32) for i in range(3)]
            sm = [k.sb("rsm%d" % i, [128, 20], F32) for i in range(2)]
            aff = [k.sb("raff%d" % i, [128, 16], F32) for i in range(2)]
            A, Bc = C.A2, C.mod[:, 24:32, :]

            def router(i, x, rstd):
                v = 1 if i < 2 else 0
                b = i % 2
                k.ts("pool", xnf[b], x, rstd, ALU.mult)
                for hh in range(2):
                    pt = P[4 + hh]
                    for kk in range(4):
                        kc = hh * 4 + kk
                        k.tr(pt[:, kk * 128:(kk + 1) * 128], xnf[b][:, kc * 128:(kc + 1) * 128], C.ident_f)
                    for kk in range(4):
                        kc = hh * 4 + kk
                        k.ts("dve", h32[b][:, kc, :], pt[:, kk * 128:(kk + 1) * 128], A[:, kc:kc + 1, v], ALU.mult,
                             Bc[:, kc:kc + 1, v], ALU.add)
                pl = P[6]
                for kk in range(8):
                    k.mm(pl[:, 0:16], h32[b][:, kk, :], wr[:, kk, :], start=(kk == 0), stop=(kk == 7))
                s = sm[b]
                k.ins("dve", lambda e: e.reduce_max(out=s[:, 0:1].ap, in_=pl[:, 0:16].ap, axis=AX.X), [pl], [s])
                k.ts("dve", s[:, 1:2], s[:, 0:1], -1.0, ALU.mult)
                k.act(aff[b], pl[:, 0:16], AF.Exp, bias=s[:, 1:2], accum=s[:, 2:3])
                k.recip(s[:, 3:4], s[:, 2:3])
                k.ts("dve", aff[b], aff[b], s[:, 3:4], ALU.mult)
                pa = P[7]
                k.tr(pa[0:16, 0:128], aff[b], C.ident_f)
                k.copy("act", coefT[0:16, i * 128:(i + 1) * 128], pa[0:16, 0:128])
            norm_to_hT(C, h2T, A, Bc, "ffn", extra=router)
            dbg_tap(C, "aff%d" % l, coefT[0:16, :], [16, T])
            work = k.sb("rwork", [16, T], F32)
            m8 = k.sb("rm8", [16, 8], F32)
            k.copy("dve", work, coefT[0:16, :])
            for (lo, hi, cap) in ((0, CTX, 2 * CTX // 16), (CTX, T, 2 * SEQ // 16)):
                wv_ = work[:, lo:hi]
                for r in range(cap // 8):
                    k.ins("dve", lambda e, w=wv_: e.max(out=m8.ap, in_=w.ap), [wv_], [m8])
                    if r < cap // 8 - 1:
                        k.ins("dve", lambda e, w=wv_: e.match_replace(out=w.ap, in_to_replace=m8.ap, in_values=w.ap,
                                                                     imm_value=-1.0), [m8, wv_], [wv_])
                msk = k.sb("rmsk", [16, hi - lo], F32)
                k.ts("dve", msk, coefT[0:16, lo:hi], m8[:, 7:8], ALU.is_ge)
                k.tt("dve", coefT[0:16, lo:hi], coefT[0:16, lo:hi], msk, ALU.mult)
        dbg_tap(C, "coefT%d" % l, coefT[0:16, :], [16, T])
        dbg_tap(C, "h2T%d" % l, h2T, [128, 8, T], BF16)
        if DEVSTOP == "router":
            return
        with Scope(k):
            sel = k.sb("selT", [128, 16, 128], F32)
            k.dma("sp", [(sel, C.selT_in)])
            Wg = k.sb("Wg", [128, 8, 1024], BF16)
            Wu = k.sb("Wu", [128, 8, 1024], BF16)
            Wd = k.sb("Wd", [128, 8, 1024], BF16)
            stg = Stager(C, 4096, n=2)
            cbs = [k.sb("fcb%d" % i, [128, 512], F32) for i in range(2)]
            actT = [k.sb("factT%d" % i, [128, 8, 512], BF16) for i in range(2)]
            sg = [k.sb("fsg%d" % i, [128, 512], F32) for i in range(2)]
            ost = [k.sb("fost%d" % i, [128, D], F32) for i in range(2)]
            it = 0
            nexp = DEVNEXP
            for e in range(nexp):
                for (Wt, src) in ((Wg, Lw.w_gate), (Wu, Lw.w_up), (Wd, Lw.w_down)):
                    for hh in range(2):
                        stg.load(Wt[:, :, hh * 512:(hh + 1) * 512], src[e, :, hh * 512:(hh + 1) * 512])
                for bi, (t0, n) in enumerate(TBLK):
                    cb = cbs[bi % 2]
                    at = actT[bi % 2]
                    pc = P[6]
                    k.mm(pc[:, 0:n], sel[:, e, :], coefT[:, t0:t0 + n])
                    k.copy("act", cb[:, 0:n], pc[:, 0:n])
                    for f in range(8):
                        b = it % 2
                        it += 1
                        pg, pu = P[2 * b], P[2 * b + 1]
                        proj_fm(C, h2T, Wg[:, :, f * 128:(f + 1) * 128], t0, n, pg)
                        proj_fm(C, h2T, Wu[:, :, f * 128:(f + 1) * 128], t0, n, pu)
                        k.act(sg[b][:, 0:n], pg[:, 0:n], AF.Silu)
                        k.tt("dve", sg[b][:, 0:n], sg[b][:, 0:n], pu[:, 0:n], ALU.mult)
                        k.tt("pool", at[:, f, 0:n], sg[b][:, 0:n], cb[:, 0:n], ALU.mult)
                    for j in range(n // 128):
                        ti = t0 // 128 + j
                        o = ost[ti % 2]
                        for hh in range(2):
                            po = P[4 + hh]
                            for f in range(8):
                                k.mm(po, at[:, f, j * 128:(j + 1) * 128], Wd[:, f, hh * 512:(hh + 1) * 512],
                                     start=(f == 0), stop=(f == 7))
                            k.copy("act" if hh == 0 else "dve", o[:, hh * 512:(hh + 1) * 512], po)
                        k.dma("pool", [(C.facc_t[ti], o)], accum=(e > 0))
    with Scope(k):
        fin = [k.sb("ffin%d" % i, [128, D], F32) for i in range(2)]

        def src(ti, dst):
            f = fin[ti % 2]
            k.dma("pool", [(f, C.facc_t[ti])])
            return [f[:, 0:512], f[:, 512:1024]]
        residual_epilogue(C, src, C.G2b, "ffn%d" % l, final=(l == C.last_layer and "xres" not in C.dbg_names))


CSEL = 2 * CTX // 16 + 2 * SEQ // 16


def host_ffn_consts2():
    ic = np.broadcast_to(np.arange(CSEL, dtype=np.float32)[None, :], (128, CSEL))
    ip = np.arange(128, dtype=np.float32)[:, None] + 128.0 * np.arange(3, dtype=np.float32)[None, :]
    return {"iotac": np.ascontiguousarray(ic), "iotap": np.ascontiguousarray(ip)}


def ffn_phase_gather(C, l):
    k = C.k
    Lw = C.L[l]
    P = C.ps
    A, Bc = C.A2, C.mod[:, 24:32, :]
    with Scope(k):
        xn_tok = k.sb("xn_tok", [128, NT, D], BF16)
        coefT = k.sb("coefT", [128, T], F32)
        posr = k.sb("posr", [128, T], F32)
        tokc = k.sb("tokc", [128, NT, 32], F32)
        k.memset("pool", coefT, 0.0)
        k.memset("pool", posr, 0.0)
        with Scope(k):
            wr = k.sb("wr", [128, 8, 16], F32)
            k.dma("sp", [(wr, Lw.w_router.v(Lw.w_router.ap.rearrange("(k p) e -> p k e", p=128)))])
            xin = [k.sb("gx%d" % i, [128, D], F32) for i in range(3)]
            junk = k.sb("gjunk", [128, D], BF16)
            st = [k.sb("gst%d" % i, [128, 2], F32) for i in range(3)]
            xnf = [k.sb("rxnf%d" % i, [128, D], F32) for i in range(3)]
            h32 = [k.sb("rh32%d" % i, [128, 8, 128], F32) for i in range(3)]
            sm = [k.sb("rsm%d" % i, [128, 20], F32) for i in range(2)]
            aff = [k.sb("raff%d" % i, [128, 16], F32) for i in range(2)]
            def stA(i):
                x, s_ = xin[i % 3], st[i % 3]
                k.dma("sp", [(x, C.xr[i])])
                k.act(junk, x, AF.Square, accum=s_[:, 0:1])
                k.act(s_[:, 1:2], s_[:, 0:1], AF.Sqrt, bias=EPS, scale=1.0 / D)
                k.recip(s_[:, 1:2], s_[:, 1:2])
                k.ts("dve", xn_tok[:, i, :], x, s_[:, 1:2], ALU.mult)
                k.act(xnf[i % 3], x, AF.Identity, scale=s_[:, 1:2])

            def stB(i):
                v = 1 if i < 2 else 0
                b = i % 3
                for hh in range(2):
                    pt = P[4 + hh]
                    for kk in range(4):
                        kc = hh * 4 + kk
                        k.tr(pt[:, kk * 128:(kk + 1) * 128], xnf[b][:, kc * 128:(kc + 1) * 128], C.ident_f)
                    for kk in range(4):
                        kc = hh * 4 + kk
                        if hh == 0:
                            k.ts("dve", h32[b][:, kc, :], pt[:, kk * 128:(kk + 1) * 128], A[:, kc:kc + 1, v], ALU.mult,
                                 Bc[:, kc:kc + 1, v], ALU.add)
                        else:
                            k.act(h32[b][:, kc, :], pt[:, kk * 128:(kk + 1) * 128], AF.Identity,
                                  bias=Bc[:, kc:kc + 1, v], scale=A[:, kc:kc + 1, v])
                pl = P[6 + (i % 2)]
                for kk in range(8):
                    k.mm(pl[:, 0:16], h32[b][:, kk, :], wr[:, kk, :], start=(kk == 0), stop=(kk == 7))

            def stC(i):
                b = i % 2
                pl = P[6 + (i % 2)]
                sv = sm[b]
                k.ins("dve", lambda e, sv=sv, pl=pl: e.reduce_max(out=sv[:, 0:1].ap, in_=pl[:, 0:16].ap, axis=AX.X), [pl], [sv])
                k.ts("dve", sv[:, 1:2], sv[:, 0:1], -1.0, ALU.mult)
                k.act(aff[b], pl[:, 0:16], AF.Exp, bias=sv[:, 1:2], accum=sv[:, 2:3])
                k.recip(sv[:, 3:4], sv[:, 2:3])
                k.ts("dve", aff[b], aff[b], sv[:, 3:4], ALU.mult)
                pa = P[2 + (i % 2)]
                k.tr(pa[0:16, 0:128], aff[b], C.ident_f)
                k.copy("act", coefT[0:16, i * 128:(i + 1) * 128], pa[0:16, 0:128])
            tiles = list(range(NT)) if C.ctx_out else list(range(2, NT))
            if not C.ctx_out:
                k.memset("pool", xn_tok[:, 0:2, :], 0.0)
            for step in range(len(tiles) + 2):
                if step < len(tiles):
                    stA(tiles[step])
                if 1 <= step <= len(tiles):
                    stB(tiles[step - 1])
                if step >= 2:
                    stC(tiles[step - 2])
            work = k.sb("rwork", [16, T], F32)
            msk = k.sb("rmsk", [16, T], F32)
            m8 = k.sb("rm8", [16, 8], F32)
            k.copy("dve", work, coefT[0:16, :])
            if not C.ctx_out:
                k.memset("dve", msk[:, 0:CTX], 0.0)
            for (lo, hi, cap, off) in ((0, CTX, 2 * CTX // 16, 0), (CTX, T, 2 * SEQ // 16, 2 * CTX // 16)):
                if lo == 0 and not C.ctx_out:
                    continue
                wv_ = work[:, lo:hi]
                for r in range(cap // 8):
                    k.ins("dve", lambda e, w=wv_: e.max(out=m8.ap, in_=w.ap), [wv_], [m8])
                    if r < cap // 8 - 1:
                        k.ins("dve", lambda e, w=wv_: e.match_replace(out=w.ap, in_to_replace=m8.ap, in_values=w.ap,
                                                                     imm_value=-1.0), [m8, wv_], [wv_])
                k.ts("dve", msk[:, lo:hi], coefT[0:16, lo:hi], m8[:, 7:8], ALU.is_ge)
                k.tt("dve", coefT[0:16, lo:hi], coefT[0:16, lo:hi], msk[:, lo:hi], ALU.mult)
                k.scan(posr[0:16, lo:hi], msk[:, lo:hi], msk[:, lo:hi], 0.0, ALU.add, ALU.max)
                k.ts("dve", posr[0:16, lo:hi], posr[0:16, lo:hi], float(off - 1), ALU.add)
            if not C.ctx_out:
                k.memset("dve", tokc[:, 0:2, :], 0.0)
            for ti in (range(NT) if C.ctx_out else range(2, NT)):
                pa = P[6 + (ti % 2)]
                k.tr(pa[:, 0:16], posr[0:16, ti * 128:(ti + 1) * 128], C.ident_f[0:16, 0:16])
                k.tr(pa[:, 16:32], msk[:, ti * 128:(ti + 1) * 128], C.ident_f[0:16, 0:16])
                k.copy("act", tokc[:, ti, :], pa[:, 0:32])
        dbg_tap(C, "coefT%d" % l, coefT[0:16, :], [16, T])
        dbg_tap(C, "posr%d" % l, posr[0:16, :], [16, T])
        dbg_tap(C, "tokc%d" % l, tokc, [128, NT, 32])
        with Scope(k):
            sel = k.sb("selT", [128, 16, 128], F32)
            iotac = k.sb("iotac", [128, CSEL], F32)
            iotap = k.sb("iotap", [128, 3], F32)
            k.dma("sp", [(sel, C.selT_in), (iotac, C.iotac_in), (iotap, C.iotap_in)])
            Wg = k.sb("Wg", [128, 8, 1024], BF16)
            Wu = k.sb("Wu", [128, 8, 1024], BF16)
            Wd = k.sb("Wd", [128, 8, 1024], BF16)
            stg = Stager(C, 2048, n=3)
            Sel = k.sb("Sel", [128, NT, CSEL], BF16)
            SelT2 = [k.sb("SelT%d" % i, [128, 3, T], BF16) for i in range(2)]
            xsT = k.sb("xsT", [128, 8, CSEL], BF16)
            actT = k.sb("actT", [128, 8, 384], BF16)
            k.memset("pool", actT, 0.0)
            ys2 = [k.sb("ys%d" % i, [128, 3, D], BF16) for i in range(2)]
            sg = [k.sb("fsg%d" % i, [128, CSEL], F32) for i in range(2)]
            cbs = [k.sb("fcb%d" % i, [128, 512], F32) for i in range(2)]
            ost = [k.sb("fost%d" % i, [128, D], F32) for i in range(2)]
            it = 0
            nld = [0]

            def load_w(e, Wt, src):
                if "W" in DEVFLAGS and e > 0:
                    return
                for qq in range(2):
                    srcv = src[e, :, qq * 512:(qq + 1) * 512]
                    k.dma("pool", [(Wt[:, :, qq * 512:(qq + 1) * 512], srcv.v(srcv.ap.rearrange("(k p) n -> p k n", p=128)))])
            load_w(0, Wg, Lw.w_gate)
            load_w(0, Wu, Lw.w_up)
            load_w(0, Wd, Lw.w_down)
            ne = DEVNEXP
            c0 = 2 * CTX // 16

            def st_sel(e):
                for ti in range(NT):
                    k.ts("dve", Sel[:, ti, :], iotac, tokc[:, ti, e:e + 1], ALU.is_equal,
                         tokc[:, ti, 16 + e:17 + e], ALU.mult)

            def st_gather(e):
                for kk in range(8):
                    pgt = P[kk % 2]
                    for ti in range(NT):
                        k.mm(pgt[:, 0:CSEL], xn_tok[:, ti, kk * 128:(kk + 1) * 128], Sel[:, ti, :],
                             start=(ti == 0), stop=(ti == NT - 1))
                    k.act(xsT[:, kk, 0:c0], pgt[:, 0:c0], AF.Identity, bias=Bc[:, kk:kk + 1, 1], scale=A[:, kk:kk + 1, 1])
                    k.act(xsT[:, kk, c0:CSEL], pgt[:, c0:CSEL], AF.Identity, bias=Bc[:, kk:kk + 1, 0], scale=A[:, kk:kk + 1, 0])

            def st_gateup(e):
                for f in range(8):
                    b = f % 2
                    pg, pu = P[2 + 2 * b], P[3 + 2 * b]
                    for kk in range(8):
                        k.mm(pg[:, 0:CSEL], Wg[:, kk, f * 128:(f + 1) * 128], xsT[:, kk, :], start=(kk == 0), stop=(kk == 7))
                    for kk in range(8):
                        k.mm(pu[:, 0:CSEL], Wu[:, kk, f * 128:(f + 1) * 128], xsT[:, kk, :], start=(kk == 0), stop=(kk == 7))
                    k.act(sg[b], pg[:, 0:CSEL], AF.Silu)
                    k.tt("dve", actT[:, f, 0:CSEL], sg[b], pu[:, 0:CSEL], ALU.mult)

            def st_down(e):
                ys = ys2[e % 2]
                for ct in range(3):
                    for hh in range(2):
                        po = P[6 + hh]
                        for f in range(8):
                            k.mm(po, actT[:, f, ct * 128:(ct + 1) * 128], Wd[:, f, hh * 512:(hh + 1) * 512],
                                 start=(f == 0), stop=(f == 7))
                        k.copy("dve" if hh == 0 else "act", ys[:, ct, hh * 512:(hh + 1) * 512], po)

            def st_selT(e):
                ST = SelT2[e % 2]
                for bi, (t0, n) in enumerate(TBLK):
                    pp, pc = P[2 + 2 * (bi % 2)], P[3 + 2 * (bi % 2)]
                    k.mm(pp[:, 0:n], sel[:, e, :], posr[:, t0:t0 + n])
                    k.mm(pc[:, 0:n], sel[:, e, :], coefT[:, t0:t0 + n])
                    cb = cbs[bi % 2]
                    k.copy("act", cb[:, 0:n], pc[:, 0:n])
                    for ct in range(3):
                        k.stt(ST[:, ct, t0:t0 + n], pp[:, 0:n], iotap[:, ct:ct + 1], cb[:, 0:n], ALU.is_equal, ALU.mult)

            def st_scatter(e):
                for ti in range(NT):
                    o = ost[ti % 2]
                    for hh in range(2):
                        po = P[4 + (ti % 2) * 2 + hh]
                        n_ = 0
                        for ee in (e - 1, e):
                            for ct in range(3):
                                k.mm(po, SelT2[ee % 2][:, ct, ti * 128:(ti + 1) * 128], ys2[ee % 2][:, ct, hh * 512:(hh + 1) * 512],
                                     start=(n_ == 0), stop=(n_ == 5))
                                n_ += 1
                        k.copy("dve" if hh == 0 else "act", o[:, hh * 512:(hh + 1) * 512], po)
                    k.dma("pool", [(C.facc_t[ti], o)], accum=(e > 1))
            st_sel(0)
            st_gather(0)
            for e in range(ne):
                st_gateup(e)
                if e + 1 < ne:
                    load_w(e + 1, Wg, Lw.w_gate)
                    load_w(e + 1, Wu, Lw.w_up)
                    st_sel(e + 1)
                st_down(e)
                if e + 1 < ne:
                    load_w(e + 1, Wd, Lw.w_down)
                    st_gather(e + 1)
                st_selT(e)
                if e % 2 == 1:
                    st_scatter(e)
    with Scope(k):
        fin = [k.sb("ffin%d" % i, [128, D], F32) for i in range(2)]

        def src(ti, dst):
            f = fin[ti % 2]
            k.dma("pool", [(f, C.facc_t[ti])])
            return [f[:, 0:512], f[:, 512:1024]]
        residual_epilogue(C, src, C.G2b, "ffn%d" % l, final=(l == C.last_layer and "xres" not in C.dbg_names))


def make_in_maps(inp):
    inp = {n: np.asarray(v) for n, v in inp.items()}
    shared = dict(host_consts())
    for l in range(DEPTH):
        for fn in (host_layer_inputs, host_mixer_inputs, host_s5_inputs, host_m2_inputs, host_merge_inputs):
            for n, v in fn(inp, l).items():
                shared["%s%d" % (n, l)] = np.ascontiguousarray(v)
        for n in ("w_router", "w_gate", "w_up", "w_down"):
            shared["%s%d" % (n, l)] = np.ascontiguousarray(inp[n][l])
    cc = col_layout(inp["c_ctx"])
    maps = []
    for b in range(inp["x"].shape[0]):
        m = dict(shared)
        m["x"] = np.ascontiguousarray(inp["x"][b])
        m["ctx"] = np.ascontiguousarray(inp["ctx"][b])
        m["cvec"] = np.ascontiguousarray(np.stack([col_layout(inp["c"][b]), cc], -1))
        maps.append(m)
    return maps


def kernel(**inputs):
    maps = make_in_maps(inputs)
    nc, C = build_program()
    res = run_bass_kernel_spmd(nc, maps, core_ids=list(range(len(maps))))
    return np.stack([np.asarray(r["out"], dtype=np.float32) for r in res.results], 0)
```
